# Optimizing a Trainium2 kernel written in Bass

```python
import math
import numpy as np
import jax
import jax.numpy as jnp
from jax import lax

D_MODEL = 1024
BATCH = 32
SEQ = 2048
DEPTH = 2

PLE_DIM = 256
CONV_WIDTH = 4
RMS_EPS = 1e-6
DN_HEADS = 6
DN_HEAD_DIM = 128
DN_WIDTH = DN_HEADS * DN_HEAD_DIM
DN_CHUNK = 64
SSM_HEADS = 12
SSM_HEAD_DIM = 64
SSM_WIDTH = SSM_HEADS * SSM_HEAD_DIM
SSM_GROUPS = 2
SSM_STATE = 128
SSM_CHUNK = 64
SSM_XBC = SSM_WIDTH + 2 * SSM_GROUPS * SSM_STATE
ATTN_HEADS = 12
ATTN_HEAD_DIM = 64
DILATION_GROUPS = ((128, 1), (512, 4), (2048, 16))
ATTN_GROUP_HEADS = ATTN_HEADS // len(DILATION_GROUPS)
ATTN_OUT_WIDTH = ATTN_GROUP_HEADS * ATTN_HEAD_DIM
ATTN_BLOCK = 128
ALIBI_MAX_BIAS = 8.0
N_BRANCHES = 3
FFN_DIM = 2816
N_EXPERTS = 8
TOP_K = 2
EXPERT_DIM = 3584
IN_SPLIT_SIZES = (3 * DN_WIDTH, DN_WIDTH, DN_HEADS, DN_HEADS, SSM_XBC, SSM_WIDTH, SSM_HEADS, 3 * ATTN_HEADS * ATTN_HEAD_DIM, N_BRANCHES * D_MODEL)
IN_WIDTH = sum(IN_SPLIT_SIZES)

kernel_name = 'hybrid_deltanet_ssd_dilated_attn_moe'

F32 = jnp.float32


def rmsnorm(x, g):
    xf = x.astype(F32)
    y = xf * lax.rsqrt(jnp.mean(xf * xf, axis=-1, keepdims=True) + RMS_EPS)
    return (y * g.astype(F32)).astype(x.dtype)


def l2norm(x):
    xf = x.astype(F32)
    return xf * lax.rsqrt(jnp.sum(xf * xf, axis=-1, keepdims=True) + RMS_EPS)


def causal_conv(x, w):
    k_w = w.shape[0]
    s = x.shape[1]
    xp = jnp.pad(x, ((0, 0), (k_w - 1, 0), (0, 0)))
    y = xp[:, k_w - 1:k_w - 1 + s] * w[k_w - 1]
    for j in range(k_w - 1):
        y = y + xp[:, j:j + s] * w[j]
    return y


def swiglu(h, w_gate, w_up, w_down):
    return (jax.nn.silu(h @ w_gate) * (h @ w_up)) @ w_down


def chunked_gated_delta_rule(q, k, v, g, beta):
    bsz, s, h, dk = q.shape
    dv = v.shape[-1]
    c = DN_CHUNK
    n = s // c

    def to_chunks(t):
        return jnp.moveaxis(t.astype(F32).reshape(bsz, n, c, h, *t.shape[3:]), 3, 2)

    q, k, v, g, beta = [to_chunks(t) for t in (q, k, v, g, beta)]
    q = q * dk ** -0.5
    gam = jnp.cumsum(g, axis=-1)
    incl = jnp.tril(jnp.ones((c, c), bool))
    strict = jnp.tril(jnp.ones((c, c), bool), k=-1)
    diff = gam[..., :, None] - gam[..., None, :]
    decay = jnp.where(incl, jnp.exp(jnp.where(incl, diff, 0.0)), 0.0)
    kb = k * beta[..., None]
    a_mat = jnp.where(strict, jnp.einsum('bnhid,bnhjd->bnhij', kb, k) * decay, 0.0)
    lower = a_mat + jnp.eye(c, dtype=F32)
    rhs = jnp.concatenate([kb * jnp.exp(gam)[..., None], v * beta[..., None]], axis=-1)
    sol = lax.linalg.triangular_solve(lower, rhs, left_side=True, lower=True, unit_diagonal=True)
    w_c, u_c = sol[..., :dk], sol[..., dk:]
    q_dec = q * jnp.exp(gam)[..., None]
    attn_qk = jnp.einsum('bnhid,bnhjd->bnhij', q, k) * decay
    g_last = gam[..., -1]
    k_dec = k * jnp.exp(g_last[..., None] - gam)[..., None]

    def step(state, xs):
        w_i, u_i, qd_i, aqk_i, kd_i, gl_i = xs
        v_new = u_i - jnp.einsum('bhcd,bhde->bhce', w_i, state)
        o_i = jnp.einsum('bhcd,bhde->bhce', qd_i, state) + jnp.einsum('bhij,bhje->bhie', aqk_i, v_new)
        state = state * jnp.exp(gl_i)[..., None, None] + jnp.einsum('bhcd,bhce->bhde', kd_i, v_new)
        return state, o_i

    xs = tuple(jnp.moveaxis(t, 1, 0) for t in (w_c, u_c, q_dec, attn_qk, k_dec, g_last))
    _, o = lax.scan(step, jnp.zeros((bsz, h, dk, dv), F32), xs)
    o = jnp.moveaxis(jnp.moveaxis(o, 0, 1), 3, 2)
    return o.reshape(bsz, s, h, dv)


def gated_deltanet(qkv, z, a, b, conv_w, a_log, dt_bias, norm_g):
    bsz, s, _ = qkv.shape
    qkv = jax.nn.silu(causal_conv(qkv, conv_w))
    q, k, v = jnp.split(qkv, 3, axis=-1)
    q = l2norm(q.reshape(bsz, s, DN_HEADS, DN_HEAD_DIM))
    k = l2norm(k.reshape(bsz, s, DN_HEADS, DN_HEAD_DIM))
    v = v.reshape(bsz, s, DN_HEADS, DN_HEAD_DIM)
    g = -jnp.exp(a_log.astype(F32)) * jax.nn.softplus(a.astype(F32) + dt_bias.astype(F32))
    beta = jax.nn.sigmoid(b.astype(F32))
    o = chunked_gated_delta_rule(q, k, v, g, beta)
    o = rmsnorm(o, norm_g) * jax.nn.silu(z.reshape(bsz, s, DN_HEADS, DN_HEAD_DIM).astype(F32))
    return o.reshape(bsz, s, DN_WIDTH).astype(qkv.dtype)


def ssd_chunked(x, dt, a_neg, bm, cm):
    bsz, s, h, pd = x.shape
    grp, ns = bm.shape[2], bm.shape[3]
    hpg = h // grp
    c = SSM_CHUNK
    nc = s // c
    xdt = (x.astype(F32) * dt[..., None]).reshape(bsz, nc, c, grp, hpg, pd)
    a = jnp.moveaxis((dt * a_neg.astype(F32)).reshape(bsz, nc, c, grp, hpg), 2, -1)
    bc = bm.astype(F32).reshape(bsz, nc, c, grp, ns)
    cc = cm.astype(F32).reshape(bsz, nc, c, grp, ns)
    acs = jnp.cumsum(a, axis=-1)
    incl = jnp.tril(jnp.ones((c, c), bool))
    diff = acs[..., :, None] - acs[..., None, :]
    lmat = jnp.where(incl, jnp.exp(jnp.where(incl, diff, 0.0)), 0.0)
    cb = jnp.einsum('bcigs,bcjgs->bcgij', cc, bc)
    y_diag = jnp.einsum('bcgij,bcghij,bcjghp->bcighp', cb, lmat, xdt)
    decay_end = jnp.exp(acs[..., -1:] - acs)
    states = jnp.einsum('bcjgs,bcghj,bcjghp->bcghps', bc, decay_end, xdt)
    chunk_decay = jnp.exp(acs[..., -1])

    def step(state, xs):
        st_i, dec_i = xs
        return state * dec_i[..., None, None] + st_i, state

    _, prev = lax.scan(step, jnp.zeros((bsz, grp, hpg, pd, ns), F32), (jnp.moveaxis(states, 1, 0), jnp.moveaxis(chunk_decay, 1, 0)))
    prev = jnp.moveaxis(prev, 0, 1)
    y_off = jnp.einsum('bcigs,bcghps,bcghi->bcighp', cc, prev, jnp.exp(acs))
    return (y_diag + y_off).reshape(bsz, s, h, pd)


def mamba2_ssd(xbc, z, dt, conv_w, conv_b, a_log, dt_bias, d_skip, norm_g):
    bsz, s, _ = xbc.shape
    xbc = jax.nn.silu(causal_conv(xbc, conv_w) + conv_b)
    xs, bm, cm = jnp.split(xbc, [SSM_WIDTH, SSM_WIDTH + SSM_GROUPS * SSM_STATE], axis=-1)
    x = xs.reshape(bsz, s, SSM_HEADS, SSM_HEAD_DIM)
    bm = bm.reshape(bsz, s, SSM_GROUPS, SSM_STATE)
    cm = cm.reshape(bsz, s, SSM_GROUPS, SSM_STATE)
    dt = jax.nn.softplus(dt.astype(F32) + dt_bias.astype(F32))
    y = ssd_chunked(x, dt, -jnp.exp(a_log.astype(F32)), bm, cm) + d_skip.astype(F32)[:, None] * x.astype(F32)
    y = y.reshape(bsz, s, SSM_GROUPS, SSM_WIDTH // SSM_GROUPS) * jax.nn.silu(z.reshape(bsz, s, SSM_GROUPS, SSM_WIDTH // SSM_GROUPS).astype(F32))
    y = rmsnorm(y, norm_g.reshape(SSM_GROUPS, SSM_WIDTH // SSM_GROUPS))
    return y.reshape(bsz, s, SSM_WIDTH).astype(xbc.dtype)


def alibi_slopes(n_heads):
    return jnp.exp2(-ALIBI_MAX_BIAS * (jnp.arange(n_heads, dtype=F32) + 1.0) / n_heads)


def dilated_window_attention(q, k, v, slopes, window, dil):
    bsz, s, hg, e = q.shape
    sub_len = s // dil
    w_sub = window // dil
    c = ATTN_BLOCK
    nb = -(-sub_len // c)
    lp = nb * c

    def to_sub(t):
        return t.reshape(bsz, sub_len, dil, hg, e).transpose(0, 2, 3, 1, 4)

    qs, ks, vs = to_sub(q), to_sub(k), to_sub(v)
    qb = jnp.pad(qs, ((0, 0), (0, 0), (0, 0), (0, lp - sub_len), (0, 0))).reshape(bsz, dil, hg, nb, c, e)

    def band(t):
        tp = jnp.pad(t, ((0, 0), (0, 0), (0, 0), (c, lp - sub_len), (0, 0))).reshape(bsz, dil, hg, nb + 1, c, e)
        return jnp.concatenate([tp[:, :, :, :-1], tp[:, :, :, 1:]], axis=-2)

    kb, vb = band(ks), band(vs)
    scores = jnp.einsum('bdhnqe,bdhnke->bdhnqk', qb, kb).astype(F32) * e ** -0.5
    qpos = jnp.arange(c)[:, None]
    kpos = jnp.arange(2 * c)[None, :]
    delta = c + qpos - kpos
    key_idx = (jnp.arange(nb)[:, None, None] - 1) * c + kpos[None]
    valid = (delta >= 0) & (delta <= w_sub) & (key_idx >= 0)
    bias = -slopes.astype(F32)[:, None, None] * (delta * dil).astype(F32)
    scores = jnp.where(valid, scores + bias[:, None], -jnp.inf)
    m = jnp.max(scores, axis=-1, keepdims=True)
    ex = jnp.exp(scores - m)
    den = jnp.sum(ex, axis=-1, keepdims=True)
    o = jnp.einsum('bdhnqk,bdhnke->bdhnqe', (ex / den).astype(v.dtype), vb)
    lse = (m + jnp.log(den))[..., 0]
    o = o.reshape(bsz, dil, hg, lp, e)[:, :, :, :sub_len].transpose(0, 3, 1, 2, 4).reshape(bsz, s, hg, e)
    lse = lse.reshape(bsz, dil, hg, lp)[..., :sub_len].transpose(0, 3, 1, 2).reshape(bsz, s, hg)
    return o, lse


def dilated_attention(qkv):
    bsz, s, _ = qkv.shape
    qkv = qkv.reshape(bsz, s, 3, ATTN_HEADS, ATTN_HEAD_DIM)
    q, k, v = qkv[:, :, 0], qkv[:, :, 1], qkv[:, :, 2]
    slopes = alibi_slopes(ATTN_HEADS)
    outs, lses = [], []
    for gi, (window, dil) in enumerate(DILATION_GROUPS):
        hs = slice(gi * ATTN_GROUP_HEADS, (gi + 1) * ATTN_GROUP_HEADS)
        o, lse = dilated_window_attention(q[:, :, hs], k[:, :, hs], v[:, :, hs], slopes[hs], window, dil)
        outs.append(o)
        lses.append(lse)
    wts = jax.nn.softmax(jnp.stack(lses, axis=0), axis=0)
    o = jnp.sum(wts[..., None] * jnp.stack(outs, axis=0).astype(F32), axis=0)
    return o.reshape(bsz, s, ATTN_OUT_WIDTH).astype(qkv.dtype)


def hybrid_mixer(h, w_in, dn_conv, dn_a_log, dn_dt_bias, dn_norm, ssm_conv, ssm_conv_b, ssm_a_log, ssm_dt_bias, ssm_d, ssm_norm, w_br_dn, w_br_ssm, w_br_attn, w_out):
    cuts = np.cumsum(IN_SPLIT_SIZES)[:-1].tolist()
    dn_qkv, dn_z, dn_a, dn_b, ssm_xbc, ssm_z, ssm_dt, attn_qkv, gate_logits = jnp.split(h @ w_in, cuts, axis=-1)
    y_dn = gated_deltanet(dn_qkv, dn_z, dn_a, dn_b, dn_conv, dn_a_log, dn_dt_bias, dn_norm)
    y_ssm = mamba2_ssd(ssm_xbc, ssm_z, ssm_dt, ssm_conv, ssm_conv_b, ssm_a_log, ssm_dt_bias, ssm_d, ssm_norm)
    y_attn = dilated_attention(attn_qkv)
    g_dn, g_ssm, g_attn = jnp.split(jax.nn.sigmoid(gate_logits), N_BRANCHES, axis=-1)
    merged = g_dn * (y_dn @ w_br_dn) + g_ssm * (y_ssm @ w_br_ssm) + g_attn * (y_attn @ w_br_attn)
    return merged @ w_out


def moe_swiglu(h, w_router, w_gate, w_up, w_down):
    logits = (h @ w_router).astype(F32)
    top_vals, top_idx = lax.top_k(logits, TOP_K)
    top_w = jax.nn.softmax(top_vals, axis=-1)
    gates = jnp.sum(jax.nn.one_hot(top_idx, N_EXPERTS, dtype=F32) * top_w[..., None], axis=-2).astype(h.dtype)
    out = gates[..., 0:1] * swiglu(h, w_gate[0], w_up[0], w_down[0])
    for e in range(1, N_EXPERTS):
        out = out + gates[..., e:e + 1] * swiglu(h, w_gate[e], w_up[e], w_down[e])
    return out


def setup_inputs(seed: int = 0) -> dict:
    key = jax.random.key(seed)
    keys = list(jax.random.split(key, 40))
    n_dense = (DEPTH + 1) // 2
    n_moe = DEPTH // 2

    def nxt():
        return keys.pop()

    def nrm(shape, fan_in):
        return jax.random.normal(nxt(), shape, F32) * fan_in ** -0.5

    def gain(shape):
        return 1.0 + 0.1 * jax.random.normal(nxt(), shape, F32)

    def log_a(shape):
        return jnp.log(jax.random.uniform(nxt(), shape, F32, 1.0, 16.0))

    def dt_bias(shape):
        dt = jnp.exp(jax.random.uniform(nxt(), shape, F32, math.log(1e-3), math.log(1e-1)))
        return dt + jnp.log(-jnp.expm1(-dt))

    return {
        'x': jax.random.normal(nxt(), (BATCH, SEQ, D_MODEL), F32),
        'p': jax.random.normal(nxt(), (DEPTH, BATCH, SEQ, PLE_DIM), F32),
        'mix_norm': gain((DEPTH, D_MODEL)),
        'w_in': nrm((DEPTH, D_MODEL, IN_WIDTH), D_MODEL),
        'dn_conv': nrm((DEPTH, CONV_WIDTH, 3 * DN_WIDTH), CONV_WIDTH),
        'dn_a_log': log_a((DEPTH, DN_HEADS)),
        'dn_dt_bias': dt_bias((DEPTH, DN_HEADS)),
        'dn_norm': gain((DEPTH, DN_HEAD_DIM)),
        'ssm_conv': nrm((DEPTH, CONV_WIDTH, SSM_XBC), CONV_WIDTH),
        'ssm_conv_b': 0.01 * jax.random.normal(nxt(), (DEPTH, SSM_XBC), F32),
        'ssm_a_log': log_a((DEPTH, SSM_HEADS)),
        'ssm_dt_bias': dt_bias((DEPTH, SSM_HEADS)),
        'ssm_d': gain((DEPTH, SSM_HEADS)),
        'ssm_norm': gain((DEPTH, SSM_WIDTH)),
        'w_br_dn': nrm((DEPTH, DN_WIDTH, D_MODEL), DN_WIDTH),
        'w_br_ssm': nrm((DEPTH, SSM_WIDTH, D_MODEL), SSM_WIDTH),
        'w_br_attn': nrm((DEPTH, ATTN_OUT_WIDTH, D_MODEL), ATTN_OUT_WIDTH),
        'w_out': nrm((DEPTH, D_MODEL, D_MODEL), D_MODEL),
        'ffn_norm': gain((DEPTH, D_MODEL)),
        'w_ff_gate': nrm((n_dense, D_MODEL, FFN_DIM), D_MODEL),
        'w_ff_up': nrm((n_dense, D_MODEL, FFN_DIM), D_MODEL),
        'w_ff_down': nrm((n_dense, FFN_DIM, D_MODEL), FFN_DIM),
        'w_router': nrm((n_moe, D_MODEL, N_EXPERTS), D_MODEL),
        'w_moe_gate': nrm((n_moe, N_EXPERTS, D_MODEL, EXPERT_DIM), D_MODEL),
        'w_moe_up': nrm((n_moe, N_EXPERTS, D_MODEL, EXPERT_DIM), D_MODEL),
        'w_moe_down': nrm((n_moe, N_EXPERTS, EXPERT_DIM, D_MODEL), EXPERT_DIM),
        'ple_norm': gain((DEPTH, D_MODEL)),
        'w_ple': nrm((DEPTH, PLE_DIM, D_MODEL), PLE_DIM),
        'w_ple_gate': nrm((DEPTH, D_MODEL, D_MODEL), D_MODEL),
        'final_norm': gain((D_MODEL,)),
    }


def reference(x, p, mix_norm, w_in, dn_conv, dn_a_log, dn_dt_bias, dn_norm, ssm_conv, ssm_conv_b, ssm_a_log, ssm_dt_bias, ssm_d, ssm_norm, w_br_dn, w_br_ssm, w_br_attn, w_out, ffn_norm, w_ff_gate, w_ff_up, w_ff_down, w_router, w_moe_gate, w_moe_up, w_moe_down, ple_norm, w_ple, w_ple_gate, final_norm):
    for i in range(DEPTH):
        h = rmsnorm(x, mix_norm[i])
        x = x + hybrid_mixer(h, w_in[i], dn_conv[i], dn_a_log[i], dn_dt_bias[i], dn_norm[i], ssm_conv[i], ssm_conv_b[i], ssm_a_log[i], ssm_dt_bias[i], ssm_d[i], ssm_norm[i], w_br_dn[i], w_br_ssm[i], w_br_attn[i], w_out[i])
        h = rmsnorm(x, ffn_norm[i])
        j = i // 2
        if i % 2 == 0:
            x = x + swiglu(h, w_ff_gate[j], w_ff_up[j], w_ff_down[j])
        else:
            x = x + moe_swiglu(h, w_router[j], w_moe_gate[j], w_moe_up[j], w_moe_down[j])
        ple_gate = jax.nn.sigmoid(rmsnorm(x, ple_norm[i]) @ w_ple_gate[i])
        x = x + (p[i] @ w_ple[i]) * ple_gate
    return rmsnorm(x, final_norm)
```

```python
import numpy as np
import ml_dtypes
import concourse.bass as bass
import concourse.mybir as mybir
from concourse.bass_utils import run_bass_kernel_spmd

F32 = mybir.dt.float32
BF16 = mybir.dt.bfloat16
AF = mybir.ActivationFunctionType
ALU = mybir.AluOpType
AX = mybir.AxisListType

T = 2048
D = 1024
NEG = -30000.0
SAME_ENGINE_SYNC = True
EPOCH_LIMIT = 30000
N_DMA_SEMS = 8
IN_W = 10520
C_DNQ, C_DNK, C_DNV, C_DNZ, C_DNA, C_DNB = 0, 768, 1536, 2304, 3072, 3078
C_SX, C_SB, C_SC, C_SZ, C_SDT = 3084, 3852, 4108, 4364, 5132
C_AQ, C_AK, C_AV, C_GATE = 5144, 5912, 6680, 7448
FF = 2816
EF = 3584
SB_BASE = 16640
SB_LIMIT = 212000


def _I(name, *a, **kw):
    return (name, a, kw)


class Buf:
    __slots__ = ("w", "r", "excl")

    def __init__(self):
        self.w = None
        self.r = {}
        self.excl = False


class TT:
    def __init__(self, t):
        self.t = t
        self.bufs = {}

    def b(self, key=0):
        bb = self.bufs.get(key)
        if bb is None:
            bb = self.bufs[key] = Buf()
        return bb

    def all(self):
        if not self.bufs:
            self.b(0)
        return list(self.bufs.values())


class Queue:
    def __init__(self, name):
        self.name = name
        self.items = []
        self.sig = []
        self.waited_c = {}
        self.waited_d = {}
        self.dma_sems = []
        self.dma_vals = []
        self.dma_rr = 0


class Ctx:
    def __init__(self, nc):
        self.nc = nc
        self.q = {n: Queue(n) for n in ("tensor", "vector", "scalar", "gpsimd", "sync")}
        self._cms = []
        self.nsem = 0
        self.sb_off = SB_BASE
        self.sb_peak = 0
        self.uid = 0
        self.ps_rr = 0
        self.ev_rr = 0

    def enter(self, cm):
        v = cm.__enter__()
        self._cms.append(cm)
        return v

    def new_sem(self, name):
        self.nsem += 1
        return self.enter(self.nc.semaphore(name))

    def close(self):
        for cm in reversed(self._cms):
            cm.__exit__(None, None, None)
        self._cms = []

    def sb(self, name, shape, dtype):
        esz = 2 if dtype == BF16 else 4
        n = 1
        for s in shape[1:]:
            n *= s
        nbytes = (n * esz + 63) // 64 * 64
        off = self.sb_off
        assert off + nbytes <= SB_LIMIT, (name, off, nbytes)
        self.sb_off = off + nbytes
        self.sb_peak = max(self.sb_peak, self.sb_off)
        self.uid += 1
        h = self.nc.alloc_sbuf_tensor_at(f"{name}_{self.uid}", list(shape), dtype, offset=off)
        return TT(h)

    def mark(self):
        return self.sb_off

    def release(self, mark):
        self.barrier()
        self.sb_off = mark

    def _wait_c(self, q, qn2, idx):
        if q.waited_c.get(qn2, -1) >= idx:
            return
        q.waited_c[qn2] = idx
        self.q[qn2].sig[idx] = True
        q.items.append(("waitc", qn2, idx))

    def _wait_d(self, q, sem, val):
        if q.waited_d.get(sem, 0) >= val:
            return
        q.waited_d[sem] = val
        q.items.append(("waitd", sem, val))

    def barrier(self):
        for q in self.q.values():
            for q2 in self.q.values():
                if q2.sig and not (q2.name == "tensor" and q.name == "tensor"):
                    self._wait_c(q, q2.name, len(q2.sig) - 1)
                for s, v in zip(q2.dma_sems, q2.dma_vals):
                    if v:
                        self._wait_d(q, s, v)

    def _dep(self, q, qn, ev, dma):
        if ev[0] == "c":
            _, qn2, idx = ev
            if qn2 == qn and not dma and (qn == "tensor" or not SAME_ENGINE_SYNC):
                return
            self._wait_c(q, qn2, idx)
        else:
            self._wait_d(q, ev[1], ev[2])

    def emit(self, qn, fn, reads=(), writes=(), dma=False):
        q = self.q[qn]
        for b in reads:
            if b.w is not None:
                self._dep(q, qn, b.w, dma)
            if b.excl:
                for ev in b.r.values():
                    if not (ev[0] == "c" and ev[1] == qn):
                        self._dep(q, qn, ev, dma)
        for b in writes:
            if b.w is not None:
                self._dep(q, qn, b.w, dma)
            for ev in b.r.values():
                self._dep(q, qn, ev, dma)
        if dma:
            if not q.dma_sems:
                for i in range(N_DMA_SEMS):
                    q.dma_sems.append(self.new_sem(f"dma_{qn}_{i}"))
                    q.dma_vals.append(0)
            i = q.dma_rr
            q.dma_rr = (i + 1) % len(q.dma_sems)
            sem = q.dma_sems[i]
            if q.dma_vals[i] > 0:
                self._wait_d(q, sem, q.dma_vals[i])
            q.dma_vals[i] += 16
            ev = ("d", sem, q.dma_vals[i])
            key = ("d", id(sem))
            q.items.append(("dma", fn, sem))
        else:
            idx = len(q.sig)
            q.sig.append(False)
            ev = ("c", qn, idx)
            key = ("c", qn)
            q.items.append(("op", fn, idx))
        for b in writes:
            b.w = ev
            b.r = {}
        for b in reads:
            if b.w is ev:
                continue
            b.r[key] = ev
        return ev

    def finish(self):
        self.barrier()
        nc = self.nc
        res = {}
        self.nsig = {}
        for qn, q in self.q.items():
            sem, cnt = None, 0
            tab = [None] * len(q.sig)
            pending = []
            ns = 0
            for idx, sg in enumerate(q.sig):
                pending.append(idx)
                if sg:
                    if sem is None or cnt >= EPOCH_LIMIT:
                        sem = self.new_sem(f"c_{qn}_{self.nsem}")
                        cnt = 0
                    cnt += 1
                    ns += 1
                    for j in pending:
                        tab[j] = (sem, cnt)
                    pending = []
            res[qn] = tab
            self.nsig[qn] = ns
        with nc.Block() as block:
            def run(q):
                def body(eng):
                    for it in q.items:
                        if it[0] == "waitc":
                            sem, val = res[it[1]][it[2]]
                            eng.wait_ge(sem, val)
                        elif it[0] == "waitd":
                            eng.wait_ge(it[1], it[2])
                        elif it[0] == "dma":
                            nm, a, kw = it[1]
                            getattr(eng, nm)(*a, **kw).then_inc(it[2], 16)
                        else:
                            nm, a, kw = it[1]
                            ins = getattr(eng, nm)(*a, **kw)
                            if q.sig[it[2]]:
                                sem, val = res[q.name][it[2]]
                                ins.then_inc(sem, 1)
                return body
            block.tensor(run(self.q["tensor"]))
            block.vector(run(self.q["vector"]))
            block.scalar(run(self.q["scalar"]))
            block.gpsimd(run(self.q["gpsimd"]))
            block.sync(run(self.q["sync"]))
        self.close()


class Prog:
    def __init__(self, n_seq, dbg=None, stop_after=None, layers=(0, 1), skip_w=None):
        self.n_seq = n_seq
        self.skip_w = skip_w
        self.stop = 0
        self.pn_eng = 'vector'
        self.dbg = dbg or set()
        self.stop_after = stop_after
        self.layers = layers
        self.nc = bass.Bass("TRN2", target_bir_lowering=False)
        self.c = Ctx(self.nc)
        self.dram_in = {}
        self.dbg_out = {}

    def din(self, name, shape, dtype=F32):
        t = self.nc.dram_tensor(name, list(shape), dtype, kind="ExternalInput").ap()
        self.dram_in[name] = t
        return t

    def dscr(self, name, shape, dtype):
        return TT(self.nc.dram_tensor(name, list(shape), dtype, kind="Internal").ap())

    def dump(self, name, tt, ap, shape, dtype=F32):
        if name not in self.dbg:
            return
        o = self.nc.dram_tensor("dbg_" + name, list(shape), dtype, kind="ExternalOutput").ap()
        self.dbg_out[name] = o
        self.c.emit("sync", _I("dma_start", out=o, in_=ap), reads=tt.all(), dma=True)

    def op(self, eng, fn, r=(), w=()):
        return self.c.emit(eng, fn, r, w)

    def dma(self, out, in_, r=(), w=(), eng="sync"):
        return self.c.emit(eng, _I("dma_start", out=out, in_=in_), r, w, dma=True)

    def ps(self):
        i = self.c.ps_rr
        self.c.ps_rr = (i + 1) % 8
        return self.PS[i]

    def ev_eng(self):
        self.c.ev_rr ^= 1
        return "scalar" if self.c.ev_rr else "vector"

    def copy(self, eng, out, in_, r, w):
        if eng == "scalar":
            return self.op("scalar", _I("copy", out=out, in_=in_), r, w)
        return self.op(eng, _I("tensor_copy", out=out, in_=in_), r, w)

    def mm(self, out, lhsT, rhs, start, stop, r, w):
        return self.op("tensor", _I("matmul", out, lhsT=lhsT, rhs=rhs, start=start, stop=stop), r, w)

    def tr(self, out, in_, ident, r, w):
        return self.op("tensor", _I("transpose", out=out, in_=in_, identity=ident), r, w)


def _setup(P):
    c, nc, ns = P.c, P.nc, P.n_seq
    P.xT = P.din("xT", [ns, 128, 8, T])
    P.pT = P.din("pT", [2, ns, 128, 2, T])
    P.outT = nc.dram_tensor("outT", [ns, 128, 8, T], F32, kind="ExternalOutput").ap()
    big = {
        "w_in": [2, 128, 8 * IN_W], "w_br_dn": [2, 128, 6 * D], "w_br_ssm": [2, 128, 6 * D],
        "w_br_attn": [2, 64, 4 * D], "w_out": [2, 128, 8 * D], "w_ple_gate": [2, 128, 8 * D],
        "w_ple": [2, 128, 2 * D], "w_ff_gate": [1, 128, 8 * FF], "w_ff_up": [1, 128, 8 * FF],
        "w_ff_down": [1, 128, 22 * D], "w_moe_gate": [8, 128, 8 * EF], "w_moe_up": [8, 128, 8 * EF],
        "w_moe_down": [8, 128, 28 * D],
    }
    P.wsrc = {k: P.din(k, v) for k, v in big.items()}
    P.wb = {k: P.dscr(k + "_b", v, BF16) for k, v in big.items()}
    P.w_router = P.din("w_router", [128, 8 * 8])
    small = {"gains": [128, 56], "dnconv": [128, 144], "dnp6": [6, 4], "dnnorm": [128, 2],
             "ssmconv": [128, 80], "ssmconvb": [128, 20], "ssmp12": [12, 4], "ssmd": [128, 24],
             "ssmnorm": [128, 12], "ident": [128, 128], "masks": [128, 384], "sel6": [6, 768],
             "sel12": [12, 1536], "sel8": [8, 1024]}
    P.abias_d = P.din("abias", [128, 12 * 256])
    P.PS = []
    for i in range(8):
        t = TT(c.enter(nc.psum_tensor(f"psb{i}", [128, 512], F32)))
        t.busy = False
        t.b(0).excl = True
        P.PS.append(t)
    P.k = {}
    for name, shp in small.items():
        src = P.din(name, shp)
        tt = c.sb("k_" + name, shp, F32)
        P.dma(tt.t[:], src, w=tt.all())
        P.k[name] = tt
    tt = c.sb("k_router", [128, 64], F32)
    P.dma(tt.t[:], P.w_router, w=tt.all())
    P.k["router"] = tt
    P.ones_b = c.sb("ones_b", [128, 128], BF16)
    P.op("gpsimd", _I("memset", P.ones_b.t[:], 1.0), w=P.ones_b.all())
    P.ones_f = c.sb("ones_f", [128, 128], F32)
    P.op("gpsimd", _I("memset", P.ones_f.t[:], 1.0), w=P.ones_f.all())
    P.xscr = P.dscr("xscr", [ns, 128, 8, T], F32)
    P.ydn = P.dscr("ydn_s", [128, 6, T], BF16)
    P.yssm = P.dscr("yssm_s", [128, 6, T], BF16)
    P.yattn = P.dscr("yattn_s", [64, 4, T], BF16)
    P.conv_pending = []
    first = [n for n in big if not n.startswith("w_moe")]
    later = [n for n in big if n.startswith("w_moe")]
    for name in first + later:
        if P.skip_w and name in P.skip_w:
            continue
        shp = big[name]
        for j in range(shp[0]):
            src = P.wsrc[name][j]
            dst = P.wb[name].t[j]
            N = shp[2]
            off = 0
            ci = -1
            while off < N:
                ci += 1
                w = min(8192, N - off)
                w -= w % 2048
                if w == 0:
                    w = N - off
                    item = (dst[:, off:off + w], src[:, off:off + w], P.wb[name].b((j, ci)))
                else:
                    item = (dst[:, off:off + w].rearrange("p (a b) -> p a b", b=2048), src[:, off:off + w].rearrange("p (a b) -> p a b", b=2048), P.wb[name].b((j, ci)))
                off += w
                if name in later:
                    P.conv_pending.append(item)
                else:
                    P.c.emit("gpsimd", _I("dma_start", out=item[0], in_=item[1]), writes=[item[2]], dma=True)


def _wbufs(P, name, j):
    return [b for key, b in P.wb[name].bufs.items() if isinstance(key, tuple) and key[0] == j]


def _conv_some(P, n):
    for _ in range(n):
        if not P.conv_pending:
            return
        o, i_, b = P.conv_pending.pop(0)
        P.c.emit("gpsimd", _I("dma_start", out=o, in_=i_), writes=[b], dma=True)


def _ps(P):
    for _ in range(8):
        i = P.c.ps_rr
        P.c.ps_rr = (i + 1) % 8
        if not P.PS[i].busy:
            P.PS[i].busy = True
            return P.PS[i]
    raise RuntimeError("no free PSUM bank")


def _rstd(P, ps_ap, ps_bufs, out_tt, out_ap, scale, extra_bias=0.0):
    P.op("vector", _I("tensor_scalar", out=out_ap, in0=ps_ap, scalar1=scale, scalar2=1e-6, op0=ALU.mult, op1=ALU.add),
         ps_bufs, out_tt.all())
    P.op("scalar", _I("activation", out=out_ap, in_=out_ap, func=AF.Ln), out_tt.all(), out_tt.all())
    if extra_bias != 0.0:
        P.op("scalar", _I("activation", out=out_ap, in_=out_ap, func=AF.Exp, scale=-0.5, bias=float(extra_bias)), out_tt.all(), out_tt.all())
    else:
        P.op("scalar", _I("activation", out=out_ap, in_=out_ap, func=AF.Exp, scale=-0.5), out_tt.all(), out_tt.all())


def _norm_phase(P, s, i, src_ap, src_bufs, gcol, hT):
    c = P.c
    m = c.mark()
    xt = [c.sb(f"np_x{j}", [128, 8, 512], F32) for j in range(2)]
    sq = [c.sb(f"np_sq{j}", [128, 8, 512], BF16) for j in range(2)]
    rs = [c.sb(f"np_rs{j}", [128, 512], F32) for j in range(2)]
    for nt in range(4):
        a, q, r = xt[nt % 2], sq[nt % 2], rs[nt % 2]
        sl = slice(nt * 512, (nt + 1) * 512)
        P.dma(a.t[:], src_ap[:, :, sl], r=src_bufs, w=a.all())
        _rms_tile(P, a, a.t, q, r, gcol, hT.t[:, :, sl], [hT.b(nt)])
    c.release(m)


def _rms_tile(P, x_tt, x_ap, sq, rs, gcol, out_ap, out_bufs, xbufs=None):
    xb = xbufs if xbufs is not None else x_tt.all()
    P.op("scalar", _I("activation", out=sq.t[:], in_=x_ap[:], func=AF.Square), xb, sq.all())
    ps = _ps(P)
    for k in range(8):
        P.mm(ps.t[:], P.ones_b.t[:], sq.t[:, k, :], k == 0, k == 7, P.ones_b.all() + sq.all(), ps.all())
    _rstd(P, ps.t[:], ps.all(), rs, rs.t[:], 1.0 / D)
    ps.busy = False
    g = P.k["gains"]
    for k in range(8):
        P.op("vector", _I("scalar_tensor_tensor", out=out_ap[:, k, :], in0=x_ap[:, k, :], scalar=g.t[:, gcol + k:gcol + k + 1],
                                                         in1=rs.t[:], op0=ALU.mult, op1=ALU.mult),
             xb + rs.all() + g.all(), out_bufs)


def _load_wcols(P, dst, i, col0, ncols):
    src = P.wb["w_in"].t[i].rearrange("p (k n) -> p k n", k=8)[:, :, col0:col0 + ncols]
    P.dma(dst.t[:, :, 0:ncols], src, r=_wbufs(P, "w_in", i), w=dst.all())


def _proj_fm(P, hT, w, wcol0, M, out_fn):
    for nt in range(4):
        ps = _ps(P)
        sl = slice(nt * 512, (nt + 1) * 512)
        for k in range(8):
            P.mm(ps.t[0:M, :], w.t[:, k, wcol0:wcol0 + M], hT.t[:, k, sl], k == 0, k == 7, w.all() + [hT.b(nt)], ps.all())
        out_fn(nt, ps)
        ps.busy = False


def _cumsum64(P, a, b, npart):
    src, dst = a, b
    s = 1
    while s < 64:
        sv = src.t[0:npart, :].rearrange("p (c t) -> p c t", t=64)
        dv = dst.t[0:npart, :].rearrange("p (c t) -> p c t", t=64)
        P.op("vector", _I("tensor_tensor", out=dv[:, :, s:], in0=sv[:, :, s:], in1=sv[:, :, :64 - s], op=ALU.add),
             src.all(), dst.all())
        P.op("gpsimd", _I("tensor_copy", out=dv[:, :, 0:s], in_=sv[:, :, 0:s]), src.all(), dst.all())
        src, dst = dst, src
        s *= 2
    return src


def _deltanet(P, s, i, hT):
    c, k = P.c, P.k
    ident, masks, sel6 = k["ident"], k["masks"], k["sel6"]
    mDA, mDP, mDQ = masks.t[:, 0:128], masks.t[:, 128:256], masks.t[:, 256:384]
    m_phase = c.mark()
    gamT = c.sb("dn_gamT", [6, T], F32)
    gbT = c.sb("dn_gbT", [6, T], F32)
    tok = c.sb("dn_tok", [128, 16, 128], F32)
    bgtok = c.sb("dn_bgtok", [128, 16, 8], F32)
    eglb = c.sb("dn_eglb", [128, 6, 32], F32)
    negA = c.sb("dn_negA", [6, 1], F32)
    m1 = c.mark()
    wab = c.sb("dn_wab", [128, 8, 12], BF16)
    _load_wcols(P, wab, i, C_DNA, 12)
    t0_ = c.sb("dn_t0", [6, T], F32)
    t1_ = c.sb("dn_t1", [6, T], F32)
    lnb = c.sb("dn_lnb", [6, T], F32)
    stk = c.sb("dn_stk", [128, T], F32)
    P.op("gpsimd", _I("memset", stk.t[:], 0.0), w=stk.all())
    p6 = k["dnp6"]
    P.op("scalar", _I("activation", out=negA.t[:], in_=p6.t[:, 2 * i:2 * i + 1], func=AF.Exp), p6.all(), negA.all())
    P.op("vector", _I("tensor_scalar", out=negA.t[:], in0=negA.t[:], scalar1=-1.0, scalar2=None, op0=ALU.mult), negA.all(), negA.all())

    def ev_a(nt, ps):
        sl = slice(nt * 512, (nt + 1) * 512)
        P.op("scalar", _I("activation", out=t0_.t[:, sl], in_=ps.t[0:6, :], func=AF.Exp, bias=p6.t[:, 2 * i + 1:2 * i + 2]),
             ps.all() + p6.all(), t0_.all())
    _proj_fm(P, hT, wab, 0, 6, ev_a)
    P.op("scalar", _I("activation", out=t0_.t[:], in_=t0_.t[:], func=AF.Ln, bias=1.0), t0_.all(), t0_.all())
    P.op("vector", _I("tensor_scalar", out=t0_.t[:], in0=t0_.t[:], scalar1=negA.t[:, 0:1], scalar2=None, op0=ALU.mult),
         t0_.all() + negA.all(), t0_.all())
    gres = _cumsum64(P, t0_, t1_, 6)
    P.copy("vector", gamT.t[:], gres.t[:], gres.all(), gamT.all())

    def ev_b(nt, ps):
        sl = slice(nt * 512, (nt + 1) * 512)
        P.op("scalar", _I("activation", out=lnb.t[:, sl], in_=ps.t[0:6, :], func=AF.Exp, scale=-1.0), ps.all(), lnb.all())
    _proj_fm(P, hT, wab, 6, 6, ev_b)
    P.op("scalar", _I("activation", out=lnb.t[:], in_=lnb.t[:], func=AF.Ln, bias=1.0), lnb.all(), lnb.all())
    P.op("vector", _I("tensor_scalar", out=lnb.t[:], in0=lnb.t[:], scalar1=-1.0, scalar2=None, op0=ALU.mult), lnb.all(), lnb.all())
    P.op("vector", _I("tensor_tensor", out=gbT.t[:], in0=gamT.t[:], in1=lnb.t[:], op=ALU.add), gamT.all() + lnb.all(), gbT.all())
    P.op("vector", _I("tensor_scalar", out=stk.t[0:6, :], in0=gamT.t[:], scalar1=-1.0, scalar2=None, op0=ALU.mult), gamT.all(), stk.all())
    P.copy("vector", stk.t[32:38, :], gbT.t[:], gbT.all(), stk.all())
    g3 = gamT.t[:].rearrange("p (c t) -> p c t", t=64)
    P.op("vector", _I("tensor_tensor", out=t0_.t[:].rearrange("p (c t) -> p c t", t=64), in0=g3[:, :, 63:64].to_broadcast([6, 32, 64]),
                                             in1=g3, op=ALU.subtract), gamT.all(), t0_.all())
    P.op("scalar", _I("activation", out=stk.t[64:70, :], in_=t0_.t[:], func=AF.Exp), t0_.all(), stk.all())
    P.op("scalar", _I("activation", out=stk.t[96:102, :], in_=lnb.t[:], func=AF.Exp), lnb.all(), stk.all())
    for u4 in range(4):
        ps = _ps(P)
        for j in range(4):
            u = u4 * 4 + j
            P.tr(ps.t[:, j * 128:(j + 1) * 128], stk.t[:, u * 128:(u + 1) * 128], ident.t[:], stk.all() + ident.all(), ps.all())
        P.copy("vector", tok.t[:, u4 * 4:(u4 + 1) * 4, :].rearrange("p a b -> p (a b)"), ps.t[:], ps.all(), tok.all())
        ps.busy = False
    P.op("scalar", _I("activation", out=bgtok.t[:, :, 0:6], in_=tok.t[:, :, 32:38], func=AF.Exp), tok.all(), bgtok.all())
    P.op("scalar", _I("activation", out=t1_.t[:, 0:32], in_=g3[:, :, 63], func=AF.Exp), gamT.all(), t1_.all())
    ps = _ps(P)
    for h in range(6):
        P.mm(ps.t[:, h * 32:(h + 1) * 32], sel6.t[:, h * 128:(h + 1) * 128], t1_.t[:, 0:32], True, True, sel6.all() + t1_.all(), ps.all())
    P.copy("vector", eglb.t[:].rearrange("p a b -> p (a b)"), ps.t[:, 0:192], ps.all(), eglb.all())
    ps.busy = False
    c.release(m1)
    if P.stop == 1:
        c.release(m_phase); return
    wq = [c.sb(f"dn_w{j}", [128, 8, 128], BF16) for j in range(4)]
    raw = c.sb("dn_raw", [128, T], F32)
    cq = c.sb("dn_cq", [128, T], F32)
    ck = c.sb("dn_ck", [128, T], F32)
    cv = c.sb("dn_cv", [128, T], F32)
    zs = c.sb("dn_zs", [128, T], F32)
    yT = c.sb("dn_yT", [128, T], BF16)
    S = [c.sb(f"dn_S{j}", [128, 128], F32) for j in range(3)]
    NT = 30
    tmp = [[c.sb(f"dn_tmp{p}_{j}", [128, 128], F32) for j in range(NT)] for p in range(2)]
    ss = [c.sb(f"dn_ss{p}", [128, 2], F32) for p in range(2)]
    for p in range(2):
        P.op("gpsimd", _I("memset", tmp[p][0].t[:], 0.0), w=tmp[p][0].all())
        P.op("gpsimd", _I("memset", tmp[p][1].t[:], 0.0), w=tmp[p][1].all())
    cw = k["dnconv"]
    for h in range(6):
        cols = [C_DNQ + h * 128, C_DNK + h * 128, C_DNV + h * 128, C_DNZ + h * 128]
        for j in range(4):
            _load_wcols(P, wq[j], i, cols[j], 128)
        for j, dst in enumerate((cq, ck, cv)):
            def ev_raw(nt, ps):
                sl = slice(nt * 512, (nt + 1) * 512)
                P.copy(P.ev_eng(), raw.t[:, sl], ps.t[:], ps.all(), raw.all())
            _proj_fm(P, hT, wq[j], 0, 128, ev_raw)
            cb = (i * 18 + j * 6 + h) * 4
            P.op("vector", _I("tensor_scalar", out=dst.t[:], in0=raw.t[:], scalar1=cw.t[:, cb + 3:cb + 4], scalar2=None, op0=ALU.mult),
                 raw.all() + cw.all(), dst.all())
            for sft in (1, 2, 3):
                eng = "vector" if sft != 2 else "gpsimd"
                P.op("vector", _I("scalar_tensor_tensor",
                    out=dst.t[:, sft:], in0=raw.t[:, :T - sft], scalar=cw.t[:, cb + 3 - sft:cb + 4 - sft], in1=dst.t[:, sft:], op0=ALU.mult, op1=ALU.add),
                    raw.all() + cw.all() + dst.all(), dst.all())
            P.op("scalar", _I("activation", out=dst.t[:], in_=dst.t[:], func=AF.Silu), dst.all(), dst.all())
            if j < 2:
                P.op("scalar", _I("activation", out=raw.t[:], in_=dst.t[:], func=AF.Square), dst.all(), raw.all())
                for nt in range(4):
                    sl = slice(nt * 512, (nt + 1) * 512)
                    ps = _ps(P)
                    P.mm(ps.t[:], P.ones_f.t[:], raw.t[:, sl], True, True, P.ones_f.all() + raw.all(), ps.all())
                    rr = tmp[0][2 + nt]
                    _rstd(P, ps.t[:], ps.all(), zs, zs.t[:, sl], 1.0, extra_bias=(-0.5 * np.log(128.0) if j == 0 else 0.0))
                    ps.busy = False
                P.op("vector", _I("tensor_tensor", out=dst.t[:], in0=dst.t[:], in1=zs.t[:], op=ALU.mult), dst.all() + zs.all(), dst.all())

        def ev_z(nt, ps):
            sl = slice(nt * 512, (nt + 1) * 512)
            P.op("scalar", _I("activation", out=zs.t[:, sl], in_=ps.t[:], func=AF.Silu), ps.all(), zs.all())
        _proj_fm(P, hT, wq[3], 0, 128, ev_z)
        if P.stop == 2:
            c.release(m_phase); return
        Sc = S[0]
        P.op("gpsimd", _I("memset", Sc.t[:], 0.0), w=Sc.all())
        si = 0
        for u in range(16):
            tp = tmp[u % 2]
            qdA, qdB = tp[0], tp[1]
            (tA, tB, tC, DA, DP, DQ, Eb, kbg, kdec, vb, A0, P0, attnT, X0, X1, A1, A2, P1, P2, TTt, WT, U, vnew, o_n) = tp[2:26]
            ssu = ss[u % 2]
            t0 = u * 128
            usl = slice(t0, t0 + 128)
            ngam = tok.t[:, u, h:h + 1]
            gbt = tok.t[:, u, 32 + h:33 + h]
            kd = tok.t[:, u, 64 + h:65 + h]
            beta = tok.t[:, u, 96 + h:97 + h]
            bg = bgtok.t[:, u, h:h + 1]
            selh = sel6.t[:, h * 128:(h + 1) * 128]
            psb = _ps(P)
            P.mm(psb.t[:, 0:128], selh, gamT.t[:, usl], True, True, sel6.all() + gamT.all(), psb.all())
            P.mm(psb.t[:, 128:256], selh, gbT.t[:, usl], True, True, sel6.all() + gbT.all(), psb.all())
            P.op("vector", _I("scalar_tensor_tensor", out=tA.t[:], in0=psb.t[:, 0:128], scalar=-1.0, in1=mDA, op0=ALU.mult, op1=ALU.add),
                 psb.all() + masks.all(), tA.all())
            P.op("scalar", _I("activation", out=DA.t[:], in_=tA.t[:], func=AF.Exp, bias=gbt), tA.all() + tok.all(), DA.all())
            P.op("vector", _I("tensor_tensor", out=tB.t[:], in0=psb.t[:, 128:256], in1=mDP, op=ALU.add), psb.all() + masks.all(), tB.all())
            P.op("scalar", _I("activation", out=DP.t[:], in_=tB.t[:], func=AF.Exp, bias=ngam), tB.all() + tok.all(), DP.all())
            P.op("vector", _I("tensor_tensor", out=tC.t[:], in0=psb.t[:, 0:128], in1=mDQ, op=ALU.add), psb.all() + masks.all(), tC.all())
            P.op("scalar", _I("activation", out=DQ.t[:], in_=tC.t[:], func=AF.Exp, bias=ngam), tC.all() + tok.all(), DQ.all())
            P.op("scalar", _I("activation", out=Eb.t[:], in_=psb.t[:, 0:128], func=AF.Exp), psb.all(), Eb.all())
            psb.busy = False
            P.op("gpsimd", _I("tensor_tensor", out=qdA.t[:, 0:64], in0=cq.t[:, t0:t0 + 64], in1=Eb.t[:, 0:64], op=ALU.mult),
                 cq.all() + Eb.all(), qdA.all())
            P.op("gpsimd", _I("tensor_tensor", out=qdB.t[:, 64:128], in0=cq.t[:, t0 + 64:t0 + 128], in1=Eb.t[:, 64:128], op=ALU.mult),
                 cq.all() + Eb.all(), qdB.all())
            if P.stop == 3:
                c.release(m_phase); return
            pst = _ps(P)
            P.tr(pst.t[:, 0:128], ck.t[:, usl], ident.t[:], ck.all() + ident.all(), pst.all())
            P.tr(pst.t[:, 128:256], cv.t[:, usl], ident.t[:], cv.all() + ident.all(), pst.all())
            P.op("vector", _I("tensor_scalar", out=kbg.t[:], in0=pst.t[:, 0:128], scalar1=bg, scalar2=None, op0=ALU.mult),
                 pst.all() + bgtok.all(), kbg.all())
            P.op("scalar", _I("activation", out=kdec.t[:], in_=pst.t[:, 0:128], func=AF.Copy, scale=kd),
                 pst.all() + tok.all(), kdec.all())
            P.op("vector", _I("tensor_scalar", out=vb.t[:], in0=pst.t[:, 128:256], scalar1=beta, scalar2=None, op0=ALU.mult),
                 pst.all() + tok.all(), vb.all())
            pst.busy = False
            psk = _ps(P)
            P.mm(psk.t[:, 0:128], ck.t[:, usl], ck.t[:, usl], True, True, ck.all(), psk.all())
            P.mm(psk.t[:, 128:256], ck.t[:, usl], cq.t[:, usl], True, True, ck.all() + cq.all(), psk.all())
            P.op("vector", _I("tensor_tensor", out=A0.t[:], in0=psk.t[:, 0:128], in1=DA.t[:], op=ALU.mult), psk.all() + DA.all(), A0.all())
            P.op("vector", _I("tensor_tensor", out=P0.t[:], in0=psk.t[:, 0:128], in1=DP.t[:], op=ALU.mult), psk.all() + DP.all(), P0.all())
            P.op("vector", _I("tensor_tensor", out=attnT.t[:], in0=psk.t[:, 128:256], in1=DQ.t[:], op=ALU.mult),
                 psk.all() + DQ.all(), attnT.all())
            psk.busy = False
            P.op("gpsimd", _I("tensor_tensor", out=X0.t[:], in0=ident.t[:], in1=P0.t[:], op=ALU.subtract), ident.all() + P0.all(), X0.all())
            if P.stop == 4:
                c.release(m_phase); return
            Am, Pm, X = A0, P0, X0
            Abuf, Pbuf, Xbuf = [A1, A2], [P1, P2], [X1, X0]
            for n in range(5):
                An, Pn, Xn = Abuf[n % 2], Pbuf[n % 2], Xbuf[n % 2]
                psn = _ps(P)
                P.mm(psn.t[:, 0:128], Pm.t[:], Am.t[:], True, True, Pm.all() + Am.all(), psn.all())
                if P.stop == 44:
                    c.release(m_phase); return
                if n < 4:
                    P.mm(psn.t[:, 128:256], Am.t[:], Pm.t[:], True, True, Pm.all() + Am.all(), psn.all())
                if P.stop == 45:
                    c.release(m_phase); return
                P.copy("scalar", An.t[:], psn.t[:, 0:128], psn.all(), An.all())
                if P.stop == 46:
                    c.release(m_phase); return
                if n < 4:
                    P.copy('vector', Pn.t[:], psn.t[:, 128:256], psn.all(), Pn.all())
                psn.busy = False
                if P.stop == 41:
                    P.dump("A0", A0, A0.t[:], [128, 128]); P.dump("P0", P0, P0.t[:], [128, 128])
                    P.dump("A1", An, An.t[:], [128, 128]); P.dump("P1", Pn, Pn.t[:], [128, 128])
                    P.dump("DA", DA, DA.t[:], [128, 128]); P.dump("DP", DP, DP.t[:], [128, 128])
                    c.release(m_phase); return
                psx = _ps(P)
                P.mm(psx.t[:, 0:128], An.t[:], X.t[:], True, True, An.all() + X.all(), psx.all())
                P.op("vector", _I("tensor_tensor", out=Xn.t[:], in0=psx.t[:, 0:128], in1=X.t[:], op=ALU.add), psx.all() + X.all(), Xn.all())
                psx.busy = False
                if P.stop == 42:
                    c.release(m_phase); return
                if P.stop == 43 and n == 1:
                    c.release(m_phase); return
                Am, Pm, X = An, Pn, Xn
            TTm = X
            psw = _ps(P)
            P.mm(psw.t[:, 0:128], kbg.t[:], TTm.t[:], True, True, kbg.all() + TTm.all(), psw.all())
            P.mm(psw.t[:, 128:256], TTm.t[:], vb.t[:], True, True, vb.all() + TTm.all(), psw.all())
            P.copy("scalar", WT.t[:], psw.t[:, 0:128], psw.all(), WT.all())
            P.copy("vector", U.t[:], psw.t[:, 128:256], psw.all(), U.all())
            psw.busy = False
            if P.stop == 5:
                c.release(m_phase); return
            Sa, Sb, Sn = S[si % 3], S[(si + 1) % 3], S[(si + 2) % 3]
            si += 2
            ps1 = _ps(P)
            P.mm(ps1.t[:, 0:128], WT.t[:], Sa.t[:], True, True, WT.all() + Sa.all(), ps1.all())
            P.op("vector", _I("tensor_tensor", out=vnew.t[0:64, :], in0=U.t[0:64, :], in1=ps1.t[0:64, 0:128], op=ALU.subtract),
                 U.all() + ps1.all(), vnew.all())
            ps1.busy = False
            pso = _ps(P)
            P.mm(pso.t[:, 0:128], qdA.t[:], Sa.t[:], True, False, qdA.all() + Sa.all(), pso.all())
            pss = _ps(P)
            P.mm(pss.t[:, 0:128], kdec.t[0:64, :], vnew.t[0:64, :], True, True, kdec.all() + vnew.all(), pss.all())
            P.op("vector", _I("scalar_tensor_tensor", out=Sb.t[:], in0=Sa.t[:], scalar=eglb.t[:, h, 2 * u:2 * u + 1], in1=pss.t[:, 0:128],
                                                                                      op0=ALU.mult, op1=ALU.add), Sa.all() + pss.all() + eglb.all(), Sb.all())
            pss.busy = False
            ps2 = _ps(P)
            P.mm(ps2.t[:, 0:128], WT.t[:], Sb.t[:], True, True, WT.all() + Sb.all(), ps2.all())
            P.op("vector", _I("tensor_tensor", out=vnew.t[64:128, :], in0=U.t[64:128, :], in1=ps2.t[64:128, 0:128], op=ALU.subtract),
                 U.all() + ps2.all(), vnew.all())
            ps2.busy = False
            P.mm(pso.t[:, 0:128], qdB.t[:], Sb.t[:], False, False, qdB.all() + Sb.all(), pso.all())
            P.mm(pso.t[:, 0:128], attnT.t[:], vnew.t[:], False, True, attnT.all() + vnew.all(), pso.all())
            pss2 = _ps(P)
            P.mm(pss2.t[:, 0:128], kdec.t[64:128, :], vnew.t[64:128, :], True, True, kdec.all() + vnew.all(), pss2.all())
            P.op("vector", _I("scalar_tensor_tensor", out=Sn.t[:], in0=Sb.t[:], scalar=eglb.t[:, h, 2 * u + 1:2 * u + 2], in1=pss2.t[:, 0:128],
                                                                                       op0=ALU.mult, op1=ALU.add), Sb.all() + pss2.all() + eglb.all(), Sn.all())
            pss2.busy = False
            if P.stop == 6:
                c.release(m_phase); return
            P.op("scalar", _I("activation", out=tA.t[:], in_=pso.t[:, 0:128], func=AF.Square, accum_out=ssu.t[:, 0:1]),
                 pso.all(), tA.all() + ssu.all())
            _rstd(P, ssu.t[:, 0:1], ssu.all(), ssu, ssu.t[:, 1:2], 1.0 / 128)
            P.op("vector", _I("tensor_scalar", out=o_n.t[:], in0=pso.t[:, 0:128], scalar1=ssu.t[:, 1:2], scalar2=None, op0=ALU.mult),
                 pso.all() + ssu.all(), o_n.all())
            pso.busy = False
            psy = _ps(P)
            P.tr(psy.t[:, 0:128], o_n.t[:], ident.t[:], o_n.all() + ident.all(), psy.all())
            nrm = k["dnnorm"]
            P.op("vector", _I("scalar_tensor_tensor", out=yT.t[:, usl], in0=psy.t[:, 0:128], scalar=nrm.t[:, i:i + 1], in1=zs.t[:, usl],
                                                                              op0=ALU.mult, op1=ALU.mult), psy.all() + nrm.all() + zs.all(), yT.all())
            psy.busy = False
        P.dma(P.ydn.t[:, h, :], yT.t[:], r=yT.all(), w=P.ydn.all())
    c.release(m_phase)


def _kmajor(w, kc=128):
    K, N = w.shape
    return np.ascontiguousarray(w.reshape(K // kc, kc, N).transpose(1, 0, 2)).reshape(kc, (K // kc) * N)


def _host_weights(inp):
    f = lambda a: np.ascontiguousarray(a, dtype=np.float32)
    o = {}
    o["w_in"] = np.stack([_kmajor(f(inp["w_in"][i])) for i in range(2)])
    o["w_br_dn"] = np.stack([_kmajor(f(inp["w_br_dn"][i])) for i in range(2)])
    o["w_br_ssm"] = np.stack([_kmajor(f(inp["w_br_ssm"][i])) for i in range(2)])
    o["w_br_attn"] = np.stack([_kmajor(f(inp["w_br_attn"][i]), 64) for i in range(2)])
    o["w_out"] = np.stack([_kmajor(f(inp["w_out"][i])) for i in range(2)])
    o["w_ple_gate"] = np.stack([_kmajor(f(inp["w_ple_gate"][i])) for i in range(2)])
    o["w_ple"] = np.stack([_kmajor(f(inp["w_ple"][i])) for i in range(2)])
    o["w_ff_gate"] = _kmajor(f(inp["w_ff_gate"][0]))[None]
    o["w_ff_up"] = _kmajor(f(inp["w_ff_up"][0]))[None]
    o["w_ff_down"] = _kmajor(f(inp["w_ff_down"][0]))[None]
    o["w_moe_gate"] = np.stack([_kmajor(f(inp["w_moe_gate"][0, e])) for e in range(8)])
    o["w_moe_up"] = np.stack([_kmajor(f(inp["w_moe_up"][0, e])) for e in range(8)])
    o["w_moe_down"] = np.stack([_kmajor(f(inp["w_moe_down"][0, e])) for e in range(8)])
    o["w_router"] = _kmajor(f(inp["w_router"][0]))
    pm = lambda v: np.ascontiguousarray(f(v).reshape(-1, 128).T)
    gains = [pm(inp[n][i]) for i in range(2) for n in ("mix_norm", "ffn_norm", "ple_norm")] + [pm(inp["final_norm"])]
    o["gains"] = np.concatenate(gains, axis=1)
    def convtab(w):
        C = w.shape[1]
        return np.ascontiguousarray(f(w).T.reshape(C // 128, 128, 4).transpose(1, 0, 2)).reshape(128, -1)
    o["dnconv"] = np.concatenate([convtab(inp["dn_conv"][i]) for i in range(2)], axis=1)
    o["dnp6"] = np.stack([f(inp["dn_a_log"][0]), f(inp["dn_dt_bias"][0]), f(inp["dn_a_log"][1]), f(inp["dn_dt_bias"][1])], axis=1)
    o["dnnorm"] = np.ascontiguousarray(f(inp["dn_norm"]).T)
    o["ssmconv"] = np.concatenate([convtab(inp["ssm_conv"][i]) for i in range(2)], axis=1)
    o["ssmconvb"] = np.concatenate([pm(inp["ssm_conv_b"][i]) for i in range(2)], axis=1)
    o["ssmp12"] = np.stack([f(inp["ssm_a_log"][0]), f(inp["ssm_dt_bias"][0]), f(inp["ssm_a_log"][1]), f(inp["ssm_dt_bias"][1])], axis=1)
    o["ssmd"] = np.ascontiguousarray(np.broadcast_to(f(inp["ssm_d"]).reshape(1, 24), (128, 24)))
    o["ssmnorm"] = np.concatenate([pm(inp["ssm_norm"][i]) for i in range(2)], axis=1)
    o["ident"] = np.eye(128, dtype=np.float32)
    ii = np.arange(128)[:, None]
    jj = np.arange(128)[None, :]
    same = (ii // 64) == (jj // 64)
    mDA = np.where(same & (ii > jj), 0.0, NEG)
    mDP = np.where(same & (jj > ii), 0.0, NEG)
    mDQ = np.where(same & (jj >= ii), 0.0, NEG)
    o["masks"] = np.concatenate([mDA, mDP, mDQ], axis=1).astype(np.float32)
    def sel(n):
        s = np.zeros((n, n * 128), np.float32)
        for h in range(n):
            s[h, h * 128:(h + 1) * 128] = 1.0
        return s
    o["sel6"], o["sel12"], o["sel8"] = sel(6), sel(12), sel(8)
    slopes = np.exp2(-8.0 * (np.arange(12, dtype=np.float64) + 1.0) / 12.0)
    dil = [1] * 4 + [4] * 4 + [16] * 4
    kk = np.arange(128)[:, None].astype(np.float64)
    qq = np.arange(128)[None, :].astype(np.float64)
    tabs = []
    for h in range(12):
        prev = np.where(kk >= qq, -slopes[h] * dil[h] * (128 + qq - kk), NEG)
        cur = np.where(kk <= qq, -slopes[h] * dil[h] * (qq - kk), NEG)
        tabs.append(np.concatenate([prev, cur], axis=1))
    o["abias"] = np.concatenate(tabs, axis=1).astype(np.float32)
    return o


def _host_acts(inp, b0, ns):
    x = np.asarray(inp["x"][b0:b0 + ns], dtype=np.float32)
    xT = np.ascontiguousarray(x.reshape(ns, T, 8, 128).transpose(0, 3, 2, 1))
    p = np.asarray(inp["p"][:, b0:b0 + ns], dtype=np.float32)
    pT = np.ascontiguousarray(p.reshape(2, ns, T, 2, 128).transpose(0, 1, 4, 3, 2))
    return {"xT": xT, "pT": pT}


def _dump_dram(P, name, tt, shape, dtype):
    if name not in P.dbg:
        return
    o = P.nc.dram_tensor("dbg_" + name, list(shape), dtype, kind="ExternalOutput").ap()
    P.dbg_out[name] = o
    P.c.emit("sync", _I("dma_start", out=o, in_=tt.t), reads=tt.all(), dma=True)


def _conv_silu(P, raw, dst, cw, cb, bias_ap=None, bias_bufs=()):
    P.op("vector", _I("tensor_scalar", out=dst.t[:], in0=raw.t[:], scalar1=cw.t[:, cb + 3:cb + 4], scalar2=None, op0=ALU.mult),
         raw.all() + cw.all(), dst.all())
    for sft in (1, 2, 3):
        P.op("vector", _I("scalar_tensor_tensor",
            out=dst.t[:, sft:], in0=raw.t[:, :T - sft], scalar=cw.t[:, cb + 3 - sft:cb + 4 - sft], in1=dst.t[:, sft:], op0=ALU.mult, op1=ALU.add),
            raw.all() + cw.all() + dst.all(), dst.all())
    if bias_ap is None:
        P.op("scalar", _I("activation", out=dst.t[:], in_=dst.t[:], func=AF.Silu), dst.all(), dst.all())
    else:
        P.op("scalar", _I("activation", out=dst.t[:], in_=dst.t[:], func=AF.Silu, bias=bias_ap), dst.all() + list(bias_bufs), dst.all())


def _conv_silu2(P, raw, cac, dst, cw, cb, bias_ap, bias_bufs):
    P.op("vector", _I("tensor_scalar", out=cac.t[:], in0=raw.t[:], scalar1=cw.t[:, cb + 3:cb + 4], scalar2=None, op0=ALU.mult),
         raw.all() + cw.all(), cac.all())
    for sft in (1, 2, 3):
        P.op("vector", _I("scalar_tensor_tensor", out=cac.t[:, sft:], in0=raw.t[:, :T - sft], scalar=cw.t[:, cb + 3 - sft:cb + 4 - sft], in1=cac.t[:, sft:],
                          op0=ALU.mult, op1=ALU.add), raw.all() + cw.all() + cac.all(), cac.all())
    P.op("scalar", _I("activation", out=dst.t[:], in_=cac.t[:], func=AF.Silu, bias=bias_ap), cac.all() + list(bias_bufs), dst.all())


def _ssd(P, s, i, hT):
    c, k = P.c, P.k
    ident, masks, sel12 = k["ident"], k["masks"], k["sel12"]
    mDQ = masks.t[:, 256:384]
    p12 = k["ssmp12"]
    m_phase = c.mark()
    acsT = c.sb("ss_acsT", [12, T], F32)
    tok2 = c.sb("ss_tok2", [128, 16, 128], F32)
    cdb = c.sb("ss_cdb", [128, 12, 32], F32)
    negA = c.sb("ss_negA", [12, 1], F32)
    acshl = c.sb("ss_acshl", [44, T], BF16)
    sel44 = c.sb("ss_sel44", [44, 1536], BF16)
    identb = c.sb("ss_identb", [128, 128], BF16)
    P.copy("vector", identb.t[:], ident.t[:], ident.all(), identb.all())
    m1 = c.mark()
    wdt = c.sb("ss_wdt", [128, 8, 12], BF16)
    _load_wcols(P, wdt, i, C_SDT, 12)
    t0_ = c.sb("ss_t0", [12, T], F32)
    t1_ = c.sb("ss_t1", [12, T], F32)
    stk = c.sb("ss_stk", [128, T], F32)
    P.op("gpsimd", _I("memset", stk.t[:], 0.0), w=stk.all())
    P.op("scalar", _I("activation", out=negA.t[:], in_=p12.t[:, 2 * i:2 * i + 1], func=AF.Exp), p12.all(), negA.all())
    P.op("vector", _I("tensor_scalar", out=negA.t[:], in0=negA.t[:], scalar1=-1.0, scalar2=None, op0=ALU.mult), negA.all(), negA.all())

    def ev_dt(nt, ps):
        sl = slice(nt * 512, (nt + 1) * 512)
        P.op("scalar", _I("activation", out=stk.t[0:12, sl], in_=ps.t[0:12, :], func=AF.Exp, bias=p12.t[:, 2 * i + 1:2 * i + 2]),
             ps.all() + p12.all(), stk.all())
    _proj_fm(P, hT, wdt, 0, 12, ev_dt)
    P.op("scalar", _I("activation", out=stk.t[0:12, :], in_=stk.t[0:12, :], func=AF.Ln, bias=1.0), stk.all(), stk.all())
    P.op("vector", _I("tensor_scalar", out=t0_.t[:], in0=stk.t[0:12, :], scalar1=negA.t[:, 0:1], scalar2=None, op0=ALU.mult),
         stk.all() + negA.all(), t0_.all())
    ares = _cumsum64(P, t0_, t1_, 12)
    P.copy("vector", acsT.t[:], ares.t[:], ares.all(), acsT.all())
    a3 = acsT.t[:].rearrange("p (c t) -> p c t", t=64)
    P.op("vector", _I("tensor_tensor", out=t0_.t[:].rearrange("p (c t) -> p c t", t=64), in0=a3[:, :, 63:64].to_broadcast([12, 32, 64]),
                                             in1=a3, op=ALU.subtract), acsT.all(), t0_.all())
    P.op("scalar", _I("activation", out=t0_.t[:], in_=t0_.t[:], func=AF.Exp), t0_.all(), t0_.all())
    P.op("vector", _I("tensor_tensor", out=stk.t[32:44, :], in0=t0_.t[:], in1=stk.t[0:12, :], op=ALU.mult), t0_.all() + stk.all(), stk.all())
    P.op("scalar", _I("activation", out=stk.t[64:76, :], in_=acsT.t[:], func=AF.Exp), acsT.all(), stk.all())
    P.op("vector", _I("tensor_scalar", out=stk.t[96:108, :], in0=acsT.t[:], scalar1=-1.0, scalar2=None, op0=ALU.mult), acsT.all(), stk.all())
    for u4 in range(4):
        ps = _ps(P)
        for j in range(4):
            u = u4 * 4 + j
            P.tr(ps.t[:, j * 128:(j + 1) * 128], stk.t[:, u * 128:(u + 1) * 128], ident.t[:], stk.all() + ident.all(), ps.all())
        P.copy("vector", tok2.t[:, u4 * 4:(u4 + 1) * 4, :].rearrange("p a b -> p (a b)"), ps.t[:], ps.all(), tok2.all())
        ps.busy = False
    P.op("scalar", _I("activation", out=t1_.t[:, 0:32], in_=a3[:, :, 63], func=AF.Exp), acsT.all(), t1_.all())
    ps = _ps(P)
    for h in range(12):
        P.mm(ps.t[:, h * 32:(h + 1) * 32], sel12.t[:, h * 128:(h + 1) * 128], t1_.t[:, 0:32], True, True, sel12.all() + t1_.all(), ps.all())
    P.copy("vector", cdb.t[:].rearrange("p a b -> p (a b)"), ps.t[:, 0:384], ps.all(), cdb.all())
    ps.busy = False
    P.op("gpsimd", _I("memset", acshl.t[:], 0.0), w=acshl.all())
    P.copy("vector", acshl.t[0:12, :], acsT.t[:], acsT.all(), acshl.all())
    P.op("vector", _I("tensor_tensor", out=t0_.t[:], in0=acsT.t[:], in1=acshl.t[0:12, :], op=ALU.subtract), acsT.all() + acshl.all(), t0_.all())
    P.copy("vector", acshl.t[32:44, :], t0_.t[:], t0_.all(), acshl.all())
    P.op("gpsimd", _I("memset", sel44.t[:], 0.0), w=sel44.all())
    P.copy("vector", sel44.t[0:12, :], sel12.t[:], sel12.all(), sel44.all())
    P.copy("vector", sel44.t[32:44, :], sel12.t[:], sel12.all(), sel44.all())
    c.release(m1)
    if P.stop == 101:
        c.release(m_phase); return
    wx = [c.sb(f"ss_wx{j}", [128, 8, 128], BF16) for j in range(5)]
    wz = c.sb("ss_wz", [128, 8, 384], BF16)
    raw = c.sb("ss_raw", [128, T], F32)
    xs = [c.sb(f"ss_xs{j}", [128, T], BF16) for j in range(3)]
    Bm = c.sb("ss_Bm", [128, T], BF16)
    Cm = c.sb("ss_Cm", [128, T], BF16)
    cac = c.sb("ss_cac", [128, T], F32)
    ySs = c.sb("ss_y", [128, 3, T], BF16)
    prev = [c.sb(f"ss_prev{j}", [128, 6, 64], F32) for j in range(3)]
    prev16 = [c.sb(f"ss_prevb{j}", [128, 6, 64], BF16) for j in range(3)]
    Btok = c.sb("ss_Btok", [128, 128], BF16)
    xdt = c.sb("ss_xdt", [128, 6, 64], BF16)
    xdtd = c.sb("ss_xdtd", [128, 6, 64], BF16)
    xsk = c.sb("ss_xsk", [128, 6, 64], F32)
    tmpL = c.sb("ss_tmpL", [128, 6, 128], F32)
    LT = c.sb("ss_LT", [128, 6, 128], F32)
    Mh = c.sb("ss_Mh", [128, 6, 128], BF16)
    CmA = c.sb("ss_CmA", [128, 128], BF16)
    CmB = c.sb("ss_CmB", [128, 128], BF16)
    tpr = c.sb("ss_tpr", [128, 6, 64], F32)
    yt = c.sb("ss_yt", [128, 384], F32)
    sz = c.sb("ss_sz", [128, 384], F32)
    junk = c.sb("ss_junk", [128, 384], F32)
    ssq = c.sb("ss_ssq", [128, 2], F32)
    P.op("gpsimd", _I("memset", CmA.t[:], 0.0), w=CmA.all())
    P.op("gpsimd", _I("memset", CmB.t[:], 0.0), w=CmB.all())
    cw, cbias, dsk, nrm = k["ssmconv"], k["ssmconvb"], k["ssmd"], k["ssmnorm"]
    for g in range(2):
        chunks = [3 * g, 3 * g + 1, 3 * g + 2, 6 + g, 8 + g]
        cols = [C_SX + g * 384, C_SX + g * 384 + 128, C_SX + g * 384 + 256, C_SB + g * 128, C_SC + g * 128]
        for j in range(5):
            _load_wcols(P, wx[j], i, cols[j], 128)
        _load_wcols(P, wz, i, C_SZ + g * 384, 384)
        for j, dst in enumerate(xs + [Bm, Cm]):
            def ev_raw(nt, ps):
                sl = slice(nt * 512, (nt + 1) * 512)
                P.copy(P.ev_eng(), raw.t[:, sl], ps.t[:], ps.all(), raw.all())
            _proj_fm(P, hT, wx[j], 0, 128, ev_raw)
            ch = chunks[j]
            _conv_silu2(P, raw, cac, dst, cw, (i * 10 + ch) * 4, cbias.t[:, i * 10 + ch:i * 10 + ch + 1], cbias.all())
        if P.stop == 102:
            c.release(m_phase); return
        pa = prev[0]
        P.op("gpsimd", _I("memset", pa.t[:], 0.0), w=pa.all())
        P.op("gpsimd", _I("memset", prev16[0].t[:], 0.0), w=prev16[0].all())
        pi = 0
        for u in range(16):
            usl = slice(u * 128, (u + 1) * 128)
            hs = slice(6 * g, 6 * g + 6)
            pst = _ps(P)
            pstb = pst.t[:].bitcast(BF16)
            for j in range(3):
                P.tr(pstb[:, j * 128:(j + 1) * 128], xs[j].t[:, usl], identb.t[:], xs[j].all() + identb.all(), pst.all())
            P.tr(pstb[:, 384:512], Bm.t[:, usl], identb.t[:], Bm.all() + identb.all(), pst.all())
            px3 = pstb[:, 0:384].rearrange("p (h d) -> p h d", d=64)
            P.copy("scalar", Btok.t[:], pstb[:, 384:512], pst.all(), Btok.all())
            P.op("vector", _I("tensor_tensor", out=xdt.t[:], in0=px3, in1=tok2.t[:, u, 6 * g:6 * g + 6].unsqueeze(2).to_broadcast([128, 6, 64]), op=ALU.mult),
                 pst.all() + tok2.all(), xdt.all())
            P.op("vector", _I("tensor_tensor", out=xdtd.t[:], in0=px3, in1=tok2.t[:, u, 32 + 6 * g:38 + 6 * g].unsqueeze(2).to_broadcast([128, 6, 64]), op=ALU.mult),
                 pst.all() + tok2.all(), xdtd.all())
            P.op("vector", _I("tensor_tensor", out=xsk.t[:], in0=px3, in1=dsk.t[:, i * 12 + 6 * g:i * 12 + 6 * g + 6].unsqueeze(2).to_broadcast([128, 6, 64]), op=ALU.mult),
                 pst.all() + dsk.all(), xsk.all())
            pst.busy = False
            if P.stop == 103:
                c.release(m_phase); return
            P.copy("gpsimd", CmA.t[:, 0:64], Cm.t[:, u * 128:u * 128 + 64], Cm.all(), CmA.all())
            P.copy("gpsimd", CmB.t[:, 64:128], Cm.t[:, u * 128 + 64:u * 128 + 128], Cm.all(), CmB.all())
            pcb = _ps(P)
            P.mm(pcb.t[:, 0:128], Bm.t[:, usl], Cm.t[:, usl], True, True, Bm.all() + Cm.all(), pcb.all())
            pa0 = _ps(P)
            pa1 = _ps(P)
            for h in range(6):
                hh = 6 * g + h
                pp = pa0 if h < 4 else pa1
                col = (h % 4) * 128
                P.mm(pp.t[:, col:col + 128], sel44.t[:, hh * 128:(hh + 1) * 128], acshl.t[:, usl], True, True, sel44.all() + acshl.all(), pp.all())
            P.op("vector", _I("tensor_tensor", out=tmpL.t[:, 0:4, :], in0=pa0.t[:].rearrange("p (h d) -> p h d", d=128),
                                                     in1=mDQ.unsqueeze(1).to_broadcast([128, 4, 128]), op=ALU.add), pa0.all() + masks.all(), tmpL.all())
            P.op("vector", _I("tensor_tensor", out=tmpL.t[:, 4:6, :], in0=pa1.t[:, 0:256].rearrange("p (h d) -> p h d", d=128),
                                                     in1=mDQ.unsqueeze(1).to_broadcast([128, 2, 128]), op=ALU.add), pa1.all() + masks.all(), tmpL.all())
            pa0.busy = False
            pa1.busy = False
            for h in range(6):
                hh = 6 * g + h
                P.op("scalar", _I("activation", out=LT.t[:, h, :], in_=tmpL.t[:, h, :], func=AF.Exp, bias=tok2.t[:, u, 96 + hh:97 + hh]),
                     tmpL.all() + tok2.all(), LT.all())
            P.op("vector", _I("tensor_tensor", out=Mh.t[:], in0=LT.t[:], in1=pcb.t[:, 0:128].unsqueeze(1).to_broadcast([128, 6, 128]), op=ALU.mult),
                 LT.all() + pcb.all(), Mh.all())
            pcb.busy = False
            if P.stop == 104:
                c.release(m_phase); return
            py = _ps(P)
            for h in range(6):
                P.mm(py.t[:, h * 64:(h + 1) * 64], Mh.t[:, h, :], xdt.t[:, h, :], True, True, Mh.all() + xdt.all(), py.all())
            pa_, pb_, pn_ = prev[pi % 3], prev[(pi + 1) % 3], prev[(pi + 2) % 3]
            pa6, pb6, pn6 = prev16[pi % 3], prev16[(pi + 1) % 3], prev16[(pi + 2) % 3]
            pi += 2
            pss = _ps(P)
            P.mm(pss.t[:, 0:384], Btok.t[0:64, :], xdtd.t[0:64].rearrange("p h d -> p (h d)"), True, True, Btok.all() + xdtd.all(), pss.all())
            P.op("gpsimd", _I("tensor_tensor", out=tpr.t[:], in0=pa_.t[:], in1=cdb.t[:, hs, 2 * u:2 * u + 1].to_broadcast([128, 6, 64]), op=ALU.mult),
                 pa_.all() + cdb.all(), tpr.all())
            P.op("vector", _I("tensor_tensor", out=pb_.t[:].rearrange("p h d -> p (h d)"), in0=pss.t[:, 0:384], in1=tpr.t[:].rearrange("p h d -> p (h d)"), op=ALU.add),
                 pss.all() + tpr.all(), pb_.all())
            pss.busy = False
            P.copy("scalar", pb6.t[:], pb_.t[:], pb_.all(), pb6.all())
            pss2 = _ps(P)
            P.mm(pss2.t[:, 0:384], Btok.t[64:128, :], xdtd.t[64:128].rearrange("p h d -> p (h d)"), True, True, Btok.all() + xdtd.all(), pss2.all())
            P.op("gpsimd", _I("tensor_tensor", out=tpr.t[:], in0=pb_.t[:], in1=cdb.t[:, hs, 2 * u + 1:2 * u + 2].to_broadcast([128, 6, 64]), op=ALU.mult),
                 pb_.all() + cdb.all(), tpr.all())
            P.op("vector", _I("tensor_tensor", out=pn_.t[:].rearrange("p h d -> p (h d)"), in0=pss2.t[:, 0:384], in1=tpr.t[:].rearrange("p h d -> p (h d)"), op=ALU.add),
                 pss2.all() + tpr.all(), pn_.all())
            pss2.busy = False
            P.copy("scalar", pn6.t[:], pn_.t[:], pn_.all(), pn6.all())
            if P.stop == 105:
                c.release(m_phase); return
            po = _ps(P)
            P.mm(po.t[:, 0:384], CmA.t[:], pa6.t[:].rearrange("p h d -> p (h d)"), True, False, CmA.all() + pa6.all(), po.all())
            P.mm(po.t[:, 0:384], CmB.t[:], pb6.t[:].rearrange("p h d -> p (h d)"), False, True, CmB.all() + pb6.all(), po.all())
            P.op("vector", _I("tensor_tensor", out=yt.t[:].rearrange("p (h d) -> p h d", d=64), in0=po.t[:, 0:384].rearrange("p (h d) -> p h d", d=64),
                                                                 in1=tok2.t[:, u, 64 + 6 * g:70 + 6 * g].unsqueeze(2).to_broadcast([128, 6, 64]), op=ALU.mult),
                 po.all() + tok2.all(), yt.all())
            po.busy = False
            P.op("gpsimd", _I("tensor_tensor", out=yt.t[:], in0=yt.t[:], in1=xsk.t[:].rearrange("p h d -> p (h d)"), op=ALU.add), yt.all() + xsk.all(), yt.all())
            P.op("vector", _I("tensor_tensor", out=yt.t[:], in0=py.t[:, 0:384], in1=yt.t[:], op=ALU.add), py.all() + yt.all(), yt.all())
            py.busy = False
            if P.stop == 106:
                c.release(m_phase); return
            pz = _ps(P)
            for kk in range(8):
                P.mm(pz.t[:, 0:384], hT.t[:, kk, usl], wz.t[:, kk, :], kk == 0, kk == 7, [hT.b(u // 4)] + wz.all(), pz.all())
            P.op("scalar", _I("activation", out=sz.t[:], in_=pz.t[:, 0:384], func=AF.Silu), pz.all(), sz.all())
            pz.busy = False
            P.op("gpsimd", _I("tensor_tensor", out=yt.t[:], in0=yt.t[:], in1=sz.t[:], op=ALU.mult), yt.all() + sz.all(), yt.all())
            P.op("scalar", _I("activation", out=junk.t[:], in_=yt.t[:], func=AF.Square, accum_out=ssq.t[:, 0:1]), yt.all(), junk.all() + ssq.all())
            _rstd(P, ssq.t[:, 0:1], ssq.all(), ssq, ssq.t[:, 1:2], 1.0 / 384)
            P.op("vector", _I("tensor_scalar", out=yt.t[:], in0=yt.t[:], scalar1=ssq.t[:, 1:2], scalar2=None, op0=ALU.mult), yt.all() + ssq.all(), yt.all())
            if P.stop == 107:
                c.release(m_phase); return
            pT_ = _ps(P)
            for j in range(3):
                P.tr(pT_.t[:, j * 128:(j + 1) * 128], yt.t[:, j * 128:(j + 1) * 128], ident.t[:], yt.all() + ident.all(), pT_.all())
            for j in range(3):
                nc_ = i * 6 + 3 * g + j
                P.op("vector" if j != 1 else "scalar",
                     (_I("tensor_scalar", out=ySs.t[:, j, usl], in0=pT_.t[:, j * 128:(j + 1) * 128], scalar1=nrm.t[:, nc_:nc_ + 1], scalar2=None, op0=ALU.mult))
                     if j != 1 else
                     (_I("activation", out=ySs.t[:, j, usl], in_=pT_.t[:, j * 128:(j + 1) * 128], func=AF.Copy, scale=nrm.t[:, nc_:nc_ + 1])),
                     pT_.all() + nrm.all(), ySs.all())
            pT_.busy = False
            if P.stop == 108 or (P.stop == 110 and u == 1):
                c.release(m_phase); return
        if P.stop == 109:
            c.release(m_phase); return
        P.dma(P.yssm.t[:, 3 * g:3 * g + 3, :], ySs.t[:], r=ySs.all(), w=P.yssm.all())
    c.release(m_phase)


def _attn(P, s, i, hT):
    c, k = P.c, P.k
    m_phase = c.mark()
    abias = c.sb("at_abias", [128, 12 * 256], F32)
    P.dma(abias.t[:], P.abias_d, w=abias.all())
    acc = c.sb("at_acc", [128, 4, T], F32)
    qT = [c.sb(f"at_qT{j}", [128, T], BF16) for j in range(2)]
    kT = [c.sb(f"at_kT{j}", [128, T], BF16) for j in range(2)]
    wqk = [c.sb(f"at_wqk{j}", [128, 8, 128], BF16) for j in range(4)]
    wv = c.sb("at_wv", [128, 8, 256], BF16)
    Vext = c.sb("at_Vext", [128, 16, 4, 128], BF16)
    tmp = [c.sb(f"at_tmp{j}", [128, 256], F32) for j in range(4)]
    PT = [c.sb(f"at_PT{j}", [128, 256], BF16) for j in range(4)]
    P.op("gpsimd", _I("memset", Vext.t[:], 1.0), w=Vext.all())
    bi = 0
    for g in range(3):
        dil = (1, 4, 16)[g]
        tpp = (T // dil) // 128
        for cc in range(2):
            _load_wcols(P, wqk[cc], i, C_AQ + (4 * g + 2 * cc) * 64, 128)
            _load_wcols(P, wqk[2 + cc], i, C_AK + (4 * g + 2 * cc) * 64, 128)
        _load_wcols(P, wv, i, C_AV + 4 * g * 64, 256)
        for j, dst in enumerate(qT + kT):
            def ev_qk(nt, ps, dst=dst):
                sl = slice(nt * 512, (nt + 1) * 512)
                P.copy(P.ev_eng(), dst.t[:, sl], ps.t[:], ps.all(), dst.all())
            _proj_fm(P, hT, wqk[j], 0, 128, ev_qk)

        def tsl(ti):
            r, n = ti // tpp, ti % tpp
            st = r + dil * n * 128
            return slice(st, st + 127 * dil + 1, dil) if dil > 1 else slice(st, st + 128)
        for ti in range(16):
            ps = _ps(P)
            for kk in range(8):
                P.mm(ps.t[:, 0:256], hT.t[:, kk, tsl(ti)], wv.t[:, kk, :], kk == 0, kk == 7, hT.all() + wv.all(), ps.all())
            P.copy(P.ev_eng(), Vext.t[:, ti, :, 0:64], ps.t[:, 0:256].rearrange("p (h d) -> p h d", d=64), ps.all(), Vext.all())
            ps.busy = False
        for ti in range(16):
            for hg in range(4):
                h = 4 * g + hg
                cc, pb = hg // 2, 64 * (hg % 2)
                kh, qh = kT[cc], qT[cc]
                has_prev = (ti % tpp) >= 1
                cur = tsl(ti)
                tm, pt = tmp[bi % 4], PT[bi % 4]
                bi += 1
                pss = _ps(P)
                if has_prev:
                    P.mm(pss.t[:, 0:128], kh.t[pb:pb + 64, tsl(ti - 1)], qh.t[pb:pb + 64, cur], True, True, kh.all() + qh.all(), pss.all())
                P.mm(pss.t[:, 128:256], kh.t[pb:pb + 64, cur], qh.t[pb:pb + 64, cur], True, True, kh.all() + qh.all(), pss.all())
                lo = 0 if has_prev else 128
                P.op("vector", _I("scalar_tensor_tensor", out=tm.t[:, lo:256], in0=pss.t[:, lo:256], scalar=0.125, in1=abias.t[:, h * 256 + lo:(h + 1) * 256],
                                  op0=ALU.mult, op1=ALU.add), pss.all() + abias.all(), tm.all())
                pss.busy = False
                P.op("scalar", _I("activation", out=pt.t[:, lo:256], in_=tm.t[:, lo:256], func=AF.Exp), tm.all(), pt.all())
                pso = _ps(P)
                if has_prev:
                    P.mm(pso.t[:, 0:128], Vext.t[:, ti - 1, hg, :], pt.t[:, 0:128], True, False, Vext.all() + pt.all(), pso.all())
                P.mm(pso.t[:, 0:128], Vext.t[:, ti, hg, :], pt.t[:, 128:256], not has_prev, True, Vext.all() + pt.all(), pso.all())
                if dil == 1:
                    ab = [acc.b((hg, ti // 4))]
                elif dil == 4:
                    ab = [acc.b((hg, ti % tpp))]
                else:
                    ab = [acc.b((hg, q_)) for q_ in range(4)]
                if g == 0:
                    P.copy("vector", acc.t[:, hg, cur], pso.t[:, 0:128], pso.all(), ab)
                else:
                    P.op("vector", _I("tensor_tensor", out=acc.t[:, hg, cur], in0=acc.t[:, hg, cur], in1=pso.t[:, 0:128], op=ALU.add), pso.all() + ab, ab)
                pso.busy = False
    rd = c.sb("at_rd", [64, T], F32)
    yb = c.sb("at_yb", [64, 4, T], BF16)
    for hg in range(4):
        P.op("vector", _I("reciprocal", out=acc.t[64:128, hg, :], in_=acc.t[64:128, hg, :]), acc.all(), acc.all())
        P.copy("vector", rd.t[:], acc.t[64:128, hg, :], acc.all(), rd.all())
        P.op("gpsimd", _I("tensor_tensor", out=yb.t[:, hg, :], in0=acc.t[0:64, hg, :], in1=rd.t[:], op=ALU.mult), acc.all() + rd.all(), yb.all())
    P.dma(P.yattn.t[:], yb.t[:], r=yb.all(), w=P.yattn.all())
    c.release(m_phase)


def _load_w(P, dst, name, j, K, c0, ncols, kp=128):
    src = P.wb[name].t[j].rearrange("p (k n) -> p k n", k=K)[:, :, c0:c0 + ncols]
    P.dma(dst.t[0:kp, 0:K, 0:ncols], src, r=_wbufs(P, name, j), w=dst.all())


def _merge(P, s, i, hT, x_sb, src_ap, src_bufs):
    c = P.c
    m = c.mark()
    yd = [c.sb(f"mg_yd{j}", [128, 6, 512], BF16) for j in range(1)]
    ys = [c.sb(f"mg_ys{j}", [128, 6, 512], BF16) for j in range(1)]
    ya = [c.sb(f"mg_ya{j}", [64, 4, 512], BF16) for j in range(1)]
    mg = [c.sb(f"mg_m{j}", [128, 8, 512], BF16) for j in range(1)]
    wbd = [c.sb(f"mg_wbd{j}", [128, 6, 128], BF16) for j in range(2)]
    wbs = [c.sb(f"mg_wbs{j}", [128, 6, 128], BF16) for j in range(2)]
    wba = [c.sb(f"mg_wba{j}", [64, 4, 128], BF16) for j in range(2)]
    wg = [c.sb(f"mg_wg{j}", [128, 8, 384], BF16) for j in range(2)]
    wo = [c.sb(f"mg_wo{j}", [128, 8, 128], BF16) for j in range(2)]
    sg = [c.sb(f"mg_sg{j}", [128, 512], F32) for j in range(3)]
    t1 = c.sb("mg_t1", [128, 512], F32)
    t2 = c.sb("mg_t2", [128, 512], F32)
    wi = 0
    for nt in range(4):
        sl = slice(nt * 512, (nt + 1) * 512)
        a, b_, cc, mm_ = yd[0], ys[0], ya[0], mg[0]
        P.dma(x_sb.t[:, :, sl], src_ap[:, :, sl], r=src_bufs, w=[x_sb.b(nt)])
        P.dma(a.t[:], P.ydn.t[:, :, sl], r=P.ydn.all(), w=a.all())
        P.dma(b_.t[:], P.yssm.t[:, :, sl], r=P.yssm.all(), w=b_.all())
        P.dma(cc.t[:], P.yattn.t[:, :, sl], r=P.yattn.all(), w=cc.all())
        for oc in range(8):
            w1, w2, w3, w4 = wbd[wi % 2], wbs[wi % 2], wba[wi % 2], wg[wi % 2]
            wi += 1
            _load_w(P, w1, "w_br_dn", i, 6, oc * 128, 128)
            _load_w(P, w2, "w_br_ssm", i, 6, oc * 128, 128)
            _load_w(P, w3, "w_br_attn", i, 4, oc * 128, 128, kp=64)
            for b in range(3):
                src = P.wb["w_in"].t[i].rearrange("p (k n) -> p k n", k=8)[:, :, C_GATE + b * 1024 + oc * 128:C_GATE + b * 1024 + (oc + 1) * 128]
                P.dma(w4.t[:, :, b * 128:(b + 1) * 128], src, r=_wbufs(P, "w_in", i), w=w4.all())
            for b in range(3):
                ps = _ps(P)
                for kk in range(8):
                    P.mm(ps.t[:], w4.t[:, kk, b * 128:(b + 1) * 128], hT.t[:, kk, sl], kk == 0, kk == 7, w4.all() + [hT.b(nt)], ps.all())
                P.op("scalar", _I("activation", out=sg[b].t[:], in_=ps.t[:], func=AF.Sigmoid), ps.all(), sg[b].all())
                ps.busy = False
            psd = _ps(P)
            for kk in range(6):
                P.mm(psd.t[:], w1.t[:, kk, :], a.t[:, kk, :], kk == 0, kk == 5, w1.all() + a.all(), psd.all())
            P.op("vector", _I("tensor_tensor", out=t1.t[:], in0=psd.t[:], in1=sg[0].t[:], op=ALU.mult), psd.all() + sg[0].all(), t1.all())
            psd.busy = False
            pss = _ps(P)
            for kk in range(6):
                P.mm(pss.t[:], w2.t[:, kk, :], b_.t[:, kk, :], kk == 0, kk == 5, w2.all() + b_.all(), pss.all())
            P.op("vector", _I("tensor_tensor", out=t2.t[:], in0=pss.t[:], in1=sg[1].t[:], op=ALU.mult), pss.all() + sg[1].all(), t2.all())
            pss.busy = False
            P.op("gpsimd", _I("tensor_tensor", out=t1.t[:], in0=t1.t[:], in1=t2.t[:], op=ALU.add), t1.all() + t2.all(), t1.all())
            psa = _ps(P)
            for kk in range(4):
                P.mm(psa.t[:], w3.t[0:64, kk, :], cc.t[0:64, kk, :], kk == 0, kk == 3, w3.all() + cc.all(), psa.all())
            P.op("vector", _I("tensor_tensor", out=t2.t[:], in0=psa.t[:], in1=sg[2].t[:], op=ALU.mult), psa.all() + sg[2].all(), t2.all())
            psa.busy = False
            P.op("gpsimd", _I("tensor_tensor", out=mm_.t[:, oc, :], in0=t1.t[:], in1=t2.t[:], op=ALU.add), t1.all() + t2.all(), mm_.all())
        for oc in range(8):
            w5 = wo[oc % 2]
            _load_w(P, w5, "w_out", i, 8, oc * 128, 128)
            ps = _ps(P)
            for kk in range(8):
                P.mm(ps.t[:], w5.t[:, kk, :], mm_.t[:, kk, :], kk == 0, kk == 7, w5.all() + mm_.all(), ps.all())
            P.op("vector", _I("tensor_tensor", out=x_sb.t[:, oc, sl], in0=x_sb.t[:, oc, sl], in1=ps.t[:], op=ALU.add), ps.all() + [x_sb.b(nt)], [x_sb.b(nt)])
            ps.busy = False
    c.release(m)


def _ffn_core(P, hT, x_sb, nt, gname, uname, dname, j, FC, act, wgu, wd, sgt, gbc=None, tg=None):
    sl = slice(nt * 512, (nt + 1) * 512)
    F = FC * 128
    for fc in range(FC):
        w = wgu[fc % 2]
        _load_w(P, w[0], gname, j, 8, fc * 128, 128)
        _load_w(P, w[1], uname, j, 8, fc * 128, 128)
        psg = _ps(P)
        for kk in range(8):
            P.mm(psg.t[:], w[0].t[:, kk, :], hT.t[:, kk, sl], kk == 0, kk == 7, w[0].all() + [hT.b(nt)], psg.all())
        psu = _ps(P)
        for kk in range(8):
            P.mm(psu.t[:], w[1].t[:, kk, :], hT.t[:, kk, sl], kk == 0, kk == 7, w[1].all() + [hT.b(nt)], psu.all())
        st = sgt[fc % 2]
        P.op("scalar", _I("activation", out=st.t[:], in_=psg.t[:], func=AF.Silu), psg.all(), st.all())
        psg.busy = False
        P.op("vector", _I("tensor_tensor", out=act.t[:, fc, :], in0=psu.t[:], in1=st.t[:], op=ALU.mult), psu.all() + st.all(), [act.b(fc)])
        psu.busy = False
    for oc in range(8):
        w = wd[oc % 2]
        _load_w(P, w, dname, j, FC, oc * 128, 128)
        ps = _ps(P)
        for fc in range(FC):
            P.mm(ps.t[:], w.t[:, fc, :], act.t[:, fc, :], fc == 0, fc == FC - 1, w.all() + [act.b(fc)], ps.all())
        if gbc is None:
            P.op("vector", _I("tensor_tensor", out=x_sb.t[:, oc, sl], in0=x_sb.t[:, oc, sl], in1=ps.t[:], op=ALU.add), ps.all() + [x_sb.b(nt)], [x_sb.b(nt)])
        else:
            t = tg[oc % 2]
            P.op("vector", _I("tensor_tensor", out=t.t[:], in0=ps.t[:], in1=gbc.t[:], op=ALU.mult), ps.all() + gbc.all(), t.all())
            P.op("gpsimd", _I("tensor_tensor", out=x_sb.t[:, oc, sl], in0=x_sb.t[:, oc, sl], in1=t.t[:], op=ALU.add), t.all() + [x_sb.b(nt)], [x_sb.b(nt)])
        ps.busy = False


def _ffn_phase(P, s, i, hT, x_sb):
    c, k = P.c, P.k
    _conv_some(P, 10 ** 6)
    m = c.mark()
    moe = (i % 2 == 1)
    FC = 28 if moe else 22
    sq = c.sb("ff_sq", [128, 8, 512], BF16)
    rs = c.sb("ff_rs", [128, 512], F32)
    act = c.sb("ff_act", [128, FC, 512], BF16)
    wgu = [(c.sb(f"ff_wg{j}", [128, 8, 128], BF16), c.sb(f"ff_wu{j}", [128, 8, 128], BF16)) for j in range(2)]
    wd = [c.sb(f"ff_wd{j}", [128, FC, 128], BF16) for j in range(2)]
    sgt = [c.sb(f"ff_sg{j}", [128, 512], F32) for j in range(2)]
    gcol = i * 24 + 8
    if moe:
        h32 = c.sb("ff_h32", [128, 8, 128], F32)
        lg = c.sb("ff_lg", [128, 4, 8], F32)
        m8 = c.sb("ff_m8", [128, 4, 8], F32)
        nm1 = c.sb("ff_nm1", [128, 4], F32)
        ex = c.sb("ff_ex", [128, 4, 8], F32)
        msk = c.sb("ff_msk", [128, 4, 8], F32)
        den = c.sb("ff_den", [128, 4], F32)
        gT = c.sb("ff_gT", [8, 512], F32)
        gbc = c.sb("ff_gbc", [128, 512], F32)
        tg = [c.sb(f"ff_tg{j}", [128, 512], F32) for j in range(2)]
        wr, sel8, ident = k["router"], k["sel8"], k["ident"]
    for nt in range(4):
        sl = slice(nt * 512, (nt + 1) * 512)
        xb = [x_sb.b(nt)]
        _rms_tile(P, x_sb, x_sb.t[:, :, sl], sq, rs, gcol, hT.t[:, :, sl], [hT.b(nt)], xbufs=xb)
        if not moe:
            _ffn_core(P, hT, x_sb, nt, "w_ff_gate", "w_ff_up", "w_ff_down", 0, FC, act, wgu, wd, sgt)
            continue
        g = k["gains"]
        psl = _ps(P)
        for sub in range(4):
            ssl = slice(nt * 512 + sub * 128, nt * 512 + (sub + 1) * 128)
            for kk in range(8):
                P.op("vector", _I("scalar_tensor_tensor", out=h32.t[:, kk, :], in0=x_sb.t[:, kk, ssl], scalar=g.t[:, gcol + kk:gcol + kk + 1],
                                  in1=rs.t[:, sub * 128:(sub + 1) * 128], op0=ALU.mult, op1=ALU.mult), xb + rs.all() + g.all(), h32.all())
            for kk in range(8):
                P.mm(psl.t[:, sub * 8:(sub + 1) * 8], h32.t[:, kk, :], wr.t[:, kk * 8:(kk + 1) * 8], kk == 0, kk == 7,
                     h32.all() + wr.all(), psl.all())
        P.copy("vector", lg.t[:].rearrange("p a b -> p (a b)"), psl.t[:, 0:32], psl.all(), lg.all())
        psl.busy = False
        for sub in range(4):
            P.op("vector", _I("max", out=m8.t[:, sub, :], in_=lg.t[:, sub, :]), lg.all(), m8.all())
        P.op("vector", _I("tensor_scalar", out=nm1.t[:], in0=m8.t[:, :, 0], scalar1=-1.0, scalar2=None, op0=ALU.mult), m8.all(), nm1.all())
        for sub in range(4):
            P.op("scalar", _I("activation", out=ex.t[:, sub, :], in_=lg.t[:, sub, :], func=AF.Exp, bias=nm1.t[:, sub:sub + 1]), lg.all() + nm1.all(), ex.all())
            P.op("vector", _I("tensor_scalar", out=msk.t[:, sub, :], in0=lg.t[:, sub, :], scalar1=m8.t[:, sub, 1:2], scalar2=None, op0=ALU.is_ge), lg.all() + m8.all(), msk.all())
        P.op("vector", _I("tensor_tensor", out=ex.t[:], in0=ex.t[:], in1=msk.t[:], op=ALU.mult), ex.all() + msk.all(), ex.all())
        P.op("vector", _I("tensor_reduce", out=den.t[:], in_=ex.t[:], axis=AX.X, op=ALU.add), ex.all(), den.all())
        P.op("vector", _I("reciprocal", out=den.t[:], in_=den.t[:]), den.all(), den.all())
        P.op("vector", _I("tensor_tensor", out=ex.t[:], in0=ex.t[:], in1=den.t[:].unsqueeze(2).to_broadcast([128, 4, 8]), op=ALU.mult), ex.all() + den.all(), ex.all())
        pst = _ps(P)
        for sub in range(4):
            P.tr(pst.t[0:8, sub * 128:(sub + 1) * 128], ex.t[:, sub, :], ident.t[:], ex.all() + ident.all(), pst.all())
        P.copy("vector", gT.t[:], pst.t[0:8, :], pst.all(), gT.all())
        pst.busy = False
        P.dump(f"gT{nt}", gT, gT.t[:], [8, 512])
        for e in range(8):
            psb = _ps(P)
            P.mm(psb.t[:], sel8.t[:, e * 128:(e + 1) * 128], gT.t[:], True, True, sel8.all() + gT.all(), psb.all())
            P.copy("scalar", gbc.t[:], psb.t[:], psb.all(), gbc.all())
            psb.busy = False
            _ffn_core(P, hT, x_sb, nt, "w_moe_gate", "w_moe_up", "w_moe_down", e, FC, act, wgu, wd, sgt, gbc=gbc, tg=tg)
    c.release(m)


def _ple(P, s, i, x_sb):
    c = P.c
    m = c.mark()
    sq = c.sb("pl_sq", [128, 8, 512], BF16)
    rs = c.sb("pl_rs", [128, 512], F32)
    h3 = c.sb("pl_h3", [128, 8, 512], BF16)
    pf = c.sb("pl_pf", [128, 2, 512], F32)
    pb = c.sb("pl_pb", [128, 2, 512], BF16)
    wpg = [c.sb(f"pl_wpg{j}", [128, 8, 128], BF16) for j in range(2)]
    wpl = [c.sb(f"pl_wpl{j}", [128, 2, 128], BF16) for j in range(2)]
    sg = [c.sb(f"pl_sg{j}", [128, 512], F32) for j in range(2)]
    tt_ = [c.sb(f"pl_t{j}", [128, 512], F32) for j in range(2)]
    for nt in range(4):
        sl = slice(nt * 512, (nt + 1) * 512)
        xb = [x_sb.b(nt)]
        _rms_tile(P, x_sb, x_sb.t[:, :, sl], sq, rs, i * 24 + 16, h3.t[:], h3.all(), xbufs=xb)
        P.dma(pf.t[:], P.pT[i, s][:, :, sl], w=pf.all())
        P.copy("gpsimd", pb.t[:], pf.t[:], pf.all(), pb.all())
        for oc in range(8):
            w1, w2 = wpg[oc % 2], wpl[oc % 2]
            _load_w(P, w1, "w_ple_gate", i, 8, oc * 128, 128)
            _load_w(P, w2, "w_ple", i, 2, oc * 128, 128)
            psg = _ps(P)
            for kk in range(8):
                P.mm(psg.t[:], w1.t[:, kk, :], h3.t[:, kk, :], kk == 0, kk == 7, w1.all() + h3.all(), psg.all())
            st = sg[oc % 2]
            P.op("scalar", _I("activation", out=st.t[:], in_=psg.t[:], func=AF.Sigmoid), psg.all(), st.all())
            psg.busy = False
            psp = _ps(P)
            for kk in range(2):
                P.mm(psp.t[:], w2.t[:, kk, :], pb.t[:, kk, :], kk == 0, kk == 1, w2.all() + pb.all(), psp.all())
            t = tt_[oc % 2]
            P.op("vector", _I("tensor_tensor", out=t.t[:], in0=psp.t[:], in1=st.t[:], op=ALU.mult), psp.all() + st.all(), t.all())
            psp.busy = False
            P.op("gpsimd", _I("tensor_tensor", out=x_sb.t[:, oc, sl], in0=x_sb.t[:, oc, sl], in1=t.t[:], op=ALU.add), t.all() + xb, xb)
    c.release(m)


def _final(P, s, x_sb):
    c = P.c
    m = c.mark()
    sq = c.sb("fn_sq", [128, 8, 512], BF16)
    rs = c.sb("fn_rs", [128, 512], F32)
    o = [c.sb(f"fn_o{j}", [128, 8, 512], F32) for j in range(2)]
    for nt in range(4):
        sl = slice(nt * 512, (nt + 1) * 512)
        ot = o[nt % 2]
        _rms_tile(P, x_sb, x_sb.t[:, :, sl], sq, rs, 48, ot.t[:], ot.all(), xbufs=[x_sb.b(nt)])
        P.dma(P.outT[s][:, :, sl], ot.t[:], r=ot.all())
    c.release(m)


def _layer(P, s, i, last, first=None):
    c = P.c
    m = c.mark()
    hT = c.sb("hT", [128, 8, T], BF16)
    if first is None:
        first = (i == 0)
    if first:
        src, sb_ = P.xT[s], []
    else:
        src, sb_ = P.xscr.t[s], [P.xscr.b(s)]
    _norm_phase(P, s, i, src, sb_, i * 24, hT)
    _deltanet3(P, s, i, hT)
    _ssd(P, s, i, hT)
    _attn(P, s, i, hT)
    x_sb = c.sb("x_sb", [128, 8, T], F32)
    _merge(P, s, i, hT, x_sb, src, sb_)
    P.dump(f"xmix{i}", x_sb, x_sb.t[:], [128, 8, T])
    _ffn_phase(P, s, i, hT, x_sb)
    P.dump(f"xffn{i}", x_sb, x_sb.t[:], [128, 8, T])
    _ple(P, s, i, x_sb)
    P.dump(f"xout{i}", x_sb, x_sb.t[:], [128, 8, T])
    if not last:
        P.dma(P.xscr.t[s], x_sb.t[:], r=x_sb.all(), w=[P.xscr.b(s)])
    else:
        _final(P, s, x_sb)
    c.release(m)


def build_program(n_seq, dbg=None, layers=(0, 1), skip_w=None):
    P = Prog(n_seq, dbg=dbg, skip_w=skip_w)
    _setup(P)
    for s in range(n_seq):
        for i in layers:
            _layer(P, s, i, last=(i == layers[-1]), first=(i == layers[0]))
    P.c.finish()
    return P


_CACHE = {}


def kernel(**inputs):
    n_cores = 8
    B = inputs["x"].shape[0]
    ns = B // n_cores
    if "P" not in _CACHE:
        _CACHE["P"] = build_program(ns)
    P = _CACHE["P"]
    hw = _host_weights(inputs)
    in_maps = []
    for cidx in range(n_cores):
        acts = _host_acts(inputs, cidx * ns, ns)
        im = {}
        for name in P.dram_in:
            im[name] = hw[name] if name in hw else acts[name]
        in_maps.append(im)
    res = run_bass_kernel_spmd(P.nc, in_maps, core_ids=list(range(n_cores)))
    outs = []
    for cidx in range(n_cores):
        oT = np.asarray(res.results[cidx]["outT"])
        outs.append(np.ascontiguousarray(oT.transpose(0, 3, 2, 1)).reshape(ns, T, D))
    return np.concatenate(outs, axis=0).astype(np.float32)


DN_NH = 2


def _deltanet2(P, s, i, hT):
    c, k = P.c, P.k
    ident, masks, sel6 = k["ident"], k["masks"], k["sel6"]
    mDA, mDP, mDQ = masks.t[:, 0:128], masks.t[:, 128:256], masks.t[:, 256:384]
    NH = DN_NH
    m_phase = c.mark()
    gamT = c.sb("dn_gamT", [6, T], F32)
    gbT = c.sb("dn_gbT", [6, T], F32)
    tok = c.sb("dn_tok", [128, 16, 128], F32)
    bgtok = c.sb("dn_bgtok", [128, 16, 8], F32)
    eglb = c.sb("dn_eglb", [128, 6, 32], F32)
    negA = c.sb("dn_negA", [6, 1], F32)
    identb = c.sb("dn_identb", [128, 128], BF16)
    P.copy("vector", identb.t[:], ident.t[:], ident.all(), identb.all())
    m1 = c.mark()
    wab = c.sb("dn_wab", [128, 8, 12], BF16)
    _load_wcols(P, wab, i, C_DNA, 12)
    t0_ = c.sb("dn_t0", [6, T], F32)
    t1_ = c.sb("dn_t1", [6, T], F32)
    lnb = c.sb("dn_lnb", [6, T], F32)
    stk = c.sb("dn_stk", [128, T], F32)
    P.op("gpsimd", _I("memset", stk.t[:], 0.0), w=stk.all())
    p6 = k["dnp6"]
    P.op("scalar", _I("activation", out=negA.t[:], in_=p6.t[:, 2 * i:2 * i + 1], func=AF.Exp), p6.all(), negA.all())
    P.op("vector", _I("tensor_scalar", out=negA.t[:], in0=negA.t[:], scalar1=-1.0, scalar2=None, op0=ALU.mult), negA.all(), negA.all())

    def ev_a(nt, ps):
        sl = slice(nt * 512, (nt + 1) * 512)
        P.op("scalar", _I("activation", out=t0_.t[:, sl], in_=ps.t[0:6, :], func=AF.Exp, bias=p6.t[:, 2 * i + 1:2 * i + 2]),
             ps.all() + p6.all(), t0_.all())
    _proj_fm(P, hT, wab, 0, 6, ev_a)
    P.op("scalar", _I("activation", out=t0_.t[:], in_=t0_.t[:], func=AF.Ln, bias=1.0), t0_.all(), t0_.all())
    P.op("vector", _I("tensor_scalar", out=t0_.t[:], in0=t0_.t[:], scalar1=negA.t[:, 0:1], scalar2=None, op0=ALU.mult),
         t0_.all() + negA.all(), t0_.all())
    gres = _cumsum64(P, t0_, t1_, 6)
    P.copy("vector", gamT.t[:], gres.t[:], gres.all(), gamT.all())

    def ev_b(nt, ps):
        sl = slice(nt * 512, (nt + 1) * 512)
        P.op("scalar", _I("activation", out=lnb.t[:, sl], in_=ps.t[0:6, :], func=AF.Exp, scale=-1.0), ps.all(), lnb.all())
    _proj_fm(P, hT, wab, 6, 6, ev_b)
    P.op("scalar", _I("activation", out=lnb.t[:], in_=lnb.t[:], func=AF.Ln, bias=1.0), lnb.all(), lnb.all())
    P.op("vector", _I("tensor_scalar", out=lnb.t[:], in0=lnb.t[:], scalar1=-1.0, scalar2=None, op0=ALU.mult), lnb.all(), lnb.all())
    P.op("vector", _I("tensor_tensor", out=gbT.t[:], in0=gamT.t[:], in1=lnb.t[:], op=ALU.add), gamT.all() + lnb.all(), gbT.all())
    P.op("vector", _I("tensor_scalar", out=stk.t[0:6, :], in0=gamT.t[:], scalar1=-1.0, scalar2=None, op0=ALU.mult), gamT.all(), stk.all())
    P.copy("vector", stk.t[32:38, :], gbT.t[:], gbT.all(), stk.all())
    g3 = gamT.t[:].rearrange("p (c t) -> p c t", t=64)
    P.op("vector", _I("tensor_tensor", out=t0_.t[:].rearrange("p (c t) -> p c t", t=64), in0=g3[:, :, 63:64].to_broadcast([6, 32, 64]),
                      in1=g3, op=ALU.subtract), gamT.all(), t0_.all())
    P.op("scalar", _I("activation", out=stk.t[64:70, :], in_=t0_.t[:], func=AF.Exp), t0_.all(), stk.all())
    P.op("scalar", _I("activation", out=stk.t[96:102, :], in_=lnb.t[:], func=AF.Exp), lnb.all(), stk.all())
    for u4 in range(4):
        ps = _ps(P)
        for j in range(4):
            u = u4 * 4 + j
            P.tr(ps.t[:, j * 128:(j + 1) * 128], stk.t[:, u * 128:(u + 1) * 128], ident.t[:], stk.all() + ident.all(), ps.all())
        P.copy("vector", tok.t[:, u4 * 4:(u4 + 1) * 4, :].rearrange("p a b -> p (a b)"), ps.t[:], ps.all(), tok.all())
        ps.busy = False
    P.op("scalar", _I("activation", out=bgtok.t[:, :, 0:6], in_=tok.t[:, :, 32:38], func=AF.Exp), tok.all(), bgtok.all())
    P.op("scalar", _I("activation", out=t1_.t[:, 0:32], in_=g3[:, :, 63], func=AF.Exp), gamT.all(), t1_.all())
    ps = _ps(P)
    for h in range(6):
        P.mm(ps.t[:, h * 32:(h + 1) * 32], sel6.t[:, h * 128:(h + 1) * 128], t1_.t[:, 0:32], True, True, sel6.all() + t1_.all(), ps.all())
    P.copy("vector", eglb.t[:].rearrange("p a b -> p (a b)"), ps.t[:, 0:192], ps.all(), eglb.all())
    ps.busy = False
    c.release(m1)
    wq = [c.sb(f"dn_w{j}", [128, 8, 128], BF16) for j in range(4)]
    raw = c.sb("dn_raw", [128, T], F32)
    cac = c.sb("dn_cac", [128, T], F32)
    rin = c.sb("dn_rin", [128, T], F32)
    cw = k["dnconv"]
    nrm = k["dnnorm"]
    HB = []
    for hh in range(NH):
        d = {}
        d["cq"] = c.sb(f"dn_cq{hh}", [128, T], BF16)
        d["ck"] = c.sb(f"dn_ck{hh}", [128, T], BF16)
        d["cv"] = c.sb(f"dn_cv{hh}", [128, T], BF16)
        d["zs"] = c.sb(f"dn_zs{hh}", [128, T], BF16)
        d["yT"] = c.sb(f"dn_yT{hh}", [128, T], BF16)
        d["S"] = [c.sb(f"dn_S{hh}_{j}", [128, 128], F32) for j in range(3)]
        d["tl"] = [c.sb(f"dn_tl{hh}_{j}", [128, 128], F32) for j in range(18)]
        d["ss"] = c.sb(f"dn_ss{hh}", [128, 2], F32)
        P.op("gpsimd", _I("memset", d["tl"][0].t[:], 0.0), w=d["tl"][0].all())
        P.op("gpsimd", _I("memset", d["tl"][1].t[:], 0.0), w=d["tl"][1].all())
        HB.append(d)

    def head_prep(h, d):
        cols = [C_DNQ + h * 128, C_DNK + h * 128, C_DNV + h * 128, C_DNZ + h * 128]
        for j in range(4):
            _load_wcols(P, wq[j], i, cols[j], 128)
        for j, dst in enumerate((d["cq"], d["ck"], d["cv"])):
            def ev_raw(nt, ps):
                sl = slice(nt * 512, (nt + 1) * 512)
                P.copy(P.ev_eng(), raw.t[:, sl], ps.t[:], ps.all(), raw.all())
            _proj_fm(P, hT, wq[j], 0, 128, ev_raw)
            cb = (i * 18 + j * 6 + h) * 4
            P.op("vector", _I("tensor_scalar", out=cac.t[:], in0=raw.t[:], scalar1=cw.t[:, cb + 3:cb + 4], scalar2=None, op0=ALU.mult),
                 raw.all() + cw.all(), cac.all())
            for sft in (1, 2, 3):
                P.op("vector", _I("scalar_tensor_tensor", out=cac.t[:, sft:], in0=raw.t[:, :T - sft], scalar=cw.t[:, cb + 3 - sft:cb + 4 - sft],
                                  in1=cac.t[:, sft:], op0=ALU.mult, op1=ALU.add), raw.all() + cw.all() + cac.all(), cac.all())
            if j == 2:
                P.op("scalar", _I("activation", out=dst.t[:], in_=cac.t[:], func=AF.Silu), cac.all(), dst.all())
                continue
            P.op("scalar", _I("activation", out=cac.t[:], in_=cac.t[:], func=AF.Silu), cac.all(), cac.all())
            P.op("scalar", _I("activation", out=raw.t[:], in_=cac.t[:], func=AF.Square), cac.all(), raw.all())
            for nt in range(4):
                sl = slice(nt * 512, (nt + 1) * 512)
                ps = _ps(P)
                P.mm(ps.t[:], P.ones_f.t[:], raw.t[:, sl], True, True, P.ones_f.all() + raw.all(), ps.all())
                _rstd(P, ps.t[:], ps.all(), rin, rin.t[:, sl], 1.0, extra_bias=(-0.5 * np.log(128.0) if j == 0 else 0.0))
                ps.busy = False
            P.op("vector", _I("tensor_tensor", out=dst.t[:], in0=cac.t[:], in1=rin.t[:], op=ALU.mult), cac.all() + rin.all(), dst.all())
        zs = d["zs"]

        def ev_z(nt, ps):
            sl = slice(nt * 512, (nt + 1) * 512)
            P.op("scalar", _I("activation", out=zs.t[:, sl], in_=ps.t[:], func=AF.Silu), ps.all(), zs.all())
        _proj_fm(P, hT, wq[3], 0, 128, ev_z)
        P.op("gpsimd", _I("memset", d["S"][0].t[:], 0.0), w=d["S"][0].all())

    def unit_gen(h, d, u):
        cq, ck, cv, zs, yT, S, tl, ssu = d["cq"], d["ck"], d["cv"], d["zs"], d["yT"], d["S"], d["tl"], d["ss"]
        qdA, qdB, kbg, kdec, vb, attnT, WT, U, vnew, o_n = tl[0:10]
        t = tl[10:18]
        tA, DA, tB, DP, tC, DQ, Eb = t[0], t[1], t[2], t[3], t[4], t[5], t[6]
        t0 = u * 128
        usl = slice(t0, t0 + 128)
        ngam = tok.t[:, u, h:h + 1]
        gbt = tok.t[:, u, 32 + h:33 + h]
        kd = tok.t[:, u, 64 + h:65 + h]
        beta = tok.t[:, u, 96 + h:97 + h]
        bg = bgtok.t[:, u, h:h + 1]
        selh = sel6.t[:, h * 128:(h + 1) * 128]
        psb = _ps(P)
        P.mm(psb.t[:, 0:128], selh, gamT.t[:, usl], True, True, sel6.all() + gamT.all(), psb.all())
        P.mm(psb.t[:, 128:256], selh, gbT.t[:, usl], True, True, sel6.all() + gbT.all(), psb.all())
        pst = _ps(P)
        pstb = pst.t[:].bitcast(BF16)
        P.tr(pstb[:, 0:128], ck.t[:, usl], identb.t[:], ck.all() + identb.all(), pst.all())
        P.tr(pstb[:, 128:256], cv.t[:, usl], identb.t[:], cv.all() + identb.all(), pst.all())
        psk = _ps(P)
        P.mm(psk.t[:, 0:128], ck.t[:, usl], ck.t[:, usl], True, True, ck.all(), psk.all())
        P.mm(psk.t[:, 128:256], ck.t[:, usl], cq.t[:, usl], True, True, ck.all() + cq.all(), psk.all())
        yield
        P.op("vector", _I("scalar_tensor_tensor", out=tA.t[:], in0=psb.t[:, 0:128], scalar=-1.0, in1=mDA, op0=ALU.mult, op1=ALU.add),
             psb.all() + masks.all(), tA.all())
        P.op("vector", _I("tensor_tensor", out=tB.t[:], in0=psb.t[:, 128:256], in1=mDP, op=ALU.add), psb.all() + masks.all(), tB.all())
        P.op("vector", _I("tensor_tensor", out=tC.t[:], in0=psb.t[:, 0:128], in1=mDQ, op=ALU.add), psb.all() + masks.all(), tC.all())
        P.op("scalar", _I("activation", out=Eb.t[:], in_=psb.t[:, 0:128], func=AF.Exp), psb.all(), Eb.all())
        psb.busy = False
        P.op("scalar", _I("activation", out=DA.t[:], in_=tA.t[:], func=AF.Exp, bias=gbt), tA.all() + tok.all(), DA.all())
        P.op("scalar", _I("activation", out=DP.t[:], in_=tB.t[:], func=AF.Exp, bias=ngam), tB.all() + tok.all(), DP.all())
        P.op("scalar", _I("activation", out=DQ.t[:], in_=tC.t[:], func=AF.Exp, bias=ngam), tC.all() + tok.all(), DQ.all())
        P.op("vector", _I("tensor_scalar", out=kbg.t[:], in0=pstb[:, 0:128], scalar1=bg, scalar2=None, op0=ALU.mult), pst.all() + bgtok.all(), kbg.all())
        P.op("vector", _I("tensor_scalar", out=kdec.t[:], in0=pstb[:, 0:128], scalar1=kd, scalar2=None, op0=ALU.mult), pst.all() + tok.all(), kdec.all())
        P.op("vector", _I("tensor_scalar", out=vb.t[:], in0=pstb[:, 128:256], scalar1=beta, scalar2=None, op0=ALU.mult), pst.all() + tok.all(), vb.all())
        pst.busy = False
        yield
        P.op("gpsimd", _I("tensor_tensor", out=qdA.t[:, 0:64], in0=cq.t[:, t0:t0 + 64], in1=Eb.t[:, 0:64], op=ALU.mult), cq.all() + Eb.all(), qdA.all())
        P.op("gpsimd", _I("tensor_tensor", out=qdB.t[:, 64:128], in0=cq.t[:, t0 + 64:t0 + 128], in1=Eb.t[:, 64:128], op=ALU.mult), cq.all() + Eb.all(), qdB.all())
        A0, P0, X0 = t[0], t[2], t[4]
        P.op("vector", _I("tensor_tensor", out=A0.t[:], in0=psk.t[:, 0:128], in1=DA.t[:], op=ALU.mult), psk.all() + DA.all(), A0.all())
        P.op("vector", _I("tensor_tensor", out=P0.t[:], in0=psk.t[:, 0:128], in1=DP.t[:], op=ALU.mult), psk.all() + DP.all(), P0.all())
        P.op("vector", _I("tensor_tensor", out=attnT.t[:], in0=psk.t[:, 128:256], in1=DQ.t[:], op=ALU.mult), psk.all() + DQ.all(), attnT.all())
        psk.busy = False
        P.op("gpsimd", _I("tensor_tensor", out=X0.t[:], in0=ident.t[:], in1=P0.t[:], op=ALU.subtract), ident.all() + P0.all(), X0.all())
        yield
        Am, Pm, X = A0, P0, X0
        Abuf, Pbuf, Xbuf = [t[1], t[6]], [t[3], t[7]], [t[5], t[4]]
        for n in range(5):
            An, Pn, Xn = Abuf[n % 2], Pbuf[n % 2], Xbuf[n % 2]
            psn = _ps(P)
            P.mm(psn.t[:, 0:128], Pm.t[:], Am.t[:], True, True, Pm.all() + Am.all(), psn.all())
            if n < 4:
                P.mm(psn.t[:, 128:256], Am.t[:], Pm.t[:], True, True, Pm.all() + Am.all(), psn.all())
            yield
            P.copy("scalar", An.t[:], psn.t[:, 0:128], psn.all(), An.all())
            if n < 4:
                P.copy("scalar", Pn.t[:], psn.t[:, 128:256], psn.all(), Pn.all())
            psn.busy = False
            psx = _ps(P)
            P.mm(psx.t[:, 0:128], An.t[:], X.t[:], True, True, An.all() + X.all(), psx.all())
            yield
            P.op("vector", _I("tensor_tensor", out=Xn.t[:], in0=psx.t[:, 0:128], in1=X.t[:], op=ALU.add), psx.all() + X.all(), Xn.all())
            psx.busy = False
            Am, Pm, X = An, Pn, Xn
        TTm = X
        psw = _ps(P)
        P.mm(psw.t[:, 0:128], kbg.t[:], TTm.t[:], True, True, kbg.all() + TTm.all(), psw.all())
        P.mm(psw.t[:, 128:256], TTm.t[:], vb.t[:], True, True, vb.all() + TTm.all(), psw.all())
        yield
        P.copy("scalar", WT.t[:], psw.t[:, 0:128], psw.all(), WT.all())
        P.copy("vector", U.t[:], psw.t[:, 128:256], psw.all(), U.all())
        psw.busy = False
        si = 2 * u
        Sa, Sb, Sn = S[si % 3], S[(si + 1) % 3], S[(si + 2) % 3]
        ps1 = _ps(P)
        P.mm(ps1.t[:, 0:128], WT.t[:], Sa.t[:], True, True, WT.all() + Sa.all(), ps1.all())
        pso = _ps(P)
        P.mm(pso.t[:, 0:128], qdA.t[:], Sa.t[:], True, False, qdA.all() + Sa.all(), pso.all())
        yield
        P.op("vector", _I("tensor_tensor", out=vnew.t[0:64, :], in0=U.t[0:64, :], in1=ps1.t[0:64, 0:128], op=ALU.subtract), U.all() + ps1.all(), vnew.all())
        ps1.busy = False
        pss = _ps(P)
        P.mm(pss.t[:, 0:128], kdec.t[0:64, :], vnew.t[0:64, :], True, True, kdec.all() + vnew.all(), pss.all())
        yield
        P.op("vector", _I("scalar_tensor_tensor", out=Sb.t[:], in0=Sa.t[:], scalar=eglb.t[:, h, 2 * u:2 * u + 1], in1=pss.t[:, 0:128],
                          op0=ALU.mult, op1=ALU.add), Sa.all() + pss.all() + eglb.all(), Sb.all())
        pss.busy = False
        ps2 = _ps(P)
        P.mm(ps2.t[:, 0:128], WT.t[:], Sb.t[:], True, True, WT.all() + Sb.all(), ps2.all())
        P.mm(pso.t[:, 0:128], qdB.t[:], Sb.t[:], False, False, qdB.all() + Sb.all(), pso.all())
        yield
        P.op("vector", _I("tensor_tensor", out=vnew.t[64:128, :], in0=U.t[64:128, :], in1=ps2.t[64:128, 0:128], op=ALU.subtract), U.all() + ps2.all(), vnew.all())
        ps2.busy = False
        P.mm(pso.t[:, 0:128], attnT.t[:], vnew.t[:], False, True, attnT.all() + vnew.all(), pso.all())
        pss2 = _ps(P)
        P.mm(pss2.t[:, 0:128], kdec.t[64:128, :], vnew.t[64:128, :], True, True, kdec.all() + vnew.all(), pss2.all())
        yield
        P.op("vector", _I("scalar_tensor_tensor", out=Sn.t[:], in0=Sb.t[:], scalar=eglb.t[:, h, 2 * u + 1:2 * u + 2], in1=pss2.t[:, 0:128],
                          op0=ALU.mult, op1=ALU.add), Sb.all() + pss2.all() + eglb.all(), Sn.all())
        pss2.busy = False
        junk = t[0]
        P.op("scalar", _I("activation", out=junk.t[:], in_=pso.t[:, 0:128], func=AF.Square, accum_out=ssu.t[:, 0:1]), pso.all(), junk.all() + ssu.all())
        _rstd(P, ssu.t[:, 0:1], ssu.all(), ssu, ssu.t[:, 1:2], 1.0 / 128)
        P.op("vector", _I("tensor_scalar", out=o_n.t[:], in0=pso.t[:, 0:128], scalar1=ssu.t[:, 1:2], scalar2=None, op0=ALU.mult), pso.all() + ssu.all(), o_n.all())
        pso.busy = False
        psy = _ps(P)
        P.tr(psy.t[:, 0:128], o_n.t[:], ident.t[:], o_n.all() + ident.all(), psy.all())
        yield
        P.op("vector", _I("scalar_tensor_tensor", out=yT.t[:, usl], in0=psy.t[:, 0:128], scalar=nrm.t[:, i:i + 1], in1=zs.t[:, usl],
                          op0=ALU.mult, op1=ALU.mult), psy.all() + nrm.all() + zs.all(), yT.all())
        psy.busy = False

    for h0 in range(0, 6, NH):
        heads = list(range(h0, min(6, h0 + NH)))
        for hh, h in enumerate(heads):
            head_prep(h, HB[hh])
        for u in range(16):
            _conv_some(P, 2)
            gens = [unit_gen(h, HB[hh], u) for hh, h in enumerate(heads)]
            while gens:
                nxt = []
                for g in gens:
                    try:
                        next(g)
                        nxt.append(g)
                    except StopIteration:
                        pass
                gens = nxt
        for hh, h in enumerate(heads):
            P.dma(P.ydn.t[:, h, :], HB[hh]["yT"].t[:], r=HB[hh]["yT"].all(), w=P.ydn.all())
    c.release(m_phase)


def _deltanet3(P, s, i, hT):
    c, k = P.c, P.k
    ident, masks, sel6 = k["ident"], k["masks"], k["sel6"]
    mDA, mDP, mDQ = masks.t[:, 0:128], masks.t[:, 128:256], masks.t[:, 256:384]
    NH = DN_NH
    m_phase = c.mark()
    tok = c.sb("dn_tok", [128, 16, 128], F32)
    bgtok = c.sb("dn_bgtok", [128, 16, 8], F32)
    eglb = c.sb("dn_eglb", [128, 6, 32], F32)
    negA = c.sb("dn_negA", [6, 1], F32)
    identb = c.sb("dn_identb", [128, 128], BF16)
    ghl = c.sb("dn_ghl", [38, T], BF16)
    gbhl = c.sb("dn_gbhl", [38, T], BF16)
    sel38 = c.sb("dn_sel38", [38, 768], BF16)
    P.copy("vector", identb.t[:], ident.t[:], ident.all(), identb.all())
    m1 = c.mark()
    gamT = c.sb("dn_gamT", [6, T], F32)
    gbT = c.sb("dn_gbT", [6, T], F32)
    wab = c.sb("dn_wab", [128, 8, 12], BF16)
    _load_wcols(P, wab, i, C_DNA, 12)
    t0_ = c.sb("dn_t0", [6, T], F32)
    t1_ = c.sb("dn_t1", [6, T], F32)
    lnb = c.sb("dn_lnb", [6, T], F32)
    stk = c.sb("dn_stk", [128, T], F32)
    P.op("gpsimd", _I("memset", stk.t[:], 0.0), w=stk.all())
    p6 = k["dnp6"]
    P.op("scalar", _I("activation", out=negA.t[:], in_=p6.t[:, 2 * i:2 * i + 1], func=AF.Exp), p6.all(), negA.all())
    P.op("vector", _I("tensor_scalar", out=negA.t[:], in0=negA.t[:], scalar1=-1.0, scalar2=None, op0=ALU.mult), negA.all(), negA.all())

    def ev_a(nt, ps):
        sl = slice(nt * 512, (nt + 1) * 512)
        P.op("scalar", _I("activation", out=t0_.t[:, sl], in_=ps.t[0:6, :], func=AF.Exp, bias=p6.t[:, 2 * i + 1:2 * i + 2]),
             ps.all() + p6.all(), t0_.all())
    _proj_fm(P, hT, wab, 0, 6, ev_a)
    P.op("scalar", _I("activation", out=t0_.t[:], in_=t0_.t[:], func=AF.Ln, bias=1.0), t0_.all(), t0_.all())
    P.op("vector", _I("tensor_scalar", out=t0_.t[:], in0=t0_.t[:], scalar1=negA.t[:, 0:1], scalar2=None, op0=ALU.mult),
         t0_.all() + negA.all(), t0_.all())
    gres = _cumsum64(P, t0_, t1_, 6)
    P.copy("vector", gamT.t[:], gres.t[:], gres.all(), gamT.all())

    def ev_b(nt, ps):
        sl = slice(nt * 512, (nt + 1) * 512)
        P.op("scalar", _I("activation", out=lnb.t[:, sl], in_=ps.t[0:6, :], func=AF.Exp, scale=-1.0), ps.all(), lnb.all())
    _proj_fm(P, hT, wab, 6, 6, ev_b)
    P.op("scalar", _I("activation", out=lnb.t[:], in_=lnb.t[:], func=AF.Ln, bias=1.0), lnb.all(), lnb.all())
    P.op("vector", _I("tensor_scalar", out=lnb.t[:], in0=lnb.t[:], scalar1=-1.0, scalar2=None, op0=ALU.mult), lnb.all(), lnb.all())
    P.op("vector", _I("tensor_tensor", out=gbT.t[:], in0=gamT.t[:], in1=lnb.t[:], op=ALU.add), gamT.all() + lnb.all(), gbT.all())
    P.op("vector", _I("tensor_scalar", out=stk.t[0:6, :], in0=gamT.t[:], scalar1=-1.0, scalar2=None, op0=ALU.mult), gamT.all(), stk.all())
    P.copy("vector", stk.t[32:38, :], gbT.t[:], gbT.all(), stk.all())
    g3 = gamT.t[:].rearrange("p (c t) -> p c t", t=64)
    P.op("vector", _I("tensor_tensor", out=t0_.t[:].rearrange("p (c t) -> p c t", t=64), in0=g3[:, :, 63:64].to_broadcast([6, 32, 64]),
                      in1=g3, op=ALU.subtract), gamT.all(), t0_.all())
    P.op("scalar", _I("activation", out=stk.t[64:70, :], in_=t0_.t[:], func=AF.Exp), t0_.all(), stk.all())
    P.op("scalar", _I("activation", out=stk.t[96:102, :], in_=lnb.t[:], func=AF.Exp), lnb.all(), stk.all())
    for u4 in range(4):
        ps = _ps(P)
        for j in range(4):
            u = u4 * 4 + j
            P.tr(ps.t[:, j * 128:(j + 1) * 128], stk.t[:, u * 128:(u + 1) * 128], ident.t[:], stk.all() + ident.all(), ps.all())
        P.copy("vector", tok.t[:, u4 * 4:(u4 + 1) * 4, :].rearrange("p a b -> p (a b)"), ps.t[:], ps.all(), tok.all())
        ps.busy = False
    P.op("scalar", _I("activation", out=bgtok.t[:, :, 0:6], in_=tok.t[:, :, 32:38], func=AF.Exp), tok.all(), bgtok.all())
    P.op("scalar", _I("activation", out=t1_.t[:, 0:32], in_=g3[:, :, 63], func=AF.Exp), gamT.all(), t1_.all())
    ps = _ps(P)
    for h in range(6):
        P.mm(ps.t[:, h * 32:(h + 1) * 32], sel6.t[:, h * 128:(h + 1) * 128], t1_.t[:, 0:32], True, True, sel6.all() + t1_.all(), ps.all())
    P.copy("vector", eglb.t[:].rearrange("p a b -> p (a b)"), ps.t[:, 0:192], ps.all(), eglb.all())
    ps.busy = False
    for src_, dst_ in ((gamT, ghl), (gbT, gbhl)):
        P.op("gpsimd", _I("memset", dst_.t[:], 0.0), w=dst_.all())
        P.copy("vector", dst_.t[0:6, :], src_.t[:], src_.all(), dst_.all())
        P.op("vector", _I("tensor_tensor", out=t0_.t[:], in0=src_.t[:], in1=dst_.t[0:6, :], op=ALU.subtract), src_.all() + dst_.all(), t0_.all())
        P.copy("vector", dst_.t[32:38, :], t0_.t[:], t0_.all(), dst_.all())
    P.op("gpsimd", _I("memset", sel38.t[:], 0.0), w=sel38.all())
    P.copy("vector", sel38.t[0:6, :], sel6.t[:], sel6.all(), sel38.all())
    P.copy("vector", sel38.t[32:38, :], sel6.t[:], sel6.all(), sel38.all())
    c.release(m1)
    NU = DN_NU
    cw = k["dnconv"]
    nrm = k["dnnorm"]
    HB = []
    for hh in range(NH):
        d = {}
        d["cq"] = c.sb(f"dn_cq{hh}", [128, T], BF16)
        d["ck"] = c.sb(f"dn_ck{hh}", [128, T], BF16)
        d["cv"] = c.sb(f"dn_cv{hh}", [128, T], BF16)
        d["zs"] = c.sb(f"dn_zs{hh}", [128, T], BF16)
        d["yT"] = c.sb(f"dn_yT{hh}", [128, T], BF16)
        d["S"] = [c.sb(f"dn_S{hh}_{j}", [128, 128], F32) for j in range(4)]
        d["Sb16"] = [c.sb(f"dn_Sb{hh}_{j}", [128, 128], BF16) for j in range(4)]
        HB.append(d)

    class QS:
        def __init__(self, bank, q):
            self.bank, self.q = bank, q
            self.t = bank.t[:, q * 128:(q + 1) * 128]
            self.tb = bank.t[:, q * 128:(q + 1) * 128].bitcast(BF16)
            self.busy = False

        def all(self):
            return self.bank.all()
    NQ = 32 // (NH * NU)
    QP = [[QS(P.PS[(NQ * kk + j) // 4], (NQ * kk + j) % 4) for j in range(NQ)] for kk in range(NH * NU)]
    qrr = [0] * (NH * NU)

    def qa(kk):
        for _ in range(NQ):
            j = qrr[kk]
            qrr[kk] = (j + 1) % NQ
            if not QP[kk][j].busy:
                QP[kk][j].busy = True
                return QP[kk][j]
        raise RuntimeError("no free PSUM quarter")

    def head_prep(h, d, raw, cac, rin, wq):
        cols = [C_DNQ + h * 128, C_DNK + h * 128, C_DNV + h * 128, C_DNZ + h * 128]
        for j in range(4):
            _load_wcols(P, wq[j], i, cols[j], 128)
        for j, dst in enumerate((d["cq"], d["ck"], d["cv"])):
            def ev_raw(nt, ps):
                sl = slice(nt * 512, (nt + 1) * 512)
                P.copy(P.ev_eng(), raw.t[:, sl], ps.t[:], ps.all(), raw.all())
            _proj_fm(P, hT, wq[j], 0, 128, ev_raw)
            cb = (i * 18 + j * 6 + h) * 4
            P.op("vector", _I("tensor_scalar", out=cac.t[:], in0=raw.t[:], scalar1=cw.t[:, cb + 3:cb + 4], scalar2=None, op0=ALU.mult),
                 raw.all() + cw.all(), cac.all())
            for sft in (1, 2, 3):
                P.op("vector", _I("scalar_tensor_tensor", out=cac.t[:, sft:], in0=raw.t[:, :T - sft], scalar=cw.t[:, cb + 3 - sft:cb + 4 - sft],
                                  in1=cac.t[:, sft:], op0=ALU.mult, op1=ALU.add), raw.all() + cw.all() + cac.all(), cac.all())
            if j == 2:
                P.op("scalar", _I("activation", out=dst.t[:], in_=cac.t[:], func=AF.Silu), cac.all(), dst.all())
                continue
            P.op("scalar", _I("activation", out=cac.t[:], in_=cac.t[:], func=AF.Silu), cac.all(), cac.all())
            P.op("scalar", _I("activation", out=raw.t[:], in_=cac.t[:], func=AF.Square), cac.all(), raw.all())
            for nt in range(4):
                sl = slice(nt * 512, (nt + 1) * 512)
                ps = _ps(P)
                P.mm(ps.t[:], P.ones_f.t[:], raw.t[:, sl], True, True, P.ones_f.all() + raw.all(), ps.all())
                _rstd(P, ps.t[:], ps.all(), rin, rin.t[:, sl], 1.0, extra_bias=(-0.5 * np.log(128.0) if j == 0 else 0.0))
                ps.busy = False
            P.op("vector", _I("tensor_tensor", out=dst.t[:], in0=cac.t[:], in1=rin.t[:], op=ALU.mult), cac.all() + rin.all(), dst.all())
        zs = d["zs"]

        def ev_z(nt, ps):
            sl = slice(nt * 512, (nt + 1) * 512)
            P.op("scalar", _I("activation", out=zs.t[:, sl], in_=ps.t[:], func=AF.Silu), ps.all(), zs.all())
        _proj_fm(P, hT, wq[3], 0, 128, ev_z)
        P.op("gpsimd", _I("memset", d["S"][0].t[:], 0.0), w=d["S"][0].all())
        P.op("gpsimd", _I("memset", d["Sb16"][0].t[:], 0.0), w=d["Sb16"][0].all())

    def unit_gen(h, d, u, tl, ssu, kk, tlb):
        cq, ck, cv, zs, yT, S, S16 = d["cq"], d["ck"], d["cv"], d["zs"], d["yT"], d["S"], d["Sb16"]
        qdA, qdB, kbg, kdec, vb, attnT, WT, vnew = tlb[0:8]
        tb_ = tlb[8:16]
        U, o_n = tl[0:2]
        t = tl[2:10]
        tA, DA, tB, DP, tC, DQ, Eb = t[0], t[1], t[2], t[3], t[4], t[5], t[6]
        t0 = u * 128
        usl = slice(t0, t0 + 128)
        ngam = tok.t[:, u, h:h + 1]
        gbt = tok.t[:, u, 32 + h:33 + h]
        kd = tok.t[:, u, 64 + h:65 + h]
        beta = tok.t[:, u, 96 + h:97 + h]
        bg = bgtok.t[:, u, h:h + 1]
        selh = sel38.t[:, h * 128:(h + 1) * 128]
        pg, pgb, pst, pkk, pkq = qa(kk), qa(kk), qa(kk), qa(kk), qa(kk)
        P.mm(pg.t, selh, ghl.t[:, usl], True, True, sel38.all() + ghl.all(), pg.all())
        P.mm(pgb.t, selh, gbhl.t[:, usl], True, True, sel38.all() + gbhl.all(), pgb.all())
        P.tr(pst.tb[:, 0:128], ck.t[:, usl], identb.t[:], ck.all() + identb.all(), pst.all())
        P.tr(pst.tb[:, 128:256], cv.t[:, usl], identb.t[:], cv.all() + identb.all(), pst.all())
        P.mm(pkk.t, ck.t[:, usl], ck.t[:, usl], True, True, ck.all(), pkk.all())
        P.mm(pkq.t, ck.t[:, usl], cq.t[:, usl], True, True, ck.all() + cq.all(), pkq.all())
        yield
        P.op("vector", _I("scalar_tensor_tensor", out=tA.t[:], in0=pg.t, scalar=-1.0, in1=mDA, op0=ALU.mult, op1=ALU.add), pg.all() + masks.all(), tA.all())
        P.op("vector", _I("tensor_tensor", out=tB.t[:], in0=pgb.t, in1=mDP, op=ALU.add), pgb.all() + masks.all(), tB.all())
        P.op("vector", _I("tensor_tensor", out=tC.t[:], in0=pg.t, in1=mDQ, op=ALU.add), pg.all() + masks.all(), tC.all())
        P.op("scalar", _I("activation", out=Eb.t[:], in_=pg.t, func=AF.Exp), pg.all(), Eb.all())
        pg.busy = False
        pgb.busy = False
        P.op("scalar", _I("activation", out=DA.t[:], in_=tA.t[:], func=AF.Exp, bias=gbt), tA.all() + tok.all(), DA.all())
        P.op("scalar", _I("activation", out=DP.t[:], in_=tB.t[:], func=AF.Exp, bias=ngam), tB.all() + tok.all(), DP.all())
        P.op("scalar", _I("activation", out=DQ.t[:], in_=tC.t[:], func=AF.Exp, bias=ngam), tC.all() + tok.all(), DQ.all())
        P.op("vector", _I("tensor_scalar", out=kbg.t[:], in0=pst.tb[:, 0:128], scalar1=bg, scalar2=None, op0=ALU.mult), pst.all() + bgtok.all(), kbg.all())
        P.op("vector", _I("tensor_scalar", out=kdec.t[:], in0=pst.tb[:, 0:128], scalar1=kd, scalar2=None, op0=ALU.mult), pst.all() + tok.all(), kdec.all())
        P.op("vector", _I("tensor_scalar", out=vb.t[:], in0=pst.tb[:, 128:256], scalar1=beta, scalar2=None, op0=ALU.mult), pst.all() + tok.all(), vb.all())
        pst.busy = False
        yield
        P.op("gpsimd", _I("tensor_tensor", out=qdA.t[:, 0:64], in0=cq.t[:, t0:t0 + 64], in1=Eb.t[:, 0:64], op=ALU.mult), cq.all() + Eb.all(), qdA.all())
        P.op("gpsimd", _I("tensor_tensor", out=qdB.t[:, 64:128], in0=cq.t[:, t0 + 64:t0 + 128], in1=Eb.t[:, 64:128], op=ALU.mult), cq.all() + Eb.all(), qdB.all())
        A0, P0, X0 = tb_[0], tb_[2], tb_[4]
        P.op("vector", _I("tensor_tensor", out=A0.t[:], in0=pkk.t, in1=DA.t[:], op=ALU.mult), pkk.all() + DA.all(), A0.all())
        P.op("vector", _I("tensor_tensor", out=P0.t[:], in0=pkk.t, in1=DP.t[:], op=ALU.mult), pkk.all() + DP.all(), P0.all())
        P.op("vector", _I("tensor_tensor", out=attnT.t[:], in0=pkq.t, in1=DQ.t[:], op=ALU.mult), pkq.all() + DQ.all(), attnT.all())
        pkk.busy = False
        pkq.busy = False
        P.op("gpsimd", _I("tensor_tensor", out=X0.t[:], in0=ident.t[:], in1=P0.t[:], op=ALU.subtract), ident.all() + P0.all(), X0.all())
        yield
        Am, Pm, X = A0, P0, X0
        Abuf, Pbuf, Xbuf = [tb_[1], tb_[6]], [tb_[3], tb_[7]], [tb_[5], tb_[4]]
        for n in range(5):
            An, Pn, Xn = Abuf[n % 2], Pbuf[n % 2], Xbuf[n % 2]
            pa_ = qa(kk)
            P.mm(pa_.t, Pm.t[:], Am.t[:], True, True, Pm.all() + Am.all(), pa_.all())
            if n < 4:
                pp_ = qa(kk)
                P.mm(pp_.t, Am.t[:], Pm.t[:], True, True, Pm.all() + Am.all(), pp_.all())
            yield
            P.copy("scalar", An.t[:], pa_.t, pa_.all(), An.all())
            pa_.busy = False
            if n < 4:
                P.copy("scalar", Pn.t[:], pp_.t, pp_.all(), Pn.all())
                pp_.busy = False
            px_ = qa(kk)
            P.mm(px_.t, An.t[:], X.t[:], True, True, An.all() + X.all(), px_.all())
            yield
            P.op("vector", _I("tensor_tensor", out=Xn.t[:], in0=px_.t, in1=X.t[:], op=ALU.add), px_.all() + X.all(), Xn.all())
            px_.busy = False
            Am, Pm, X = An, Pn, Xn
        TTm = X
        pw_, pu_ = qa(kk), qa(kk)
        P.mm(pw_.t, kbg.t[:], TTm.t[:], True, True, kbg.all() + TTm.all(), pw_.all())
        P.mm(pu_.t, TTm.t[:], vb.t[:], True, True, vb.all() + TTm.all(), pu_.all())
        yield
        P.copy("scalar", WT.t[:], pw_.t, pw_.all(), WT.all())
        P.copy("vector", U.t[:], pu_.t, pu_.all(), U.all())
        pw_.busy = False
        pu_.busy = False
        si = 2 * u
        Sa, Sb, Sn = S[si % 4], S[(si + 1) % 4], S[(si + 2) % 4]
        Sa6, Sb6, Sn6 = S16[si % 4], S16[(si + 1) % 4], S16[(si + 2) % 4]
        ps1 = qa(kk)
        P.mm(ps1.t, WT.t[:], Sa6.t[:], True, True, WT.all() + Sa6.all(), ps1.all())
        yield
        P.op("vector", _I("tensor_tensor", out=vnew.t[0:64, :], in0=U.t[0:64, :], in1=ps1.t[0:64, :], op=ALU.subtract), U.all() + ps1.all(), vnew.all())
        ps1.busy = False
        pss = qa(kk)
        P.mm(pss.t, kdec.t[0:64, :], vnew.t[0:64, :], True, True, kdec.all() + vnew.all(), pss.all())
        yield
        P.op("vector", _I("scalar_tensor_tensor", out=Sb.t[:], in0=Sa.t[:], scalar=eglb.t[:, h, 2 * u:2 * u + 1], in1=pss.t,
                          op0=ALU.mult, op1=ALU.add), Sa.all() + pss.all() + eglb.all(), Sb.all())
        pss.busy = False
        P.copy("scalar", Sb6.t[:], Sb.t[:], Sb.all(), Sb6.all())
        ps2 = qa(kk)
        P.mm(ps2.t, WT.t[:], Sb6.t[:], True, True, WT.all() + Sb6.all(), ps2.all())
        yield
        P.op("vector", _I("tensor_tensor", out=vnew.t[64:128, :], in0=U.t[64:128, :], in1=ps2.t[64:128, :], op=ALU.subtract), U.all() + ps2.all(), vnew.all())
        ps2.busy = False
        pss2 = qa(kk)
        P.mm(pss2.t, kdec.t[64:128, :], vnew.t[64:128, :], True, True, kdec.all() + vnew.all(), pss2.all())
        pso = qa(kk)
        P.mm(pso.t, qdA.t[:], Sa6.t[:], True, False, qdA.all() + Sa6.all(), pso.all())
        P.mm(pso.t, qdB.t[:], Sb6.t[:], False, False, qdB.all() + Sb6.all(), pso.all())
        P.mm(pso.t, attnT.t[:], vnew.t[:], False, True, attnT.all() + vnew.all(), pso.all())
        yield
        P.op("vector", _I("scalar_tensor_tensor", out=Sn.t[:], in0=Sb.t[:], scalar=eglb.t[:, h, 2 * u + 1:2 * u + 2], in1=pss2.t,
                          op0=ALU.mult, op1=ALU.add), Sb.all() + pss2.all() + eglb.all(), Sn.all())
        pss2.busy = False
        P.copy("scalar", Sn6.t[:], Sn.t[:], Sn.all(), Sn6.all())
        junk = t[0]
        P.op("scalar", _I("activation", out=junk.t[:], in_=pso.t, func=AF.Square, accum_out=ssu.t[:, 0:1]), pso.all(), junk.all() + ssu.all())
        _rstd(P, ssu.t[:, 0:1], ssu.all(), ssu, ssu.t[:, 1:2], 1.0 / 128)
        P.op("vector", _I("tensor_scalar", out=o_n.t[:], in0=pso.t, scalar1=ssu.t[:, 1:2], scalar2=None, op0=ALU.mult), pso.all() + ssu.all(), o_n.all())
        pso.busy = False
        psy = qa(kk)
        P.tr(psy.t, o_n.t[:], ident.t[:], o_n.all() + ident.all(), psy.all())
        yield
        P.op("vector", _I("scalar_tensor_tensor", out=yT.t[:, usl], in0=psy.t, scalar=nrm.t[:, i:i + 1], in1=zs.t[:, usl],
                          op0=ALU.mult, op1=ALU.mult), psy.all() + nrm.all() + zs.all(), yT.all())
        psy.busy = False

    STAG = 7
    for h0 in range(0, 6, NH):
        heads = list(range(h0, min(6, h0 + NH)))
        mg = c.mark()
        wq = [c.sb(f"dn_w{j}", [128, 8, 128], BF16) for j in range(4)]
        raw = c.sb("dn_raw", [128, T], F32)
        cac = c.sb("dn_cac", [128, T], F32)
        rin = c.sb("dn_rin", [128, T], F32)
        for hh, h in enumerate(heads):
            head_prep(h, HB[hh], raw, cac, rin, wq)
        c.release(mg)
        mt = c.mark()
        TL = [[[c.sb(f"dn_tl{hh}_{uu}_{j}", [128, 128], F32) for j in range(10)] for uu in range(NU)] for hh in range(NH)]
        TLB = [[[c.sb(f"dn_tlb{hh}_{uu}_{j}", [128, 128], BF16) for j in range(16)] for uu in range(NU)] for hh in range(NH)]
        SS = [[c.sb(f"dn_ss{hh}_{uu}", [128, 2], F32) for uu in range(NU)] for hh in range(NH)]
        for hh in range(NH):
            for uu in range(NU):
                P.op("gpsimd", _I("memset", TLB[hh][uu][0].t[:], 0.0), w=TLB[hh][uu][0].all())
                P.op("gpsimd", _I("memset", TLB[hh][uu][1].t[:], 0.0), w=TLB[hh][uu][1].all())
        nxt_u = [0] * len(heads)
        act = []
        last = [None] * len(heads)
        while True:
            for hh, h in enumerate(heads):
                infl = sum(1 for a_ in act if a_[1] == hh)
                if nxt_u[hh] < 16 and infl < NU and (last[hh] is None or last[hh][2] >= STAG or last[hh] not in act):
                    u = nxt_u[hh]
                    nxt_u[hh] += 1
                    _conv_some(P, 1)
                    ent = [unit_gen(h, HB[hh], u, TL[hh][u % NU], SS[hh][u % NU], hh * NU + (u % NU), TLB[hh][u % NU]), hh, 0]
                    act.append(ent)
                    last[hh] = ent
            if not act:
                break
            keep = []
            for ent in act:
                try:
                    next(ent[0])
                    ent[2] += 1
                    keep.append(ent)
                except StopIteration:
                    pass
            act = keep
        for hh, h in enumerate(heads):
            P.dma(P.ydn.t[:, h, :], HB[hh]["yT"].t[:], r=HB[hh]["yT"].all(), w=P.ydn.all())
        c.release(mt)
    c.release(m_phase)


DN_NU = 2
```

```python
import numpy as np
import ml_dtypes
import concourse.bass as bass
import concourse.mybir as mybir
from concourse.bass_utils import run_bass_kernel_spmd

F32 = mybir.dt.float32
BF16 = mybir.dt.bfloat16
AF = mybir.ActivationFunctionType
ALU = mybir.AluOpType
AX = mybir.AxisListType

T = 2048
D = 1024
NEG = -30000.0
SAME_ENGINE_SYNC = True
EPOCH_LIMIT = 30000
N_DMA_SEMS = 8
IN_W = 10520
C_DNQ, C_DNK, C_DNV, C_DNZ, C_DNA, C_DNB = 0, 768, 1536, 2304, 3072, 3078
C_SX, C_SB, C_SC, C_SZ, C_SDT = 3084, 3852, 4108, 4364, 5132
C_AQ, C_AK, C_AV, C_GATE = 5144, 5912, 6680, 7448
FF = 2816
EF = 3584
SB_BASE = 16640
SB_LIMIT = 212000


def _I(name, *a, **kw):
    return (name, a, kw)


class Buf:
    __slots__ = ("w", "r", "excl")

    def __init__(self):
        self.w = None
        self.r = {}
        self.excl = False


class TT:
    def __init__(self, t):
        self.t = t
        self.bufs = {}

    def b(self, key=0):
        bb = self.bufs.get(key)
        if bb is None:
            bb = self.bufs[key] = Buf()
        return bb

    def all(self):
        if not self.bufs:
            self.b(0)
        return list(self.bufs.values())


class Queue:
    def __init__(self, name):
        self.name = name
        self.items = []
        self.sig = []
        self.waited_c = {}
        self.waited_d = {}
        self.dma_sems = []
        self.dma_vals = []
        self.dma_rr = 0


class Ctx:
    def __init__(self, nc):
        self.nc = nc
        self.q = {n: Queue(n) for n in ("tensor", "vector", "scalar", "gpsimd", "sync")}
        self._cms = []
        self.nsem = 0
        self.sb_off = SB_BASE
        self.sb_peak = 0
        self.uid = 0
        self.ps_rr = 0
        self.ev_rr = 0

    def enter(self, cm):
        v = cm.__enter__()
        self._cms.append(cm)
        return v

    def new_sem(self, name):
        self.nsem += 1
        return self.enter(self.nc.semaphore(name))

    def close(self):
        for cm in reversed(self._cms):
            cm.__exit__(None, None, None)
        self._cms = []

    def sb(self, name, shape, dtype):
        esz = 2 if dtype == BF16 else 4
        n = 1
        for s in shape[1:]:
            n *= s
        nbytes = (n * esz + 63) // 64 * 64
        off = self.sb_off
        assert off + nbytes <= SB_LIMIT, (name, off, nbytes)
        self.sb_off = off + nbytes
        self.sb_peak = max(self.sb_peak, self.sb_off)
        self.uid += 1
        h = self.nc.alloc_sbuf_tensor_at(f"{name}_{self.uid}", list(shape), dtype, offset=off)
        return TT(h)

    def mark(self):
        return self.sb_off

    def release(self, mark):
        self.barrier()
        self.sb_off = mark

    def _wait_c(self, q, qn2, idx):
        if q.waited_c.get(qn2, -1) >= idx:
            return
        q.waited_c[qn2] = idx
        self.q[qn2].sig[idx] = True
        q.items.append(("waitc", qn2, idx))

    def _wait_d(self, q, sem, val):
        if q.waited_d.get(sem, 0) >= val:
            return
        q.waited_d[sem] = val
        q.items.append(("waitd", sem, val))

    def barrier(self):
        for q in self.q.values():
            for q2 in self.q.values():
                if q2.sig and not (q2.name == "tensor" and q.name == "tensor"):
                    self._wait_c(q, q2.name, len(q2.sig) - 1)
                for s, v in zip(q2.dma_sems, q2.dma_vals):
                    if v:
                        self._wait_d(q, s, v)

    def _dep(self, q, qn, ev, dma):
        if ev[0] == "c":
            _, qn2, idx = ev
            if qn2 == qn and not dma and (qn == "tensor" or not SAME_ENGINE_SYNC):
                return
            self._wait_c(q, qn2, idx)
        else:
            self._wait_d(q, ev[1], ev[2])

    def emit(self, qn, fn, reads=(), writes=(), dma=False):
        q = self.q[qn]
        for b in reads:
            if b.w is not None:
                self._dep(q, qn, b.w, dma)
            if b.excl:
                for ev in b.r.values():
                    if not (ev[0] == "c" and ev[1] == qn):
                        self._dep(q, qn, ev, dma)
        for b in writes:
            if b.w is not None:
                self._dep(q, qn, b.w, dma)
            for ev in b.r.values():
                self._dep(q, qn, ev, dma)
        if dma:
            if not q.dma_sems:
                for i in range(N_DMA_SEMS):
                    q.dma_sems.append(self.new_sem(f"dma_{qn}_{i}"))
                    q.dma_vals.append(0)
            i = q.dma_rr
            q.dma_rr = (i + 1) % len(q.dma_sems)
            sem = q.dma_sems[i]
            if q.dma_vals[i] > 0:
                self._wait_d(q, sem, q.dma_vals[i])
            q.dma_vals[i] += 16
            ev = ("d", sem, q.dma_vals[i])
            key = ("d", id(sem))
            q.items.append(("dma", fn, sem))
        else:
            idx = len(q.sig)
            q.sig.append(False)
            ev = ("c", qn, idx)
            key = ("c", qn)
            q.items.append(("op", fn, idx))
        for b in writes:
            b.w = ev
            b.r = {}
        for b in reads:
            if b.w is ev:
                continue
            b.r[key] = ev
        return ev

    def finish(self):
        self.barrier()
        nc = self.nc
        res = {}
        self.nsig = {}
        for qn, q in self.q.items():
            sem, cnt = None, 0
            tab = [None] * len(q.sig)
            pending = []
            ns = 0
            for idx, sg in enumerate(q.sig):
                pending.append(idx)
                if sg:
                    if sem is None or cnt >= EPOCH_LIMIT:
                        sem = self.new_sem(f"c_{qn}_{self.nsem}")
                        cnt = 0
                    cnt += 1
                    ns += 1
                    for j in pending:
                        tab[j] = (sem, cnt)
                    pending = []
            res[qn] = tab
            self.nsig[qn] = ns
        with nc.Block() as block:
            def run(q):
                def body(eng):
                    for it in q.items:
                        if it[0] == "waitc":
                            sem, val = res[it[1]][it[2]]
                            eng.wait_ge(sem, val)
                        elif it[0] == "waitd":
                            eng.wait_ge(it[1], it[2])
                        elif it[0] == "dma":
                            nm, a, kw = it[1]
                            getattr(eng, nm)(*a, **kw).then_inc(it[2], 16)
                        else:
                            nm, a, kw = it[1]
                            ins = getattr(eng, nm)(*a, **kw)
                            if q.sig[it[2]]:
                                sem, val = res[q.name][it[2]]
                                ins.then_inc(sem, 1)
                return body
            block.tensor(run(self.q["tensor"]))
            block.vector(run(self.q["vector"]))
            block.scalar(run(self.q["scalar"]))
            block.gpsimd(run(self.q["gpsimd"]))
            block.sync(run(self.q["sync"]))
        self.close()


class Prog:
    def __init__(self, n_seq, dbg=None, stop_after=None, layers=(0, 1), skip_w=None):
        self.n_seq = n_seq
        self.skip_w = skip_w
        self.stop = 0
        self.pn_eng = 'vector'
        self.dbg = dbg or set()
        self.stop_after = stop_after
        self.layers = layers
        self.nc = bass.Bass("TRN2", target_bir_lowering=False)
        self.c = Ctx(self.nc)
        self.dram_in = {}
        self.dbg_out = {}

    def din(self, name, shape, dtype=F32):
        t = self.nc.dram_tensor(name, list(shape), dtype, kind="ExternalInput").ap()
        self.dram_in[name] = t
        return t

    def dscr(self, name, shape, dtype):
        return TT(self.nc.dram_tensor(name, list(shape), dtype, kind="Internal").ap())

    def dump(self, name, tt, ap, shape, dtype=F32):
        if name not in self.dbg:
            return
        o = self.nc.dram_tensor("dbg_" + name, list(shape), dtype, kind="ExternalOutput").ap()
        self.dbg_out[name] = o
        self.c.emit("sync", _I("dma_start", out=o, in_=ap), reads=tt.all(), dma=True)

    def op(self, eng, fn, r=(), w=()):
        return self.c.emit(eng, fn, r, w)

    def dma(self, out, in_, r=(), w=(), eng="sync"):
        return self.c.emit(eng, _I("dma_start", out=out, in_=in_), r, w, dma=True)

    def ps(self):
        i = self.c.ps_rr
        self.c.ps_rr = (i + 1) % 8
        return self.PS[i]

    def ev_eng(self):
        self.c.ev_rr ^= 1
        return "scalar" if self.c.ev_rr else "vector"

    def copy(self, eng, out, in_, r, w):
        if eng == "scalar":
            return self.op("scalar", _I("copy", out=out, in_=in_), r, w)
        return self.op(eng, _I("tensor_copy", out=out, in_=in_), r, w)

    def mm(self, out, lhsT, rhs, start, stop, r, w):
        return self.op("tensor", _I("matmul", out, lhsT=lhsT, rhs=rhs, start=start, stop=stop), r, w)

    def tr(self, out, in_, ident, r, w):
        return self.op("tensor", _I("transpose", out=out, in_=in_, identity=ident), r, w)


def _setup(P):
    c, nc, ns = P.c, P.nc, P.n_seq
    P.xT = P.din("xT", [ns, 128, 8, T])
    P.pT = P.din("pT", [2, ns, 128, 2, T])
    P.outT = nc.dram_tensor("outT", [ns, 128, 8, T], F32, kind="ExternalOutput").ap()
    big = {
        "w_in": [2, 128, 8 * IN_W], "w_br_dn": [2, 128, 6 * D], "w_br_ssm": [2, 128, 6 * D],
        "w_br_attn": [2, 64, 4 * D], "w_out": [2, 128, 8 * D], "w_ple_gate": [2, 128, 8 * D],
        "w_ple": [2, 128, 2 * D], "w_ff_gate": [1, 128, 8 * FF], "w_ff_up": [1, 128, 8 * FF],
        "w_ff_down": [1, 128, 22 * D], "w_moe_gate": [8, 128, 8 * EF], "w_moe_up": [8, 128, 8 * EF],
        "w_moe_down": [8, 128, 28 * D],
    }
    P.wsrc = {k: P.din(k, v) for k, v in big.items()}
    P.wb = {k: P.dscr(k + "_b", v, BF16) for k, v in big.items()}
    P.w_router = P.din("w_router", [128, 8 * 8])
    small = {"gains": [128, 56], "dnconv": [128, 144], "dnp6": [6, 4], "dnnorm": [128, 2],
             "ssmconv": [128, 80], "ssmconvb": [128, 20], "ssmp12": [12, 4], "ssmd": [128, 24],
             "ssmnorm": [128, 12], "ident": [128, 128], "masks": [128, 384], "sel6": [6, 768],
             "sel12": [12, 1536], "sel8": [8, 1024]}
    P.abias_d = P.din("abias", [128, 12 * 256])
    P.PS = []
    for i in range(8):
        t = TT(c.enter(nc.psum_tensor(f"psb{i}", [128, 512], F32)))
        t.busy = False
        t.b(0).excl = True
        P.PS.append(t)
    P.k = {}
    for name, shp in small.items():
        src = P.din(name, shp)
        tt = c.sb("k_" + name, shp, F32)
        P.dma(tt.t[:], src, w=tt.all())
        P.k[name] = tt
    tt = c.sb("k_router", [128, 64], F32)
    P.dma(tt.t[:], P.w_router, w=tt.all())
    P.k["router"] = tt
    P.ones_b = c.sb("ones_b", [128, 128], BF16)
    P.op("gpsimd", _I("memset", P.ones_b.t[:], 1.0), w=P.ones_b.all())
    P.ones_f = c.sb("ones_f", [128, 128], F32)
    P.op("gpsimd", _I("memset", P.ones_f.t[:], 1.0), w=P.ones_f.all())
    P.xscr = P.dscr("xscr", [ns, 128, 8, T], F32)
    P.ydn = P.dscr("ydn_s", [128, 6, T], BF16)
    P.yssm = P.dscr("yssm_s", [128, 6, T], BF16)
    P.yattn = P.dscr("yattn_s", [64, 4, T], BF16)
    P.conv_pending = []
    first = [n for n in big if not n.startswith("w_moe")]
    later = [n for n in big if n.startswith("w_moe")]
    for name in first + later:
        if P.skip_w and name in P.skip_w:
            continue
        shp = big[name]
        for j in range(shp[0]):
            src = P.wsrc[name][j]
            dst = P.wb[name].t[j]
            N = shp[2]
            off = 0
            ci = -1
            while off < N:
                ci += 1
                w = min(8192, N - off)
                w -= w % 2048
                if w == 0:
                    w = N - off
                    item = (dst[:, off:off + w], src[:, off:off + w], P.wb[name].b((j, ci)))
                else:
                    item = (dst[:, off:off + w].rearrange("p (a b) -> p a b", b=2048), src[:, off:off + w].rearrange("p (a b) -> p a b", b=2048), P.wb[name].b((j, ci)))
                off += w
                if name in later:
                    P.conv_pending.append(item)
                else:
                    P.c.emit("gpsimd", _I("dma_start", out=item[0], in_=item[1]), writes=[item[2]], dma=True)


def _wbufs(P, name, j):
    return [b for key, b in P.wb[name].bufs.items() if isinstance(key, tuple) and key[0] == j]


def _conv_some(P, n):
    for _ in range(n):
        if not P.conv_pending:
            return
        o, i_, b = P.conv_pending.pop(0)
        P.c.emit("gpsimd", _I("dma_start", out=o, in_=i_), writes=[b], dma=True)


def _ps(P):
    for _ in range(8):
        i = P.c.ps_rr
        P.c.ps_rr = (i + 1) % 8
        if not P.PS[i].busy:
            P.PS[i].busy = True
            return P.PS[i]
    raise RuntimeError("no free PSUM bank")


def _rstd(P, ps_ap, ps_bufs, out_tt, out_ap, scale, extra_bias=0.0):
    P.op("vector", _I("tensor_scalar", out=out_ap, in0=ps_ap, scalar1=scale, scalar2=1e-6, op0=ALU.mult, op1=ALU.add),
         ps_bufs, out_tt.all())
    P.op("scalar", _I("activation", out=out_ap, in_=out_ap, func=AF.Ln), out_tt.all(), out_tt.all())
    if extra_bias != 0.0:
        P.op("scalar", _I("activation", out=out_ap, in_=out_ap, func=AF.Exp, scale=-0.5, bias=float(extra_bias)), out_tt.all(), out_tt.all())
    else:
        P.op("scalar", _I("activation", out=out_ap, in_=out_ap, func=AF.Exp, scale=-0.5), out_tt.all(), out_tt.all())


def _norm_phase(P, s, i, src_ap, src_bufs, gcol, hT):
    c = P.c
    m = c.mark()
    xt = [c.sb(f"np_x{j}", [128, 8, 512], F32) for j in range(2)]
    sq = [c.sb(f"np_sq{j}", [128, 8, 512], BF16) for j in range(2)]
    rs = [c.sb(f"np_rs{j}", [128, 512], F32) for j in range(2)]
    for nt in range(4):
        a, q, r = xt[nt % 2], sq[nt % 2], rs[nt % 2]
        sl = slice(nt * 512, (nt + 1) * 512)
        P.dma(a.t[:], src_ap[:, :, sl], r=src_bufs, w=a.all())
        _rms_tile(P, a, a.t, q, r, gcol, hT.t[:, :, sl], [hT.b(nt)])
    c.release(m)


def _rms_tile(P, x_tt, x_ap, sq, rs, gcol, out_ap, out_bufs, xbufs=None):
    xb = xbufs if xbufs is not None else x_tt.all()
    P.op("scalar", _I("activation", out=sq.t[:], in_=x_ap[:], func=AF.Square), xb, sq.all())
    ps = _ps(P)
    for k in range(8):
        P.mm(ps.t[:], P.ones_b.t[:], sq.t[:, k, :], k == 0, k == 7, P.ones_b.all() + sq.all(), ps.all())
    _rstd(P, ps.t[:], ps.all(), rs, rs.t[:], 1.0 / D)
    ps.busy = False
    g = P.k["gains"]
    for k in range(8):
        P.op("vector", _I("scalar_tensor_tensor", out=out_ap[:, k, :], in0=x_ap[:, k, :], scalar=g.t[:, gcol + k:gcol + k + 1],
                                                         in1=rs.t[:], op0=ALU.mult, op1=ALU.mult),
             xb + rs.all() + g.all(), out_bufs)


def _load_wcols(P, dst, i, col0, ncols):
    src = P.wb["w_in"].t[i].rearrange("p (k n) -> p k n", k=8)[:, :, col0:col0 + ncols]
    P.dma(dst.t[:, :, 0:ncols], src, r=_wbufs(P, "w_in", i), w=dst.all())


def _proj_fm(P, hT, w, wcol0, M, out_fn):
    for nt in range(4):
        ps = _ps(P)
        sl = slice(nt * 512, (nt + 1) * 512)
        for k in range(8):
            P.mm(ps.t[0:M, :], w.t[:, k, wcol0:wcol0 + M], hT.t[:, k, sl], k == 0, k == 7, w.all() + [hT.b(nt)], ps.all())
        out_fn(nt, ps)
        ps.busy = False


def _cumsum64(P, a, b, npart):
    src, dst = a, b
    s = 1
    while s < 64:
        sv = src.t[0:npart, :].rearrange("p (c t) -> p c t", t=64)
        dv = dst.t[0:npart, :].rearrange("p (c t) -> p c t", t=64)
        P.op("vector", _I("tensor_tensor", out=dv[:, :, s:], in0=sv[:, :, s:], in1=sv[:, :, :64 - s], op=ALU.add),
             src.all(), dst.all())
        P.op("gpsimd", _I("tensor_copy", out=dv[:, :, 0:s], in_=sv[:, :, 0:s]), src.all(), dst.all())
        src, dst = dst, src
        s *= 2
    return src


def _deltanet(P, s, i, hT):
    c, k = P.c, P.k
    ident, masks, sel6 = k["ident"], k["masks"], k["sel6"]
    mDA, mDP, mDQ = masks.t[:, 0:128], masks.t[:, 128:256], masks.t[:, 256:384]
    m_phase = c.mark()
    gamT = c.sb("dn_gamT", [6, T], F32)
    gbT = c.sb("dn_gbT", [6, T], F32)
    tok = c.sb("dn_tok", [128, 16, 128], F32)
    bgtok = c.sb("dn_bgtok", [128, 16, 8], F32)
    eglb = c.sb("dn_eglb", [128, 6, 32], F32)
    negA = c.sb("dn_negA", [6, 1], F32)
    m1 = c.mark()
    wab = c.sb("dn_wab", [128, 8, 12], BF16)
    _load_wcols(P, wab, i, C_DNA, 12)
    t0_ = c.sb("dn_t0", [6, T], F32)
    t1_ = c.sb("dn_t1", [6, T], F32)
    lnb = c.sb("dn_lnb", [6, T], F32)
    stk = c.sb("dn_stk", [128, T], F32)
    P.op("gpsimd", _I("memset", stk.t[:], 0.0), w=stk.all())
    p6 = k["dnp6"]
    P.op("scalar", _I("activation", out=negA.t[:], in_=p6.t[:, 2 * i:2 * i + 1], func=AF.Exp), p6.all(), negA.all())
    P.op("vector", _I("tensor_scalar", out=negA.t[:], in0=negA.t[:], scalar1=-1.0, scalar2=None, op0=ALU.mult), negA.all(), negA.all())

    def ev_a(nt, ps):
        sl = slice(nt * 512, (nt + 1) * 512)
        P.op("scalar", _I("activation", out=t0_.t[:, sl], in_=ps.t[0:6, :], func=AF.Exp, bias=p6.t[:, 2 * i + 1:2 * i + 2]),
             ps.all() + p6.all(), t0_.all())
    _proj_fm(P, hT, wab, 0, 6, ev_a)
    P.op("scalar", _I("activation", out=t0_.t[:], in_=t0_.t[:], func=AF.Ln, bias=1.0), t0_.all(), t0_.all())
    P.op("vector", _I("tensor_scalar", out=t0_.t[:], in0=t0_.t[:], scalar1=negA.t[:, 0:1], scalar2=None, op0=ALU.mult),
         t0_.all() + negA.all(), t0_.all())
    gres = _cumsum64(P, t0_, t1_, 6)
    P.copy("vector", gamT.t[:], gres.t[:], gres.all(), gamT.all())

    def ev_b(nt, ps):
        sl = slice(nt * 512, (nt + 1) * 512)
        P.op("scalar", _I("activation", out=lnb.t[:, sl], in_=ps.t[0:6, :], func=AF.Exp, scale=-1.0), ps.all(), lnb.all())
    _proj_fm(P, hT, wab, 6, 6, ev_b)
    P.op("scalar", _I("activation", out=lnb.t[:], in_=lnb.t[:], func=AF.Ln, bias=1.0), lnb.all(), lnb.all())
    P.op("vector", _I("tensor_scalar", out=lnb.t[:], in0=lnb.t[:], scalar1=-1.0, scalar2=None, op0=ALU.mult), lnb.all(), lnb.all())
    P.op("vector", _I("tensor_tensor", out=gbT.t[:], in0=gamT.t[:], in1=lnb.t[:], op=ALU.add), gamT.all() + lnb.all(), gbT.all())
    P.op("vector", _I("tensor_scalar", out=stk.t[0:6, :], in0=gamT.t[:], scalar1=-1.0, scalar2=None, op0=ALU.mult), gamT.all(), stk.all())
    P.copy("vector", stk.t[32:38, :], gbT.t[:], gbT.all(), stk.all())
    g3 = gamT.t[:].rearrange("p (c t) -> p c t", t=64)
    P.op("vector", _I("tensor_tensor", out=t0_.t[:].rearrange("p (c t) -> p c t", t=64), in0=g3[:, :, 63:64].to_broadcast([6, 32, 64]),
                                             in1=g3, op=ALU.subtract), gamT.all(), t0_.all())
    P.op("scalar", _I("activation", out=stk.t[64:70, :], in_=t0_.t[:], func=AF.Exp), t0_.all(), stk.all())
    P.op("scalar", _I("activation", out=stk.t[96:102, :], in_=lnb.t[:], func=AF.Exp), lnb.all(), stk.all())
    for u4 in range(4):
        ps = _ps(P)
        for j in range(4):
            u = u4 * 4 + j
            P.tr(ps.t[:, j * 128:(j + 1) * 128], stk.t[:, u * 128:(u + 1) * 128], ident.t[:], stk.all() + ident.all(), ps.all())
        P.copy("vector", tok.t[:, u4 * 4:(u4 + 1) * 4, :].rearrange("p a b -> p (a b)"), ps.t[:], ps.all(), tok.all())
        ps.busy = False
    P.op("scalar", _I("activation", out=bgtok.t[:, :, 0:6], in_=tok.t[:, :, 32:38], func=AF.Exp), tok.all(), bgtok.all())
    P.op("scalar", _I("activation", out=t1_.t[:, 0:32], in_=g3[:, :, 63], func=AF.Exp), gamT.all(), t1_.all())
    ps = _ps(P)
    for h in range(6):
        P.mm(ps.t[:, h * 32:(h + 1) * 32], sel6.t[:, h * 128:(h + 1) * 128], t1_.t[:, 0:32], True, True, sel6.all() + t1_.all(), ps.all())
    P.copy("vector", eglb.t[:].rearrange("p a b -> p (a b)"), ps.t[:, 0:192], ps.all(), eglb.all())
    ps.busy = False
    c.release(m1)
    if P.stop == 1:
        c.release(m_phase); return
    wq = [c.sb(f"dn_w{j}", [128, 8, 128], BF16) for j in range(4)]
    raw = c.sb("dn_raw", [128, T], F32)
    cq = c.sb("dn_cq", [128, T], F32)
    ck = c.sb("dn_ck", [128, T], F32)
    cv = c.sb("dn_cv", [128, T], F32)
    zs = c.sb("dn_zs", [128, T], F32)
    yT = c.sb("dn_yT", [128, T], BF16)
    S = [c.sb(f"dn_S{j}", [128, 128], F32) for j in range(3)]
    NT = 30
    tmp = [[c.sb(f"dn_tmp{p}_{j}", [128, 128], F32) for j in range(NT)] for p in range(2)]
    ss = [c.sb(f"dn_ss{p}", [128, 2], F32) for p in range(2)]
    for p in range(2):
        P.op("gpsimd", _I("memset", tmp[p][0].t[:], 0.0), w=tmp[p][0].all())
        P.op("gpsimd", _I("memset", tmp[p][1].t[:], 0.0), w=tmp[p][1].all())
    cw = k["dnconv"]
    for h in range(6):
        cols = [C_DNQ + h * 128, C_DNK + h * 128, C_DNV + h * 128, C_DNZ + h * 128]
        for j in range(4):
            _load_wcols(P, wq[j], i, cols[j], 128)
        for j, dst in enumerate((cq, ck, cv)):
            def ev_raw(nt, ps):
                sl = slice(nt * 512, (nt + 1) * 512)
                P.copy(P.ev_eng(), raw.t[:, sl], ps.t[:], ps.all(), raw.all())
            _proj_fm(P, hT, wq[j], 0, 128, ev_raw)
            cb = (i * 18 + j * 6 + h) * 4
            P.op("vector", _I("tensor_scalar", out=dst.t[:], in0=raw.t[:], scalar1=cw.t[:, cb + 3:cb + 4], scalar2=None, op0=ALU.mult),
                 raw.all() + cw.all(), dst.all())
            for sft in (1, 2, 3):
                eng = "vector" if sft != 2 else "gpsimd"
                P.op("vector", _I("scalar_tensor_tensor",
                    out=dst.t[:, sft:], in0=raw.t[:, :T - sft], scalar=cw.t[:, cb + 3 - sft:cb + 4 - sft], in1=dst.t[:, sft:], op0=ALU.mult, op1=ALU.add),
                    raw.all() + cw.all() + dst.all(), dst.all())
            P.op("scalar", _I("activation", out=dst.t[:], in_=dst.t[:], func=AF.Silu), dst.all(), dst.all())
            if j < 2:
                P.op("scalar", _I("activation", out=raw.t[:], in_=dst.t[:], func=AF.Square), dst.all(), raw.all())
                for nt in range(4):
                    sl = slice(nt * 512, (nt + 1) * 512)
                    ps = _ps(P)
                    P.mm(ps.t[:], P.ones_f.t[:], raw.t[:, sl], True, True, P.ones_f.all() + raw.all(), ps.all())
                    rr = tmp[0][2 + nt]
                    _rstd(P, ps.t[:], ps.all(), zs, zs.t[:, sl], 1.0, extra_bias=(-0.5 * np.log(128.0) if j == 0 else 0.0))
                    ps.busy = False
                P.op("vector", _I("tensor_tensor", out=dst.t[:], in0=dst.t[:], in1=zs.t[:], op=ALU.mult), dst.all() + zs.all(), dst.all())

        def ev_z(nt, ps):
            sl = slice(nt * 512, (nt + 1) * 512)
            P.op("scalar", _I("activation", out=zs.t[:, sl], in_=ps.t[:], func=AF.Silu), ps.all(), zs.all())
        _proj_fm(P, hT, wq[3], 0, 128, ev_z)
        if P.stop == 2:
            c.release(m_phase); return
        Sc = S[0]
        P.op("gpsimd", _I("memset", Sc.t[:], 0.0), w=Sc.all())
        si = 0
        for u in range(16):
            tp = tmp[u % 2]
            qdA, qdB = tp[0], tp[1]
            (tA, tB, tC, DA, DP, DQ, Eb, kbg, kdec, vb, A0, P0, attnT, X0, X1, A1, A2, P1, P2, TTt, WT, U, vnew, o_n) = tp[2:26]
            ssu = ss[u % 2]
            t0 = u * 128
            usl = slice(t0, t0 + 128)
            ngam = tok.t[:, u, h:h + 1]
            gbt = tok.t[:, u, 32 + h:33 + h]
            kd = tok.t[:, u, 64 + h:65 + h]
            beta = tok.t[:, u, 96 + h:97 + h]
            bg = bgtok.t[:, u, h:h + 1]
            selh = sel6.t[:, h * 128:(h + 1) * 128]
            psb = _ps(P)
            P.mm(psb.t[:, 0:128], selh, gamT.t[:, usl], True, True, sel6.all() + gamT.all(), psb.all())
            P.mm(psb.t[:, 128:256], selh, gbT.t[:, usl], True, True, sel6.all() + gbT.all(), psb.all())
            P.op("vector", _I("scalar_tensor_tensor", out=tA.t[:], in0=psb.t[:, 0:128], scalar=-1.0, in1=mDA, op0=ALU.mult, op1=ALU.add),
                 psb.all() + masks.all(), tA.all())
            P.op("scalar", _I("activation", out=DA.t[:], in_=tA.t[:], func=AF.Exp, bias=gbt), tA.all() + tok.all(), DA.all())
            P.op("vector", _I("tensor_tensor", out=tB.t[:], in0=psb.t[:, 128:256], in1=mDP, op=ALU.add), psb.all() + masks.all(), tB.all())
            P.op("scalar", _I("activation", out=DP.t[:], in_=tB.t[:], func=AF.Exp, bias=ngam), tB.all() + tok.all(), DP.all())
            P.op("vector", _I("tensor_tensor", out=tC.t[:], in0=psb.t[:, 0:128], in1=mDQ, op=ALU.add), psb.all() + masks.all(), tC.all())
            P.op("scalar", _I("activation", out=DQ.t[:], in_=tC.t[:], func=AF.Exp, bias=ngam), tC.all() + tok.all(), DQ.all())
            P.op("scalar", _I("activation", out=Eb.t[:], in_=psb.t[:, 0:128], func=AF.Exp), psb.all(), Eb.all())
            psb.busy = False
            P.op("gpsimd", _I("tensor_tensor", out=qdA.t[:, 0:64], in0=cq.t[:, t0:t0 + 64], in1=Eb.t[:, 0:64], op=ALU.mult),
                 cq.all() + Eb.all(), qdA.all())
            P.op("gpsimd", _I("tensor_tensor", out=qdB.t[:, 64:128], in0=cq.t[:, t0 + 64:t0 + 128], in1=Eb.t[:, 64:128], op=ALU.mult),
                 cq.all() + Eb.all(), qdB.all())
            if P.stop == 3:
                c.release(m_phase); return
            pst = _ps(P)
            P.tr(pst.t[:, 0:128], ck.t[:, usl], ident.t[:], ck.all() + ident.all(), pst.all())
            P.tr(pst.t[:, 128:256], cv.t[:, usl], ident.t[:], cv.all() + ident.all(), pst.all())
            P.op("vector", _I("tensor_scalar", out=kbg.t[:], in0=pst.t[:, 0:128], scalar1=bg, scalar2=None, op0=ALU.mult),
                 pst.all() + bgtok.all(), kbg.all())
            P.op("scalar", _I("activation", out=kdec.t[:], in_=pst.t[:, 0:128], func=AF.Copy, scale=kd),
                 pst.all() + tok.all(), kdec.all())
            P.op("vector", _I("tensor_scalar", out=vb.t[:], in0=pst.t[:, 128:256], scalar1=beta, scalar2=None, op0=ALU.mult),
                 pst.all() + tok.all(), vb.all())
            pst.busy = False
            psk = _ps(P)
            P.mm(psk.t[:, 0:128], ck.t[:, usl], ck.t[:, usl], True, True, ck.all(), psk.all())
            P.mm(psk.t[:, 128:256], ck.t[:, usl], cq.t[:, usl], True, True, ck.all() + cq.all(), psk.all())
            P.op("vector", _I("tensor_tensor", out=A0.t[:], in0=psk.t[:, 0:128], in1=DA.t[:], op=ALU.mult), psk.all() + DA.all(), A0.all())
            P.op("vector", _I("tensor_tensor", out=P0.t[:], in0=psk.t[:, 0:128], in1=DP.t[:], op=ALU.mult), psk.all() + DP.all(), P0.all())
            P.op("vector", _I("tensor_tensor", out=attnT.t[:], in0=psk.t[:, 128:256], in1=DQ.t[:], op=ALU.mult),
                 psk.all() + DQ.all(), attnT.all())
            psk.busy = False
            P.op("gpsimd", _I("tensor_tensor", out=X0.t[:], in0=ident.t[:], in1=P0.t[:], op=ALU.subtract), ident.all() + P0.all(), X0.all())
            if P.stop == 4:
                c.release(m_phase); return
            Am, Pm, X = A0, P0, X0
            Abuf, Pbuf, Xbuf = [A1, A2], [P1, P2], [X1, X0]
            for n in range(5):
                An, Pn, Xn = Abuf[n % 2], Pbuf[n % 2], Xbuf[n % 2]
                psn = _ps(P)
                P.mm(psn.t[:, 0:128], Pm.t[:], Am.t[:], True, True, Pm.all() + Am.all(), psn.all())
                if P.stop == 44:
                    c.release(m_phase); return
                if n < 4:
                    P.mm(psn.t[:, 128:256], Am.t[:], Pm.t[:], True, True, Pm.all() + Am.all(), psn.all())
                if P.stop == 45:
                    c.release(m_phase); return
                P.copy("scalar", An.t[:], psn.t[:, 0:128], psn.all(), An.all())
                if P.stop == 46:
                    c.release(m_phase); return
                if n < 4:
                    P.copy('vector', Pn.t[:], psn.t[:, 128:256], psn.all(), Pn.all())
                psn.busy = False
                if P.stop == 41:
                    P.dump("A0", A0, A0.t[:], [128, 128]); P.dump("P0", P0, P0.t[:], [128, 128])
                    P.dump("A1", An, An.t[:], [128, 128]); P.dump("P1", Pn, Pn.t[:], [128, 128])
                    P.dump("DA", DA, DA.t[:], [128, 128]); P.dump("DP", DP, DP.t[:], [128, 128])
                    c.release(m_phase); return
                psx = _ps(P)
                P.mm(psx.t[:, 0:128], An.t[:], X.t[:], True, True, An.all() + X.all(), psx.all())
                P.op("vector", _I("tensor_tensor", out=Xn.t[:], in0=psx.t[:, 0:128], in1=X.t[:], op=ALU.add), psx.all() + X.all(), Xn.all())
                psx.busy = False
                if P.stop == 42:
                    c.release(m_phase); return
                if P.stop == 43 and n == 1:
                    c.release(m_phase); return
                Am, Pm, X = An, Pn, Xn
            TTm = X
            psw = _ps(P)
            P.mm(psw.t[:, 0:128], kbg.t[:], TTm.t[:], True, True, kbg.all() + TTm.all(), psw.all())
            P.mm(psw.t[:, 128:256], TTm.t[:], vb.t[:], True, True, vb.all() + TTm.all(), psw.all())
            P.copy("scalar", WT.t[:], psw.t[:, 0:128], psw.all(), WT.all())
            P.copy("vector", U.t[:], psw.t[:, 128:256], psw.all(), U.all())
            psw.busy = False
            if P.stop == 5:
                c.release(m_phase); return
            Sa, Sb, Sn = S[si % 3], S[(si + 1) % 3], S[(si + 2) % 3]
            si += 2
            ps1 = _ps(P)
            P.mm(ps1.t[:, 0:128], WT.t[:], Sa.t[:], True, True, WT.all() + Sa.all(), ps1.all())
            P.op("vector", _I("tensor_tensor", out=vnew.t[0:64, :], in0=U.t[0:64, :], in1=ps1.t[0:64, 0:128], op=ALU.subtract),
                 U.all() + ps1.all(), vnew.all())
            ps1.busy = False
            pso = _ps(P)
            P.mm(pso.t[:, 0:128], qdA.t[:], Sa.t[:], True, False, qdA.all() + Sa.all(), pso.all())
            pss = _ps(P)
            P.mm(pss.t[:, 0:128], kdec.t[0:64, :], vnew.t[0:64, :], True, True, kdec.all() + vnew.all(), pss.all())
            P.op("vector", _I("scalar_tensor_tensor", out=Sb.t[:], in0=Sa.t[:], scalar=eglb.t[:, h, 2 * u:2 * u + 1], in1=pss.t[:, 0:128],
                                                                                      op0=ALU.mult, op1=ALU.add), Sa.all() + pss.all() + eglb.all(), Sb.all())
            pss.busy = False
            ps2 = _ps(P)
            P.mm(ps2.t[:, 0:128], WT.t[:], Sb.t[:], True, True, WT.all() + Sb.all(), ps2.all())
            P.op("vector", _I("tensor_tensor", out=vnew.t[64:128, :], in0=U.t[64:128, :], in1=ps2.t[64:128, 0:128], op=ALU.subtract),
                 U.all() + ps2.all(), vnew.all())
            ps2.busy = False
            P.mm(pso.t[:, 0:128], qdB.t[:], Sb.t[:], False, False, qdB.all() + Sb.all(), pso.all())
            P.mm(pso.t[:, 0:128], attnT.t[:], vnew.t[:], False, True, attnT.all() + vnew.all(), pso.all())
            pss2 = _ps(P)
            P.mm(pss2.t[:, 0:128], kdec.t[64:128, :], vnew.t[64:128, :], True, True, kdec.all() + vnew.all(), pss2.all())
            P.op("vector", _I("scalar_tensor_tensor", out=Sn.t[:], in0=Sb.t[:], scalar=eglb.t[:, h, 2 * u + 1:2 * u + 2], in1=pss2.t[:, 0:128],
                                                                                       op0=ALU.mult, op1=ALU.add), Sb.all() + pss2.all() + eglb.all(), Sn.all())
            pss2.busy = False
            if P.stop == 6:
                c.release(m_phase); return
            P.op("scalar", _I("activation", out=tA.t[:], in_=pso.t[:, 0:128], func=AF.Square, accum_out=ssu.t[:, 0:1]),
                 pso.all(), tA.all() + ssu.all())
            _rstd(P, ssu.t[:, 0:1], ssu.all(), ssu, ssu.t[:, 1:2], 1.0 / 128)
            P.op("vector", _I("tensor_scalar", out=o_n.t[:], in0=pso.t[:, 0:128], scalar1=ssu.t[:, 1:2], scalar2=None, op0=ALU.mult),
                 pso.all() + ssu.all(), o_n.all())
            pso.busy = False
            psy = _ps(P)
            P.tr(psy.t[:, 0:128], o_n.t[:], ident.t[:], o_n.all() + ident.all(), psy.all())
            nrm = k["dnnorm"]
            P.op("vector", _I("scalar_tensor_tensor", out=yT.t[:, usl], in0=psy.t[:, 0:128], scalar=nrm.t[:, i:i + 1], in1=zs.t[:, usl],
                                                                              op0=ALU.mult, op1=ALU.mult), psy.all() + nrm.all() + zs.all(), yT.all())
            psy.busy = False
        P.dma(P.ydn.t[:, h, :], yT.t[:], r=yT.all(), w=P.ydn.all())
    c.release(m_phase)


def _kmajor(w, kc=128):
    K, N = w.shape
    return np.ascontiguousarray(w.reshape(K // kc, kc, N).transpose(1, 0, 2)).reshape(kc, (K // kc) * N)


def _host_weights(inp):
    f = lambda a: np.ascontiguousarray(a, dtype=np.float32)
    o = {}
    o["w_in"] = np.stack([_kmajor(f(inp["w_in"][i])) for i in range(2)])
    o["w_br_dn"] = np.stack([_kmajor(f(inp["w_br_dn"][i])) for i in range(2)])
    o["w_br_ssm"] = np.stack([_kmajor(f(inp["w_br_ssm"][i])) for i in range(2)])
    o["w_br_attn"] = np.stack([_kmajor(f(inp["w_br_attn"][i]), 64) for i in range(2)])
    o["w_out"] = np.stack([_kmajor(f(inp["w_out"][i])) for i in range(2)])
    o["w_ple_gate"] = np.stack([_kmajor(f(inp["w_ple_gate"][i])) for i in range(2)])
    o["w_ple"] = np.stack([_kmajor(f(inp["w_ple"][i])) for i in range(2)])
    def tile_gu(w):
        F_ = w.shape[1]
        return np.ascontiguousarray(w.reshape(8, 128, F_ // 128, 128).transpose(2, 1, 0, 3)).reshape(128, -1)

    def tile_dn(w):
        F_ = w.shape[0]
        return np.ascontiguousarray(w.reshape(F_ // 128, 128, 8, 128).transpose(2, 1, 0, 3)).reshape(128, -1)
    o["w_ff_gate"] = tile_gu(f(inp["w_ff_gate"][0]))[None]
    o["w_ff_up"] = tile_gu(f(inp["w_ff_up"][0]))[None]
    o["w_ff_down"] = tile_dn(f(inp["w_ff_down"][0]))[None]
    o["w_moe_gate"] = np.stack([tile_gu(f(inp["w_moe_gate"][0, e])) for e in range(8)])
    o["w_moe_up"] = np.stack([tile_gu(f(inp["w_moe_up"][0, e])) for e in range(8)])
    o["w_moe_down"] = np.stack([tile_dn(f(inp["w_moe_down"][0, e])) for e in range(8)])
    o["w_router"] = _kmajor(f(inp["w_router"][0]))
    pm = lambda v: np.ascontiguousarray(f(v).reshape(-1, 128).T)
    gains = [pm(inp[n][i]) for i in range(2) for n in ("mix_norm", "ffn_norm", "ple_norm")] + [pm(inp["final_norm"])]
    o["gains"] = np.concatenate(gains, axis=1)
    def convtab(w):
        C = w.shape[1]
        return np.ascontiguousarray(f(w).T.reshape(C // 128, 128, 4).transpose(1, 0, 2)).reshape(128, -1)
    o["dnconv"] = np.concatenate([convtab(inp["dn_conv"][i]) for i in range(2)], axis=1)
    o["dnp6"] = np.stack([f(inp["dn_a_log"][0]), f(inp["dn_dt_bias"][0]), f(inp["dn_a_log"][1]), f(inp["dn_dt_bias"][1])], axis=1)
    o["dnnorm"] = np.ascontiguousarray(f(inp["dn_norm"]).T)
    o["ssmconv"] = np.concatenate([convtab(inp["ssm_conv"][i]) for i in range(2)], axis=1)
    o["ssmconvb"] = np.concatenate([pm(inp["ssm_conv_b"][i]) for i in range(2)], axis=1)
    o["ssmp12"] = np.stack([f(inp["ssm_a_log"][0]), f(inp["ssm_dt_bias"][0]), f(inp["ssm_a_log"][1]), f(inp["ssm_dt_bias"][1])], axis=1)
    o["ssmd"] = np.ascontiguousarray(np.broadcast_to(f(inp["ssm_d"]).reshape(1, 24), (128, 24)))
    o["ssmnorm"] = np.concatenate([pm(inp["ssm_norm"][i]) for i in range(2)], axis=1)
    o["ident"] = np.eye(128, dtype=np.float32)
    ii = np.arange(128)[:, None]
    jj = np.arange(128)[None, :]
    same = (ii // 64) == (jj // 64)
    mDA = np.where(same & (ii > jj), 0.0, NEG)
    mDP = np.where(same & (jj > ii), 0.0, NEG)
    mDQ = np.where(same & (jj >= ii), 0.0, NEG)
    o["masks"] = np.concatenate([mDA, mDP, mDQ], axis=1).astype(np.float32)
    def sel(n):
        s = np.zeros((n, n * 128), np.float32)
        for h in range(n):
            s[h, h * 128:(h + 1) * 128] = 1.0
        return s
    o["sel6"], o["sel12"], o["sel8"] = sel(6), sel(12), sel(8)
    slopes = np.exp2(-8.0 * (np.arange(12, dtype=np.float64) + 1.0) / 12.0)
    dil = [1] * 4 + [4] * 4 + [16] * 4
    kk = np.arange(128)[:, None].astype(np.float64)
    qq = np.arange(128)[None, :].astype(np.float64)
    tabs = []
    for h in range(12):
        prev = np.where(kk >= qq, -slopes[h] * dil[h] * (128 + qq - kk), NEG)
        cur = np.where(kk <= qq, -slopes[h] * dil[h] * (qq - kk), NEG)
        tabs.append(np.concatenate([prev, cur], axis=1))
    o["abias"] = np.concatenate(tabs, axis=1).astype(np.float32)
    return o


def _host_acts(inp, b0, ns):
    x = np.asarray(inp["x"][b0:b0 + ns], dtype=np.float32)
    xT = np.ascontiguousarray(x.reshape(ns, T, 8, 128).transpose(0, 3, 2, 1))
    p = np.asarray(inp["p"][:, b0:b0 + ns], dtype=np.float32)
    pT = np.ascontiguousarray(p.reshape(2, ns, T, 2, 128).transpose(0, 1, 4, 3, 2))
    return {"xT": xT, "pT": pT}


def _dump_dram(P, name, tt, shape, dtype):
    if name not in P.dbg:
        return
    o = P.nc.dram_tensor("dbg_" + name, list(shape), dtype, kind="ExternalOutput").ap()
    P.dbg_out[name] = o
    P.c.emit("sync", _I("dma_start", out=o, in_=tt.t), reads=tt.all(), dma=True)


def _conv_silu(P, raw, dst, cw, cb, bias_ap=None, bias_bufs=()):
    P.op("vector", _I("tensor_scalar", out=dst.t[:], in0=raw.t[:], scalar1=cw.t[:, cb + 3:cb + 4], scalar2=None, op0=ALU.mult),
         raw.all() + cw.all(), dst.all())
    for sft in (1, 2, 3):
        P.op("vector", _I("scalar_tensor_tensor",
            out=dst.t[:, sft:], in0=raw.t[:, :T - sft], scalar=cw.t[:, cb + 3 - sft:cb + 4 - sft], in1=dst.t[:, sft:], op0=ALU.mult, op1=ALU.add),
            raw.all() + cw.all() + dst.all(), dst.all())
    if bias_ap is None:
        P.op("scalar", _I("activation", out=dst.t[:], in_=dst.t[:], func=AF.Silu), dst.all(), dst.all())
    else:
        P.op("scalar", _I("activation", out=dst.t[:], in_=dst.t[:], func=AF.Silu, bias=bias_ap), dst.all() + list(bias_bufs), dst.all())


def _conv_silu2(P, raw, cac, dst, cw, cb, bias_ap, bias_bufs):
    P.op("vector", _I("tensor_scalar", out=cac.t[:], in0=raw.t[:], scalar1=cw.t[:, cb + 3:cb + 4], scalar2=None, op0=ALU.mult),
         raw.all() + cw.all(), cac.all())
    for sft in (1, 2, 3):
        P.op("vector", _I("scalar_tensor_tensor", out=cac.t[:, sft:], in0=raw.t[:, :T - sft], scalar=cw.t[:, cb + 3 - sft:cb + 4 - sft], in1=cac.t[:, sft:],
                          op0=ALU.mult, op1=ALU.add), raw.all() + cw.all() + cac.all(), cac.all())
    P.op("scalar", _I("activation", out=dst.t[:], in_=cac.t[:], func=AF.Silu, bias=bias_ap), cac.all() + list(bias_bufs), dst.all())


def _ssd(P, s, i, hT):
    c, k = P.c, P.k
    ident, masks, sel12 = k["ident"], k["masks"], k["sel12"]
    mDQ = masks.t[:, 256:384]
    p12 = k["ssmp12"]
    m_phase = c.mark()
    acsT = c.sb("ss_acsT", [12, T], F32)
    tok2 = c.sb("ss_tok2", [128, 16, 128], F32)
    cdb = c.sb("ss_cdb", [128, 12, 32], F32)
    negA = c.sb("ss_negA", [12, 1], F32)
    acshl = c.sb("ss_acshl", [44, T], BF16)
    sel44 = c.sb("ss_sel44", [44, 1536], BF16)
    identb = c.sb("ss_identb", [128, 128], BF16)
    P.copy("vector", identb.t[:], ident.t[:], ident.all(), identb.all())
    m1 = c.mark()
    wdt = c.sb("ss_wdt", [128, 8, 12], BF16)
    _load_wcols(P, wdt, i, C_SDT, 12)
    t0_ = c.sb("ss_t0", [12, T], F32)
    t1_ = c.sb("ss_t1", [12, T], F32)
    stk = c.sb("ss_stk", [128, T], F32)
    P.op("gpsimd", _I("memset", stk.t[:], 0.0), w=stk.all())
    P.op("scalar", _I("activation", out=negA.t[:], in_=p12.t[:, 2 * i:2 * i + 1], func=AF.Exp), p12.all(), negA.all())
    P.op("vector", _I("tensor_scalar", out=negA.t[:], in0=negA.t[:], scalar1=-1.0, scalar2=None, op0=ALU.mult), negA.all(), negA.all())

    def ev_dt(nt, ps):
        sl = slice(nt * 512, (nt + 1) * 512)
        P.op("scalar", _I("activation", out=stk.t[0:12, sl], in_=ps.t[0:12, :], func=AF.Exp, bias=p12.t[:, 2 * i + 1:2 * i + 2]),
             ps.all() + p12.all(), stk.all())
    _proj_fm(P, hT, wdt, 0, 12, ev_dt)
    P.op("scalar", _I("activation", out=stk.t[0:12, :], in_=stk.t[0:12, :], func=AF.Ln, bias=1.0), stk.all(), stk.all())
    P.op("vector", _I("tensor_scalar", out=t0_.t[:], in0=stk.t[0:12, :], scalar1=negA.t[:, 0:1], scalar2=None, op0=ALU.mult),
         stk.all() + negA.all(), t0_.all())
    ares = _cumsum64(P, t0_, t1_, 12)
    P.copy("vector", acsT.t[:], ares.t[:], ares.all(), acsT.all())
    a3 = acsT.t[:].rearrange("p (c t) -> p c t", t=64)
    P.op("vector", _I("tensor_tensor", out=t0_.t[:].rearrange("p (c t) -> p c t", t=64), in0=a3[:, :, 63:64].to_broadcast([12, 32, 64]),
                                             in1=a3, op=ALU.subtract), acsT.all(), t0_.all())
    P.op("scalar", _I("activation", out=t0_.t[:], in_=t0_.t[:], func=AF.Exp), t0_.all(), t0_.all())
    P.op("vector", _I("tensor_tensor", out=stk.t[32:44, :], in0=t0_.t[:], in1=stk.t[0:12, :], op=ALU.mult), t0_.all() + stk.all(), stk.all())
    P.op("scalar", _I("activation", out=stk.t[64:76, :], in_=acsT.t[:], func=AF.Exp), acsT.all(), stk.all())
    P.op("vector", _I("tensor_scalar", out=stk.t[96:108, :], in0=acsT.t[:], scalar1=-1.0, scalar2=None, op0=ALU.mult), acsT.all(), stk.all())
    for u4 in range(4):
        ps = _ps(P)
        for j in range(4):
            u = u4 * 4 + j
            P.tr(ps.t[:, j * 128:(j + 1) * 128], stk.t[:, u * 128:(u + 1) * 128], ident.t[:], stk.all() + ident.all(), ps.all())
        P.copy("vector", tok2.t[:, u4 * 4:(u4 + 1) * 4, :].rearrange("p a b -> p (a b)"), ps.t[:], ps.all(), tok2.all())
        ps.busy = False
    P.op("scalar", _I("activation", out=t1_.t[:, 0:32], in_=a3[:, :, 63], func=AF.Exp), acsT.all(), t1_.all())
    ps = _ps(P)
    for h in range(12):
        P.mm(ps.t[:, h * 32:(h + 1) * 32], sel12.t[:, h * 128:(h + 1) * 128], t1_.t[:, 0:32], True, True, sel12.all() + t1_.all(), ps.all())
    P.copy("vector", cdb.t[:].rearrange("p a b -> p (a b)"), ps.t[:, 0:384], ps.all(), cdb.all())
    ps.busy = False
    P.op("gpsimd", _I("memset", acshl.t[:], 0.0), w=acshl.all())
    P.copy("vector", acshl.t[0:12, :], acsT.t[:], acsT.all(), acshl.all())
    P.op("vector", _I("tensor_tensor", out=t0_.t[:], in0=acsT.t[:], in1=acshl.t[0:12, :], op=ALU.subtract), acsT.all() + acshl.all(), t0_.all())
    P.copy("vector", acshl.t[32:44, :], t0_.t[:], t0_.all(), acshl.all())
    P.op("gpsimd", _I("memset", sel44.t[:], 0.0), w=sel44.all())
    P.copy("vector", sel44.t[0:12, :], sel12.t[:], sel12.all(), sel44.all())
    P.copy("vector", sel44.t[32:44, :], sel12.t[:], sel12.all(), sel44.all())
    c.release(m1)
    if P.stop == 101:
        c.release(m_phase); return
    wx = [c.sb(f"ss_wx{j}", [128, 8, 128], BF16) for j in range(5)]
    wz = c.sb("ss_wz", [128, 8, 384], BF16)
    raw = c.sb("ss_raw", [128, T], F32)
    xs = [c.sb(f"ss_xs{j}", [128, T], BF16) for j in range(3)]
    Bm = c.sb("ss_Bm", [128, T], BF16)
    Cm = c.sb("ss_Cm", [128, T], BF16)
    cac = c.sb("ss_cac", [128, T], F32)
    ySs = c.sb("ss_y", [128, 3, T], BF16)
    prev = [c.sb(f"ss_prev{j}", [128, 6, 64], F32) for j in range(3)]
    prev16 = [c.sb(f"ss_prevb{j}", [128, 6, 64], BF16) for j in range(3)]
    Btok = c.sb("ss_Btok", [128, 128], BF16)
    xdt = c.sb("ss_xdt", [128, 6, 64], BF16)
    xdtd = c.sb("ss_xdtd", [128, 6, 64], BF16)
    xsk = c.sb("ss_xsk", [128, 6, 64], F32)
    tmpL = c.sb("ss_tmpL", [128, 6, 128], F32)
    LT = c.sb("ss_LT", [128, 6, 128], F32)
    Mh = c.sb("ss_Mh", [128, 6, 128], BF16)
    CmA = c.sb("ss_CmA", [128, 128], BF16)
    CmB = c.sb("ss_CmB", [128, 128], BF16)
    tpr = c.sb("ss_tpr", [128, 6, 64], F32)
    yt = c.sb("ss_yt", [128, 384], F32)
    sz = c.sb("ss_sz", [128, 384], F32)
    junk = c.sb("ss_junk", [128, 384], F32)
    ssq = c.sb("ss_ssq", [128, 2], F32)
    P.op("gpsimd", _I("memset", CmA.t[:], 0.0), w=CmA.all())
    P.op("gpsimd", _I("memset", CmB.t[:], 0.0), w=CmB.all())
    cw, cbias, dsk, nrm = k["ssmconv"], k["ssmconvb"], k["ssmd"], k["ssmnorm"]
    for g in range(2):
        chunks = [3 * g, 3 * g + 1, 3 * g + 2, 6 + g, 8 + g]
        cols = [C_SX + g * 384, C_SX + g * 384 + 128, C_SX + g * 384 + 256, C_SB + g * 128, C_SC + g * 128]
        for j in range(5):
            _load_wcols(P, wx[j], i, cols[j], 128)
        _load_wcols(P, wz, i, C_SZ + g * 384, 384)
        for j, dst in enumerate(xs + [Bm, Cm]):
            def ev_raw(nt, ps):
                sl = slice(nt * 512, (nt + 1) * 512)
                P.copy(P.ev_eng(), raw.t[:, sl], ps.t[:], ps.all(), raw.all())
            _proj_fm(P, hT, wx[j], 0, 128, ev_raw)
            ch = chunks[j]
            _conv_silu2(P, raw, cac, dst, cw, (i * 10 + ch) * 4, cbias.t[:, i * 10 + ch:i * 10 + ch + 1], cbias.all())
        if P.stop == 102:
            c.release(m_phase); return
        pa = prev[0]
        P.op("gpsimd", _I("memset", pa.t[:], 0.0), w=pa.all())
        P.op("gpsimd", _I("memset", prev16[0].t[:], 0.0), w=prev16[0].all())
        pi = 0
        for u in range(16):
            usl = slice(u * 128, (u + 1) * 128)
            hs = slice(6 * g, 6 * g + 6)
            pst = _ps(P)
            pstb = pst.t[:].bitcast(BF16)
            for j in range(3):
                P.tr(pstb[:, j * 128:(j + 1) * 128], xs[j].t[:, usl], identb.t[:], xs[j].all() + identb.all(), pst.all())
            P.tr(pstb[:, 384:512], Bm.t[:, usl], identb.t[:], Bm.all() + identb.all(), pst.all())
            px3 = pstb[:, 0:384].rearrange("p (h d) -> p h d", d=64)
            P.copy("scalar", Btok.t[:], pstb[:, 384:512], pst.all(), Btok.all())
            P.op("vector", _I("tensor_tensor", out=xdt.t[:], in0=px3, in1=tok2.t[:, u, 6 * g:6 * g + 6].unsqueeze(2).to_broadcast([128, 6, 64]), op=ALU.mult),
                 pst.all() + tok2.all(), xdt.all())
            P.op("vector", _I("tensor_tensor", out=xdtd.t[:], in0=px3, in1=tok2.t[:, u, 32 + 6 * g:38 + 6 * g].unsqueeze(2).to_broadcast([128, 6, 64]), op=ALU.mult),
                 pst.all() + tok2.all(), xdtd.all())
            P.op("vector", _I("tensor_tensor", out=xsk.t[:], in0=px3, in1=dsk.t[:, i * 12 + 6 * g:i * 12 + 6 * g + 6].unsqueeze(2).to_broadcast([128, 6, 64]), op=ALU.mult),
                 pst.all() + dsk.all(), xsk.all())
            pst.busy = False
            if P.stop == 103:
                c.release(m_phase); return
            P.copy("gpsimd", CmA.t[:, 0:64], Cm.t[:, u * 128:u * 128 + 64], Cm.all(), CmA.all())
            P.copy("gpsimd", CmB.t[:, 64:128], Cm.t[:, u * 128 + 64:u * 128 + 128], Cm.all(), CmB.all())
            pcb = _ps(P)
            P.mm(pcb.t[:, 0:128], Bm.t[:, usl], Cm.t[:, usl], True, True, Bm.all() + Cm.all(), pcb.all())
            pa0 = _ps(P)
            pa1 = _ps(P)
            for h in range(6):
                hh = 6 * g + h
                pp = pa0 if h < 4 else pa1
                col = (h % 4) * 128
                P.mm(pp.t[:, col:col + 128], sel44.t[:, hh * 128:(hh + 1) * 128], acshl.t[:, usl], True, True, sel44.all() + acshl.all(), pp.all())
            P.op("vector", _I("tensor_tensor", out=tmpL.t[:, 0:4, :], in0=pa0.t[:].rearrange("p (h d) -> p h d", d=128),
                                                     in1=mDQ.unsqueeze(1).to_broadcast([128, 4, 128]), op=ALU.add), pa0.all() + masks.all(), tmpL.all())
            P.op("vector", _I("tensor_tensor", out=tmpL.t[:, 4:6, :], in0=pa1.t[:, 0:256].rearrange("p (h d) -> p h d", d=128),
                                                     in1=mDQ.unsqueeze(1).to_broadcast([128, 2, 128]), op=ALU.add), pa1.all() + masks.all(), tmpL.all())
            pa0.busy = False
            pa1.busy = False
            for h in range(6):
                hh = 6 * g + h
                P.op("scalar", _I("activation", out=LT.t[:, h, :], in_=tmpL.t[:, h, :], func=AF.Exp, bias=tok2.t[:, u, 96 + hh:97 + hh]),
                     tmpL.all() + tok2.all(), LT.all())
            P.op("vector", _I("tensor_tensor", out=Mh.t[:], in0=LT.t[:], in1=pcb.t[:, 0:128].unsqueeze(1).to_broadcast([128, 6, 128]), op=ALU.mult),
                 LT.all() + pcb.all(), Mh.all())
            pcb.busy = False
            if P.stop == 104:
                c.release(m_phase); return
            py = _ps(P)
            for h in range(6):
                P.mm(py.t[:, h * 64:(h + 1) * 64], Mh.t[:, h, :], xdt.t[:, h, :], True, True, Mh.all() + xdt.all(), py.all())
            pa_, pb_, pn_ = prev[pi % 3], prev[(pi + 1) % 3], prev[(pi + 2) % 3]
            pa6, pb6, pn6 = prev16[pi % 3], prev16[(pi + 1) % 3], prev16[(pi + 2) % 3]
            pi += 2
            pss = _ps(P)
            P.mm(pss.t[:, 0:384], Btok.t[0:64, :], xdtd.t[0:64].rearrange("p h d -> p (h d)"), True, True, Btok.all() + xdtd.all(), pss.all())
            P.op("gpsimd", _I("tensor_tensor", out=tpr.t[:], in0=pa_.t[:], in1=cdb.t[:, hs, 2 * u:2 * u + 1].to_broadcast([128, 6, 64]), op=ALU.mult),
                 pa_.all() + cdb.all(), tpr.all())
            P.op("vector", _I("tensor_tensor", out=pb_.t[:].rearrange("p h d -> p (h d)"), in0=pss.t[:, 0:384], in1=tpr.t[:].rearrange("p h d -> p (h d)"), op=ALU.add),
                 pss.all() + tpr.all(), pb_.all())
            pss.busy = False
            P.copy("scalar", pb6.t[:], pb_.t[:], pb_.all(), pb6.all())
            pss2 = _ps(P)
            P.mm(pss2.t[:, 0:384], Btok.t[64:128, :], xdtd.t[64:128].rearrange("p h d -> p (h d)"), True, True, Btok.all() + xdtd.all(), pss2.all())
            P.op("gpsimd", _I("tensor_tensor", out=tpr.t[:], in0=pb_.t[:], in1=cdb.t[:, hs, 2 * u + 1:2 * u + 2].to_broadcast([128, 6, 64]), op=ALU.mult),
                 pb_.all() + cdb.all(), tpr.all())
            P.op("vector", _I("tensor_tensor", out=pn_.t[:].rearrange("p h d -> p (h d)"), in0=pss2.t[:, 0:384], in1=tpr.t[:].rearrange("p h d -> p (h d)"), op=ALU.add),
                 pss2.all() + tpr.all(), pn_.all())
            pss2.busy = False
            P.copy("scalar", pn6.t[:], pn_.t[:], pn_.all(), pn6.all())
            if P.stop == 105:
                c.release(m_phase); return
            po = _ps(P)
            P.mm(po.t[:, 0:384], CmA.t[:], pa6.t[:].rearrange("p h d -> p (h d)"), True, False, CmA.all() + pa6.all(), po.all())
            P.mm(po.t[:, 0:384], CmB.t[:], pb6.t[:].rearrange("p h d -> p (h d)"), False, True, CmB.all() + pb6.all(), po.all())
            P.op("vector", _I("tensor_tensor", out=yt.t[:].rearrange("p (h d) -> p h d", d=64), in0=po.t[:, 0:384].rearrange("p (h d) -> p h d", d=64),
                                                                 in1=tok2.t[:, u, 64 + 6 * g:70 + 6 * g].unsqueeze(2).to_broadcast([128, 6, 64]), op=ALU.mult),
                 po.all() + tok2.all(), yt.all())
            po.busy = False
            P.op("gpsimd", _I("tensor_tensor", out=yt.t[:], in0=yt.t[:], in1=xsk.t[:].rearrange("p h d -> p (h d)"), op=ALU.add), yt.all() + xsk.all(), yt.all())
            P.op("vector", _I("tensor_tensor", out=yt.t[:], in0=py.t[:, 0:384], in1=yt.t[:], op=ALU.add), py.all() + yt.all(), yt.all())
            py.busy = False
            if P.stop == 106:
                c.release(m_phase); return
            pz = _ps(P)
            for kk in range(8):
                P.mm(pz.t[:, 0:384], hT.t[:, kk, usl], wz.t[:, kk, :], kk == 0, kk == 7, [hT.b(u // 4)] + wz.all(), pz.all())
            P.op("scalar", _I("activation", out=sz.t[:], in_=pz.t[:, 0:384], func=AF.Silu), pz.all(), sz.all())
            pz.busy = False
            P.op("gpsimd", _I("tensor_tensor", out=yt.t[:], in0=yt.t[:], in1=sz.t[:], op=ALU.mult), yt.all() + sz.all(), yt.all())
            P.op("scalar", _I("activation", out=junk.t[:], in_=yt.t[:], func=AF.Square, accum_out=ssq.t[:, 0:1]), yt.all(), junk.all() + ssq.all())
            _rstd(P, ssq.t[:, 0:1], ssq.all(), ssq, ssq.t[:, 1:2], 1.0 / 384)
            P.op("vector", _I("tensor_scalar", out=yt.t[:], in0=yt.t[:], scalar1=ssq.t[:, 1:2], scalar2=None, op0=ALU.mult), yt.all() + ssq.all(), yt.all())
            if P.stop == 107:
                c.release(m_phase); return
            pT_ = _ps(P)
            for j in range(3):
                P.tr(pT_.t[:, j * 128:(j + 1) * 128], yt.t[:, j * 128:(j + 1) * 128], ident.t[:], yt.all() + ident.all(), pT_.all())
            for j in range(3):
                nc_ = i * 6 + 3 * g + j
                P.op("vector" if j != 1 else "scalar",
                     (_I("tensor_scalar", out=ySs.t[:, j, usl], in0=pT_.t[:, j * 128:(j + 1) * 128], scalar1=nrm.t[:, nc_:nc_ + 1], scalar2=None, op0=ALU.mult))
                     if j != 1 else
                     (_I("activation", out=ySs.t[:, j, usl], in_=pT_.t[:, j * 128:(j + 1) * 128], func=AF.Copy, scale=nrm.t[:, nc_:nc_ + 1])),
                     pT_.all() + nrm.all(), ySs.all())
            pT_.busy = False
            if P.stop == 108 or (P.stop == 110 and u == 1):
                c.release(m_phase); return
        if P.stop == 109:
            c.release(m_phase); return
        P.dma(P.yssm.t[:, 3 * g:3 * g + 3, :], ySs.t[:], r=ySs.all(), w=P.yssm.all())
    c.release(m_phase)


def _attn(P, s, i, hT):
    c, k = P.c, P.k
    m_phase = c.mark()
    abias = c.sb("at_abias", [128, 12 * 256], F32)
    P.dma(abias.t[:], P.abias_d, w=abias.all())
    acc = c.sb("at_acc", [128, 4, T], F32)
    qT = [c.sb(f"at_qT{j}", [128, T], BF16) for j in range(2)]
    kT = [c.sb(f"at_kT{j}", [128, T], BF16) for j in range(2)]
    wqk = [c.sb(f"at_wqk{j}", [128, 8, 128], BF16) for j in range(4)]
    wv = c.sb("at_wv", [128, 8, 256], BF16)
    Vext = c.sb("at_Vext", [128, 16, 4, 128], BF16)
    tmp = [c.sb(f"at_tmp{j}", [128, 256], F32) for j in range(4)]
    PT = [c.sb(f"at_PT{j}", [128, 256], BF16) for j in range(4)]
    P.op("gpsimd", _I("memset", Vext.t[:], 1.0), w=Vext.all())
    bi = 0
    for g in range(3):
        dil = (1, 4, 16)[g]
        tpp = (T // dil) // 128
        for cc in range(2):
            _load_wcols(P, wqk[cc], i, C_AQ + (4 * g + 2 * cc) * 64, 128)
            _load_wcols(P, wqk[2 + cc], i, C_AK + (4 * g + 2 * cc) * 64, 128)
        _load_wcols(P, wv, i, C_AV + 4 * g * 64, 256)
        for j, dst in enumerate(qT + kT):
            def ev_qk(nt, ps, dst=dst):
                sl = slice(nt * 512, (nt + 1) * 512)
                P.copy(P.ev_eng(), dst.t[:, sl], ps.t[:], ps.all(), dst.all())
            _proj_fm(P, hT, wqk[j], 0, 128, ev_qk)

        def tsl(ti):
            r, n = ti // tpp, ti % tpp
            st = r + dil * n * 128
            return slice(st, st + 127 * dil + 1, dil) if dil > 1 else slice(st, st + 128)
        for ti in range(16):
            ps = _ps(P)
            for kk in range(8):
                P.mm(ps.t[:, 0:256], hT.t[:, kk, tsl(ti)], wv.t[:, kk, :], kk == 0, kk == 7, hT.all() + wv.all(), ps.all())
            P.copy(P.ev_eng(), Vext.t[:, ti, :, 0:64], ps.t[:, 0:256].rearrange("p (h d) -> p h d", d=64), ps.all(), Vext.all())
            ps.busy = False
        for ti in range(16):
            for hg in range(4):
                h = 4 * g + hg
                cc, pb = hg // 2, 64 * (hg % 2)
                kh, qh = kT[cc], qT[cc]
                has_prev = (ti % tpp) >= 1
                cur = tsl(ti)
                tm, pt = tmp[bi % 4], PT[bi % 4]
                bi += 1
                pss = _ps(P)
                if has_prev:
                    P.mm(pss.t[:, 0:128], kh.t[pb:pb + 64, tsl(ti - 1)], qh.t[pb:pb + 64, cur], True, True, kh.all() + qh.all(), pss.all())
                P.mm(pss.t[:, 128:256], kh.t[pb:pb + 64, cur], qh.t[pb:pb + 64, cur], True, True, kh.all() + qh.all(), pss.all())
                lo = 0 if has_prev else 128
                P.op("vector", _I("scalar_tensor_tensor", out=tm.t[:, lo:256], in0=pss.t[:, lo:256], scalar=0.125, in1=abias.t[:, h * 256 + lo:(h + 1) * 256],
                                  op0=ALU.mult, op1=ALU.add), pss.all() + abias.all(), tm.all())
                pss.busy = False
                P.op("scalar", _I("activation", out=pt.t[:, lo:256], in_=tm.t[:, lo:256], func=AF.Exp), tm.all(), pt.all())
                pso = _ps(P)
                if has_prev:
                    P.mm(pso.t[:, 0:128], Vext.t[:, ti - 1, hg, :], pt.t[:, 0:128], True, False, Vext.all() + pt.all(), pso.all())
                P.mm(pso.t[:, 0:128], Vext.t[:, ti, hg, :], pt.t[:, 128:256], not has_prev, True, Vext.all() + pt.all(), pso.all())
                if dil == 1:
                    ab = [acc.b((hg, ti // 4))]
                elif dil == 4:
                    ab = [acc.b((hg, ti % tpp))]
                else:
                    ab = [acc.b((hg, q_)) for q_ in range(4)]
                if g == 0:
                    P.copy("vector", acc.t[:, hg, cur], pso.t[:, 0:128], pso.all(), ab)
                else:
                    P.op("vector", _I("tensor_tensor", out=acc.t[:, hg, cur], in0=acc.t[:, hg, cur], in1=pso.t[:, 0:128], op=ALU.add), pso.all() + ab, ab)
                pso.busy = False
    rd = c.sb("at_rd", [64, T], F32)
    yb = c.sb("at_yb", [64, 4, T], BF16)
    for hg in range(4):
        P.op("vector", _I("reciprocal", out=acc.t[64:128, hg, :], in_=acc.t[64:128, hg, :]), acc.all(), acc.all())
        P.copy("vector", rd.t[:], acc.t[64:128, hg, :], acc.all(), rd.all())
        P.op("gpsimd", _I("tensor_tensor", out=yb.t[:, hg, :], in0=acc.t[0:64, hg, :], in1=rd.t[:], op=ALU.mult), acc.all() + rd.all(), yb.all())
    P.dma(P.yattn.t[:], yb.t[:], r=yb.all(), w=P.yattn.all())
    c.release(m_phase)


def _load_w(P, dst, name, j, K, c0, ncols, kp=128):
    src = P.wb[name].t[j].rearrange("p (k n) -> p k n", k=K)[:, :, c0:c0 + ncols]
    P.dma(dst.t[0:kp, 0:K, 0:ncols], src, r=_wbufs(P, name, j), w=dst.all())


def _load_tile(P, dst, name, j, NTILE, K, ti):
    src = P.wb[name].t[j].rearrange("a b -> (a b)").rearrange("(f p k c) -> f p k c", f=NTILE, p=128, k=K)[ti]
    P.dma(dst.t[:, 0:K, :], src, r=_wbufs(P, name, j), w=dst.all())


def _merge(P, s, i, hT, x_sb, src_ap, src_bufs):
    c = P.c
    m = c.mark()
    yd = [c.sb(f"mg_yd{j}", [128, 6, 512], BF16) for j in range(1)]
    ys = [c.sb(f"mg_ys{j}", [128, 6, 512], BF16) for j in range(1)]
    ya = [c.sb(f"mg_ya{j}", [64, 4, 512], BF16) for j in range(1)]
    mg = [c.sb(f"mg_m{j}", [128, 8, 512], BF16) for j in range(1)]
    wbd = [c.sb(f"mg_wbd{j}", [128, 6, 128], BF16) for j in range(2)]
    wbs = [c.sb(f"mg_wbs{j}", [128, 6, 128], BF16) for j in range(2)]
    wba = [c.sb(f"mg_wba{j}", [64, 4, 128], BF16) for j in range(2)]
    wg = [c.sb(f"mg_wg{j}", [128, 8, 384], BF16) for j in range(2)]
    wo = [c.sb(f"mg_wo{j}", [128, 8, 128], BF16) for j in range(2)]
    sg = [c.sb(f"mg_sg{j}", [128, 512], F32) for j in range(3)]
    t1 = c.sb("mg_t1", [128, 512], F32)
    t2 = c.sb("mg_t2", [128, 512], F32)
    wi = 0
    for nt in range(4):
        sl = slice(nt * 512, (nt + 1) * 512)
        a, b_, cc, mm_ = yd[0], ys[0], ya[0], mg[0]
        P.dma(x_sb.t[:, :, sl], src_ap[:, :, sl], r=src_bufs, w=[x_sb.b(nt)])
        P.dma(a.t[:], P.ydn.t[:, :, sl], r=P.ydn.all(), w=a.all())
        P.dma(b_.t[:], P.yssm.t[:, :, sl], r=P.yssm.all(), w=b_.all())
        P.dma(cc.t[:], P.yattn.t[:, :, sl], r=P.yattn.all(), w=cc.all())
        for oc in range(8):
            w1, w2, w3, w4 = wbd[wi % 2], wbs[wi % 2], wba[wi % 2], wg[wi % 2]
            wi += 1
            _load_w(P, w1, "w_br_dn", i, 6, oc * 128, 128)
            _load_w(P, w2, "w_br_ssm", i, 6, oc * 128, 128)
            _load_w(P, w3, "w_br_attn", i, 4, oc * 128, 128, kp=64)
            for b in range(3):
                src = P.wb["w_in"].t[i].rearrange("p (k n) -> p k n", k=8)[:, :, C_GATE + b * 1024 + oc * 128:C_GATE + b * 1024 + (oc + 1) * 128]
                P.dma(w4.t[:, :, b * 128:(b + 1) * 128], src, r=_wbufs(P, "w_in", i), w=w4.all())
            for b in range(3):
                ps = _ps(P)
                for kk in range(8):
                    P.mm(ps.t[:], w4.t[:, kk, b * 128:(b + 1) * 128], hT.t[:, kk, sl], kk == 0, kk == 7, w4.all() + [hT.b(nt)], ps.all())
                P.op("scalar", _I("activation", out=sg[b].t[:], in_=ps.t[:], func=AF.Sigmoid), ps.all(), sg[b].all())
                ps.busy = False
            psd = _ps(P)
            for kk in range(6):
                P.mm(psd.t[:], w1.t[:, kk, :], a.t[:, kk, :], kk == 0, kk == 5, w1.all() + a.all(), psd.all())
            P.op("vector", _I("tensor_tensor", out=t1.t[:], in0=psd.t[:], in1=sg[0].t[:], op=ALU.mult), psd.all() + sg[0].all(), t1.all())
            psd.busy = False
            pss = _ps(P)
            for kk in range(6):
                P.mm(pss.t[:], w2.t[:, kk, :], b_.t[:, kk, :], kk == 0, kk == 5, w2.all() + b_.all(), pss.all())
            P.op("vector", _I("tensor_tensor", out=t2.t[:], in0=pss.t[:], in1=sg[1].t[:], op=ALU.mult), pss.all() + sg[1].all(), t2.all())
            pss.busy = False
            P.op("gpsimd", _I("tensor_tensor", out=t1.t[:], in0=t1.t[:], in1=t2.t[:], op=ALU.add), t1.all() + t2.all(), t1.all())
            psa = _ps(P)
            for kk in range(4):
                P.mm(psa.t[:], w3.t[0:64, kk, :], cc.t[0:64, kk, :], kk == 0, kk == 3, w3.all() + cc.all(), psa.all())
            P.op("vector", _I("tensor_tensor", out=t2.t[:], in0=psa.t[:], in1=sg[2].t[:], op=ALU.mult), psa.all() + sg[2].all(), t2.all())
            psa.busy = False
            P.op("gpsimd", _I("tensor_tensor", out=mm_.t[:, oc, :], in0=t1.t[:], in1=t2.t[:], op=ALU.add), t1.all() + t2.all(), mm_.all())
        for oc in range(8):
            w5 = wo[oc % 2]
            _load_w(P, w5, "w_out", i, 8, oc * 128, 128)
            ps = _ps(P)
            for kk in range(8):
                P.mm(ps.t[:], w5.t[:, kk, :], mm_.t[:, kk, :], kk == 0, kk == 7, w5.all() + mm_.all(), ps.all())
            P.op("vector", _I("tensor_tensor", out=x_sb.t[:, oc, sl], in0=x_sb.t[:, oc, sl], in1=ps.t[:], op=ALU.add), ps.all() + [x_sb.b(nt)], [x_sb.b(nt)])
            ps.busy = False
    c.release(m)


def _ffn_core(P, hT, x_sb, nt, gname, uname, dname, j, FC, act, wgu, wd, sgt, gbc=None, tg=None):
    sl = slice(nt * 512, (nt + 1) * 512)
    F = FC * 128
    for fc in range(FC):
        w = wgu[fc % 2]
        _load_tile(P, w[0], gname, j, FC, 8, fc)
        _load_tile(P, w[1], uname, j, FC, 8, fc)
        psg = _ps(P)
        for kk in range(8):
            P.mm(psg.t[:], w[0].t[:, kk, :], hT.t[:, kk, sl], kk == 0, kk == 7, w[0].all() + [hT.b(nt)], psg.all())
        psu = _ps(P)
        for kk in range(8):
            P.mm(psu.t[:], w[1].t[:, kk, :], hT.t[:, kk, sl], kk == 0, kk == 7, w[1].all() + [hT.b(nt)], psu.all())
        st = sgt[fc % 2]
        P.op("scalar", _I("activation", out=st.t[:], in_=psg.t[:], func=AF.Silu), psg.all(), st.all())
        psg.busy = False
        P.op("vector", _I("tensor_tensor", out=act.t[:, fc, :], in0=psu.t[:], in1=st.t[:], op=ALU.mult), psu.all() + st.all(), [act.b(fc)])
        psu.busy = False
    for oc in range(8):
        w = wd[oc % 2]
        _load_tile(P, w, dname, j, 8, FC, oc)
        ps = _ps(P)
        for fc in range(FC):
            P.mm(ps.t[:], w.t[:, fc, :], act.t[:, fc, :], fc == 0, fc == FC - 1, w.all() + [act.b(fc)], ps.all())
        if gbc is None:
            P.op("vector", _I("tensor_tensor", out=x_sb.t[:, oc, sl], in0=x_sb.t[:, oc, sl], in1=ps.t[:], op=ALU.add), ps.all() + [x_sb.b(nt)], [x_sb.b(nt)])
        else:
            t = tg[oc % 2]
            P.op("vector", _I("tensor_tensor", out=t.t[:], in0=ps.t[:], in1=gbc.t[:], op=ALU.mult), ps.all() + gbc.all(), t.all())
            P.op("gpsimd", _I("tensor_tensor", out=x_sb.t[:, oc, sl], in0=x_sb.t[:, oc, sl], in1=t.t[:], op=ALU.add), t.all() + [x_sb.b(nt)], [x_sb.b(nt)])
        ps.busy = False


def _ffn_phase(P, s, i, hT, x_sb):
    c, k = P.c, P.k
    _conv_some(P, 10 ** 6)
    m = c.mark()
    moe = (i % 2 == 1)
    FC = 28 if moe else 22
    sq = c.sb("ff_sq", [128, 8, 512], BF16)
    rs = c.sb("ff_rs", [128, 512], F32)
    act = c.sb("ff_act", [128, FC, 512], BF16)
    wgu = [(c.sb(f"ff_wg{j}", [128, 8, 128], BF16), c.sb(f"ff_wu{j}", [128, 8, 128], BF16)) for j in range(2)]
    wd = [c.sb(f"ff_wd{j}", [128, FC, 128], BF16) for j in range(2)]
    sgt = [c.sb(f"ff_sg{j}", [128, 512], F32) for j in range(2)]
    gcol = i * 24 + 8
    if moe:
        h32 = c.sb("ff_h32", [128, 8, 128], F32)
        lg = c.sb("ff_lg", [128, 4, 8], F32)
        m8 = c.sb("ff_m8", [128, 4, 8], F32)
        nm1 = c.sb("ff_nm1", [128, 4], F32)
        ex = c.sb("ff_ex", [128, 4, 8], F32)
        msk = c.sb("ff_msk", [128, 4, 8], F32)
        den = c.sb("ff_den", [128, 4], F32)
        gT = c.sb("ff_gT", [8, 512], F32)
        gbc = c.sb("ff_gbc", [128, 512], F32)
        tg = [c.sb(f"ff_tg{j}", [128, 512], F32) for j in range(2)]
        wr, sel8, ident = k["router"], k["sel8"], k["ident"]
    for nt in range(4):
        sl = slice(nt * 512, (nt + 1) * 512)
        xb = [x_sb.b(nt)]
        _rms_tile(P, x_sb, x_sb.t[:, :, sl], sq, rs, gcol, hT.t[:, :, sl], [hT.b(nt)], xbufs=xb)
        if not moe:
            _ffn_core(P, hT, x_sb, nt, "w_ff_gate", "w_ff_up", "w_ff_down", 0, FC, act, wgu, wd, sgt)
            continue
        g = k["gains"]
        psl = _ps(P)
        for sub in range(4):
            ssl = slice(nt * 512 + sub * 128, nt * 512 + (sub + 1) * 128)
            for kk in range(8):
                P.op("vector", _I("scalar_tensor_tensor", out=h32.t[:, kk, :], in0=x_sb.t[:, kk, ssl], scalar=g.t[:, gcol + kk:gcol + kk + 1],
                                  in1=rs.t[:, sub * 128:(sub + 1) * 128], op0=ALU.mult, op1=ALU.mult), xb + rs.all() + g.all(), h32.all())
            for kk in range(8):
                P.mm(psl.t[:, sub * 8:(sub + 1) * 8], h32.t[:, kk, :], wr.t[:, kk * 8:(kk + 1) * 8], kk == 0, kk == 7,
                     h32.all() + wr.all(), psl.all())
        P.copy("vector", lg.t[:].rearrange("p a b -> p (a b)"), psl.t[:, 0:32], psl.all(), lg.all())
        psl.busy = False
        for sub in range(4):
            P.op("vector", _I("max", out=m8.t[:, sub, :], in_=lg.t[:, sub, :]), lg.all(), m8.all())
        P.op("vector", _I("tensor_scalar", out=nm1.t[:], in0=m8.t[:, :, 0], scalar1=-1.0, scalar2=None, op0=ALU.mult), m8.all(), nm1.all())
        for sub in range(4):
            P.op("scalar", _I("activation", out=ex.t[:, sub, :], in_=lg.t[:, sub, :], func=AF.Exp, bias=nm1.t[:, sub:sub + 1]), lg.all() + nm1.all(), ex.all())
            P.op("vector", _I("tensor_scalar", out=msk.t[:, sub, :], in0=lg.t[:, sub, :], scalar1=m8.t[:, sub, 1:2], scalar2=None, op0=ALU.is_ge), lg.all() + m8.all(), msk.all())
        P.op("vector", _I("tensor_tensor", out=ex.t[:], in0=ex.t[:], in1=msk.t[:], op=ALU.mult), ex.all() + msk.all(), ex.all())
        P.op("vector", _I("tensor_reduce", out=den.t[:], in_=ex.t[:], axis=AX.X, op=ALU.add), ex.all(), den.all())
        P.op("vector", _I("reciprocal", out=den.t[:], in_=den.t[:]), den.all(), den.all())
        P.op("vector", _I("tensor_tensor", out=ex.t[:], in0=ex.t[:], in1=den.t[:].unsqueeze(2).to_broadcast([128, 4, 8]), op=ALU.mult), ex.all() + den.all(), ex.all())
        pst = _ps(P)
        for sub in range(4):
            P.tr(pst.t[0:8, sub * 128:(sub + 1) * 128], ex.t[:, sub, :], ident.t[:], ex.all() + ident.all(), pst.all())
        P.copy("vector", gT.t[:], pst.t[0:8, :], pst.all(), gT.all())
        pst.busy = False
        P.dump(f"gT{nt}", gT, gT.t[:], [8, 512])
        for e in range(8):
            psb = _ps(P)
            P.mm(psb.t[:], sel8.t[:, e * 128:(e + 1) * 128], gT.t[:], True, True, sel8.all() + gT.all(), psb.all())
            P.copy("scalar", gbc.t[:], psb.t[:], psb.all(), gbc.all())
            psb.busy = False
            _ffn_core(P, hT, x_sb, nt, "w_moe_gate", "w_moe_up", "w_moe_down", e, FC, act, wgu, wd, sgt, gbc=gbc, tg=tg)
    c.release(m)


def _ple(P, s, i, x_sb):
    c = P.c
    m = c.mark()
    sq = c.sb("pl_sq", [128, 8, 512], BF16)
    rs = c.sb("pl_rs", [128, 512], F32)
    h3 = c.sb("pl_h3", [128, 8, 512], BF16)
    pf = c.sb("pl_pf", [128, 2, 512], F32)
    pb = c.sb("pl_pb", [128, 2, 512], BF16)
    wpg = [c.sb(f"pl_wpg{j}", [128, 8, 128], BF16) for j in range(2)]
    wpl = [c.sb(f"pl_wpl{j}", [128, 2, 128], BF16) for j in range(2)]
    sg = [c.sb(f"pl_sg{j}", [128, 512], F32) for j in range(2)]
    tt_ = [c.sb(f"pl_t{j}", [128, 512], F32) for j in range(2)]
    for nt in range(4):
        sl = slice(nt * 512, (nt + 1) * 512)
        xb = [x_sb.b(nt)]
        _rms_tile(P, x_sb, x_sb.t[:, :, sl], sq, rs, i * 24 + 16, h3.t[:], h3.all(), xbufs=xb)
        P.dma(pf.t[:], P.pT[i, s][:, :, sl], w=pf.all())
        P.copy("gpsimd", pb.t[:], pf.t[:], pf.all(), pb.all())
        for oc in range(8):
            w1, w2 = wpg[oc % 2], wpl[oc % 2]
            _load_w(P, w1, "w_ple_gate", i, 8, oc * 128, 128)
            _load_w(P, w2, "w_ple", i, 2, oc * 128, 128)
            psg = _ps(P)
            for kk in range(8):
                P.mm(psg.t[:], w1.t[:, kk, :], h3.t[:, kk, :], kk == 0, kk == 7, w1.all() + h3.all(), psg.all())
            st = sg[oc % 2]
            P.op("scalar", _I("activation", out=st.t[:], in_=psg.t[:], func=AF.Sigmoid), psg.all(), st.all())
            psg.busy = False
            psp = _ps(P)
            for kk in range(2):
                P.mm(psp.t[:], w2.t[:, kk, :], pb.t[:, kk, :], kk == 0, kk == 1, w2.all() + pb.all(), psp.all())
            t = tt_[oc % 2]
            P.op("vector", _I("tensor_tensor", out=t.t[:], in0=psp.t[:], in1=st.t[:], op=ALU.mult), psp.all() + st.all(), t.all())
            psp.busy = False
            P.op("gpsimd", _I("tensor_tensor", out=x_sb.t[:, oc, sl], in0=x_sb.t[:, oc, sl], in1=t.t[:], op=ALU.add), t.all() + xb, xb)
    c.release(m)


def _final(P, s, x_sb):
    c = P.c
    m = c.mark()
    sq = c.sb("fn_sq", [128, 8, 512], BF16)
    rs = c.sb("fn_rs", [128, 512], F32)
    o = [c.sb(f"fn_o{j}", [128, 8, 512], F32) for j in range(2)]
    for nt in range(4):
        sl = slice(nt * 512, (nt + 1) * 512)
        ot = o[nt % 2]
        _rms_tile(P, x_sb, x_sb.t[:, :, sl], sq, rs, 48, ot.t[:], ot.all(), xbufs=[x_sb.b(nt)])
        P.dma(P.outT[s][:, :, sl], ot.t[:], r=ot.all())
    c.release(m)


def _layer(P, s, i, last, first=None):
    c = P.c
    m = c.mark()
    hT = c.sb("hT", [128, 8, T], BF16)
    if first is None:
        first = (i == 0)
    if first:
        src, sb_ = P.xT[s], []
    else:
        src, sb_ = P.xscr.t[s], [P.xscr.b(s)]
    _norm_phase(P, s, i, src, sb_, i * 24, hT)
    _deltanet3(P, s, i, hT)
    _ssd(P, s, i, hT)
    _attn(P, s, i, hT)
    x_sb = c.sb("x_sb", [128, 8, T], F32)
    _merge(P, s, i, hT, x_sb, src, sb_)
    P.dump(f"xmix{i}", x_sb, x_sb.t[:], [128, 8, T])
    _ffn_phase(P, s, i, hT, x_sb)
    P.dump(f"xffn{i}", x_sb, x_sb.t[:], [128, 8, T])
    _ple(P, s, i, x_sb)
    P.dump(f"xout{i}", x_sb, x_sb.t[:], [128, 8, T])
    if not last:
        P.dma(P.xscr.t[s], x_sb.t[:], r=x_sb.all(), w=[P.xscr.b(s)])
    else:
        _final(P, s, x_sb)
    c.release(m)


def build_program(n_seq, dbg=None, layers=(0, 1), skip_w=None):
    P = Prog(n_seq, dbg=dbg, skip_w=skip_w)
    _setup(P)
    for s in range(n_seq):
        for i in layers:
            _layer(P, s, i, last=(i == layers[-1]), first=(i == layers[0]))
    P.c.finish()
    return P


_CACHE = {}


def kernel(**inputs):
    n_cores = 8
    B = inputs["x"].shape[0]
    ns = B // n_cores
    if "P" not in _CACHE:
        _CACHE["P"] = build_program(ns)
    P = _CACHE["P"]
    hw = _host_weights(inputs)
    in_maps = []
    for cidx in range(n_cores):
        acts = _host_acts(inputs, cidx * ns, ns)
        im = {}
        for name in P.dram_in:
            im[name] = hw[name] if name in hw else acts[name]
        in_maps.append(im)
    res = run_bass_kernel_spmd(P.nc, in_maps, core_ids=list(range(n_cores)))
    outs = []
    for cidx in range(n_cores):
        oT = np.asarray(res.results[cidx]["outT"])
        outs.append(np.ascontiguousarray(oT.transpose(0, 3, 2, 1)).reshape(ns, T, D))
    return np.concatenate(outs, axis=0).astype(np.float32)


DN_NH = 2


def _deltanet2(P, s, i, hT):
    c, k = P.c, P.k
    ident, masks, sel6 = k["ident"], k["masks"], k["sel6"]
    mDA, mDP, mDQ = masks.t[:, 0:128], masks.t[:, 128:256], masks.t[:, 256:384]
    NH = DN_NH
    m_phase = c.mark()
    gamT = c.sb("dn_gamT", [6, T], F32)
    gbT = c.sb("dn_gbT", [6, T], F32)
    tok = c.sb("dn_tok", [128, 16, 128], F32)
    bgtok = c.sb("dn_bgtok", [128, 16, 8], F32)
    eglb = c.sb("dn_eglb", [128, 6, 32], F32)
    negA = c.sb("dn_negA", [6, 1], F32)
    identb = c.sb("dn_identb", [128, 128], BF16)
    P.copy("vector", identb.t[:], ident.t[:], ident.all(), identb.all())
    m1 = c.mark()
    wab = c.sb("dn_wab", [128, 8, 12], BF16)
    _load_wcols(P, wab, i, C_DNA, 12)
    t0_ = c.sb("dn_t0", [6, T], F32)
    t1_ = c.sb("dn_t1", [6, T], F32)
    lnb = c.sb("dn_lnb", [6, T], F32)
    stk = c.sb("dn_stk", [128, T], F32)
    P.op("gpsimd", _I("memset", stk.t[:], 0.0), w=stk.all())
    p6 = k["dnp6"]
    P.op("scalar", _I("activation", out=negA.t[:], in_=p6.t[:, 2 * i:2 * i + 1], func=AF.Exp), p6.all(), negA.all())
    P.op("vector", _I("tensor_scalar", out=negA.t[:], in0=negA.t[:], scalar1=-1.0, scalar2=None, op0=ALU.mult), negA.all(), negA.all())

    def ev_a(nt, ps):
        sl = slice(nt * 512, (nt + 1) * 512)
        P.op("scalar", _I("activation", out=t0_.t[:, sl], in_=ps.t[0:6, :], func=AF.Exp, bias=p6.t[:, 2 * i + 1:2 * i + 2]),
             ps.all() + p6.all(), t0_.all())
    _proj_fm(P, hT, wab, 0, 6, ev_a)
    P.op("scalar", _I("activation", out=t0_.t[:], in_=t0_.t[:], func=AF.Ln, bias=1.0), t0_.all(), t0_.all())
    P.op("vector", _I("tensor_scalar", out=t0_.t[:], in0=t0_.t[:], scalar1=negA.t[:, 0:1], scalar2=None, op0=ALU.mult),
         t0_.all() + negA.all(), t0_.all())
    gres = _cumsum64(P, t0_, t1_, 6)
    P.copy("vector", gamT.t[:], gres.t[:], gres.all(), gamT.all())

    def ev_b(nt, ps):
        sl = slice(nt * 512, (nt + 1) * 512)
        P.op("scalar", _I("activation", out=lnb.t[:, sl], in_=ps.t[0:6, :], func=AF.Exp, scale=-1.0), ps.all(), lnb.all())
    _proj_fm(P, hT, wab, 6, 6, ev_b)
    P.op("scalar", _I("activation", out=lnb.t[:], in_=lnb.t[:], func=AF.Ln, bias=1.0), lnb.all(), lnb.all())
    P.op("vector", _I("tensor_scalar", out=lnb.t[:], in0=lnb.t[:], scalar1=-1.0, scalar2=None, op0=ALU.mult), lnb.all(), lnb.all())
    P.op("vector", _I("tensor_tensor", out=gbT.t[:], in0=gamT.t[:], in1=lnb.t[:], op=ALU.add), gamT.all() + lnb.all(), gbT.all())
    P.op("vector", _I("tensor_scalar", out=stk.t[0:6, :], in0=gamT.t[:], scalar1=-1.0, scalar2=None, op0=ALU.mult), gamT.all(), stk.all())
    P.copy("vector", stk.t[32:38, :], gbT.t[:], gbT.all(), stk.all())
    g3 = gamT.t[:].rearrange("p (c t) -> p c t", t=64)
    P.op("vector", _I("tensor_tensor", out=t0_.t[:].rearrange("p (c t) -> p c t", t=64), in0=g3[:, :, 63:64].to_broadcast([6, 32, 64]),
                      in1=g3, op=ALU.subtract), gamT.all(), t0_.all())
    P.op("scalar", _I("activation", out=stk.t[64:70, :], in_=t0_.t[:], func=AF.Exp), t0_.all(), stk.all())
    P.op("scalar", _I("activation", out=stk.t[96:102, :], in_=lnb.t[:], func=AF.Exp), lnb.all(), stk.all())
    for u4 in range(4):
        ps = _ps(P)
        for j in range(4):
            u = u4 * 4 + j
            P.tr(ps.t[:, j * 128:(j + 1) * 128], stk.t[:, u * 128:(u + 1) * 128], ident.t[:], stk.all() + ident.all(), ps.all())
        P.copy("vector", tok.t[:, u4 * 4:(u4 + 1) * 4, :].rearrange("p a b -> p (a b)"), ps.t[:], ps.all(), tok.all())
        ps.busy = False
    P.op("scalar", _I("activation", out=bgtok.t[:, :, 0:6], in_=tok.t[:, :, 32:38], func=AF.Exp), tok.all(), bgtok.all())
    P.op("scalar", _I("activation", out=t1_.t[:, 0:32], in_=g3[:, :, 63], func=AF.Exp), gamT.all(), t1_.all())
    ps = _ps(P)
    for h in range(6):
        P.mm(ps.t[:, h * 32:(h + 1) * 32], sel6.t[:, h * 128:(h + 1) * 128], t1_.t[:, 0:32], True, True, sel6.all() + t1_.all(), ps.all())
    P.copy("vector", eglb.t[:].rearrange("p a b -> p (a b)"), ps.t[:, 0:192], ps.all(), eglb.all())
    ps.busy = False
    c.release(m1)
    wq = [c.sb(f"dn_w{j}", [128, 8, 128], BF16) for j in range(4)]
    raw = c.sb("dn_raw", [128, T], F32)
    cac = c.sb("dn_cac", [128, T], F32)
    rin = c.sb("dn_rin", [128, T], F32)
    cw = k["dnconv"]
    nrm = k["dnnorm"]
    HB = []
    for hh in range(NH):
        d = {}
        d["cq"] = c.sb(f"dn_cq{hh}", [128, T], BF16)
        d["ck"] = c.sb(f"dn_ck{hh}", [128, T], BF16)
        d["cv"] = c.sb(f"dn_cv{hh}", [128, T], BF16)
        d["zs"] = c.sb(f"dn_zs{hh}", [128, T], BF16)
        d["yT"] = c.sb(f"dn_yT{hh}", [128, T], BF16)
        d["S"] = [c.sb(f"dn_S{hh}_{j}", [128, 128], F32) for j in range(3)]
        d["tl"] = [c.sb(f"dn_tl{hh}_{j}", [128, 128], F32) for j in range(18)]
        d["ss"] = c.sb(f"dn_ss{hh}", [128, 2], F32)
        P.op("gpsimd", _I("memset", d["tl"][0].t[:], 0.0), w=d["tl"][0].all())
        P.op("gpsimd", _I("memset", d["tl"][1].t[:], 0.0), w=d["tl"][1].all())
        HB.append(d)

    def head_prep(h, d):
        cols = [C_DNQ + h * 128, C_DNK + h * 128, C_DNV + h * 128, C_DNZ + h * 128]
        for j in range(4):
            _load_wcols(P, wq[j], i, cols[j], 128)
        for j, dst in enumerate((d["cq"], d["ck"], d["cv"])):
            def ev_raw(nt, ps):
                sl = slice(nt * 512, (nt + 1) * 512)
                P.copy(P.ev_eng(), raw.t[:, sl], ps.t[:], ps.all(), raw.all())
            _proj_fm(P, hT, wq[j], 0, 128, ev_raw)
            cb = (i * 18 + j * 6 + h) * 4
            P.op("vector", _I("tensor_scalar", out=cac.t[:], in0=raw.t[:], scalar1=cw.t[:, cb + 3:cb + 4], scalar2=None, op0=ALU.mult),
                 raw.all() + cw.all(), cac.all())
            for sft in (1, 2, 3):
                P.op("vector", _I("scalar_tensor_tensor", out=cac.t[:, sft:], in0=raw.t[:, :T - sft], scalar=cw.t[:, cb + 3 - sft:cb + 4 - sft],
                                  in1=cac.t[:, sft:], op0=ALU.mult, op1=ALU.add), raw.all() + cw.all() + cac.all(), cac.all())
            if j == 2:
                P.op("scalar", _I("activation", out=dst.t[:], in_=cac.t[:], func=AF.Silu), cac.all(), dst.all())
                continue
            P.op("scalar", _I("activation", out=cac.t[:], in_=cac.t[:], func=AF.Silu), cac.all(), cac.all())
            P.op("scalar", _I("activation", out=raw.t[:], in_=cac.t[:], func=AF.Square), cac.all(), raw.all())
            for nt in range(4):
                sl = slice(nt * 512, (nt + 1) * 512)
                ps = _ps(P)
                P.mm(ps.t[:], P.ones_f.t[:], raw.t[:, sl], True, True, P.ones_f.all() + raw.all(), ps.all())
                _rstd(P, ps.t[:], ps.all(), rin, rin.t[:, sl], 1.0, extra_bias=(-0.5 * np.log(128.0) if j == 0 else 0.0))
                ps.busy = False
            P.op("vector", _I("tensor_tensor", out=dst.t[:], in0=cac.t[:], in1=rin.t[:], op=ALU.mult), cac.all() + rin.all(), dst.all())
        zs = d["zs"]

        def ev_z(nt, ps):
            sl = slice(nt * 512, (nt + 1) * 512)
            P.op("scalar", _I("activation", out=zs.t[:, sl], in_=ps.t[:], func=AF.Silu), ps.all(), zs.all())
        _proj_fm(P, hT, wq[3], 0, 128, ev_z)
        P.op("gpsimd", _I("memset", d["S"][0].t[:], 0.0), w=d["S"][0].all())

    def unit_gen(h, d, u):
        cq, ck, cv, zs, yT, S, tl, ssu = d["cq"], d["ck"], d["cv"], d["zs"], d["yT"], d["S"], d["tl"], d["ss"]
        qdA, qdB, kbg, kdec, vb, attnT, WT, U, vnew, o_n = tl[0:10]
        t = tl[10:18]
        tA, DA, tB, DP, tC, DQ, Eb = t[0], t[1], t[2], t[3], t[4], t[5], t[6]
        t0 = u * 128
        usl = slice(t0, t0 + 128)
        ngam = tok.t[:, u, h:h + 1]
        gbt = tok.t[:, u, 32 + h:33 + h]
        kd = tok.t[:, u, 64 + h:65 + h]
        beta = tok.t[:, u, 96 + h:97 + h]
        bg = bgtok.t[:, u, h:h + 1]
        selh = sel6.t[:, h * 128:(h + 1) * 128]
        psb = _ps(P)
        P.mm(psb.t[:, 0:128], selh, gamT.t[:, usl], True, True, sel6.all() + gamT.all(), psb.all())
        P.mm(psb.t[:, 128:256], selh, gbT.t[:, usl], True, True, sel6.all() + gbT.all(), psb.all())
        pst = _ps(P)
        pstb = pst.t[:].bitcast(BF16)
        P.tr(pstb[:, 0:128], ck.t[:, usl], identb.t[:], ck.all() + identb.all(), pst.all())
        P.tr(pstb[:, 128:256], cv.t[:, usl], identb.t[:], cv.all() + identb.all(), pst.all())
        psk = _ps(P)
        P.mm(psk.t[:, 0:128], ck.t[:, usl], ck.t[:, usl], True, True, ck.all(), psk.all())
        P.mm(psk.t[:, 128:256], ck.t[:, usl], cq.t[:, usl], True, True, ck.all() + cq.all(), psk.all())
        yield
        P.op("vector", _I("scalar_tensor_tensor", out=tA.t[:], in0=psb.t[:, 0:128], scalar=-1.0, in1=mDA, op0=ALU.mult, op1=ALU.add),
             psb.all() + masks.all(), tA.all())
        P.op("vector", _I("tensor_tensor", out=tB.t[:], in0=psb.t[:, 128:256], in1=mDP, op=ALU.add), psb.all() + masks.all(), tB.all())
        P.op("vector", _I("tensor_tensor", out=tC.t[:], in0=psb.t[:, 0:128], in1=mDQ, op=ALU.add), psb.all() + masks.all(), tC.all())
        P.op("scalar", _I("activation", out=Eb.t[:], in_=psb.t[:, 0:128], func=AF.Exp), psb.all(), Eb.all())
        psb.busy = False
        P.op("scalar", _I("activation", out=DA.t[:], in_=tA.t[:], func=AF.Exp, bias=gbt), tA.all() + tok.all(), DA.all())
        P.op("scalar", _I("activation", out=DP.t[:], in_=tB.t[:], func=AF.Exp, bias=ngam), tB.all() + tok.all(), DP.all())
        P.op("scalar", _I("activation", out=DQ.t[:], in_=tC.t[:], func=AF.Exp, bias=ngam), tC.all() + tok.all(), DQ.all())
        P.op("vector", _I("tensor_scalar", out=kbg.t[:], in0=pstb[:, 0:128], scalar1=bg, scalar2=None, op0=ALU.mult), pst.all() + bgtok.all(), kbg.all())
        P.op("vector", _I("tensor_scalar", out=kdec.t[:], in0=pstb[:, 0:128], scalar1=kd, scalar2=None, op0=ALU.mult), pst.all() + tok.all(), kdec.all())
        P.op("vector", _I("tensor_scalar", out=vb.t[:], in0=pstb[:, 128:256], scalar1=beta, scalar2=None, op0=ALU.mult), pst.all() + tok.all(), vb.all())
        pst.busy = False
        yield
        P.op("gpsimd", _I("tensor_tensor", out=qdA.t[:, 0:64], in0=cq.t[:, t0:t0 + 64], in1=Eb.t[:, 0:64], op=ALU.mult), cq.all() + Eb.all(), qdA.all())
        P.op("gpsimd", _I("tensor_tensor", out=qdB.t[:, 64:128], in0=cq.t[:, t0 + 64:t0 + 128], in1=Eb.t[:, 64:128], op=ALU.mult), cq.all() + Eb.all(), qdB.all())
        A0, P0, X0 = t[0], t[2], t[4]
        P.op("vector", _I("tensor_tensor", out=A0.t[:], in0=psk.t[:, 0:128], in1=DA.t[:], op=ALU.mult), psk.all() + DA.all(), A0.all())
        P.op("vector", _I("tensor_tensor", out=P0.t[:], in0=psk.t[:, 0:128], in1=DP.t[:], op=ALU.mult), psk.all() + DP.all(), P0.all())
        P.op("vector", _I("tensor_tensor", out=attnT.t[:], in0=psk.t[:, 128:256], in1=DQ.t[:], op=ALU.mult), psk.all() + DQ.all(), attnT.all())
        psk.busy = False
        P.op("gpsimd", _I("tensor_tensor", out=X0.t[:], in0=ident.t[:], in1=P0.t[:], op=ALU.subtract), ident.all() + P0.all(), X0.all())
        yield
        Am, Pm, X = A0, P0, X0
        Abuf, Pbuf, Xbuf = [t[1], t[6]], [t[3], t[7]], [t[5], t[4]]
        for n in range(5):
            An, Pn, Xn = Abuf[n % 2], Pbuf[n % 2], Xbuf[n % 2]
            psn = _ps(P)
            P.mm(psn.t[:, 0:128], Pm.t[:], Am.t[:], True, True, Pm.all() + Am.all(), psn.all())
            if n < 4:
                P.mm(psn.t[:, 128:256], Am.t[:], Pm.t[:], True, True, Pm.all() + Am.all(), psn.all())
            yield
            P.copy("scalar", An.t[:], psn.t[:, 0:128], psn.all(), An.all())
            if n < 4:
                P.copy("scalar", Pn.t[:], psn.t[:, 128:256], psn.all(), Pn.all())
            psn.busy = False
            psx = _ps(P)
            P.mm(psx.t[:, 0:128], An.t[:], X.t[:], True, True, An.all() + X.all(), psx.all())
            yield
            P.op("vector", _I("tensor_tensor", out=Xn.t[:], in0=psx.t[:, 0:128], in1=X.t[:], op=ALU.add), psx.all() + X.all(), Xn.all())
            psx.busy = False
            Am, Pm, X = An, Pn, Xn
        TTm = X
        psw = _ps(P)
        P.mm(psw.t[:, 0:128], kbg.t[:], TTm.t[:], True, True, kbg.all() + TTm.all(), psw.all())
        P.mm(psw.t[:, 128:256], TTm.t[:], vb.t[:], True, True, vb.all() + TTm.all(), psw.all())
        yield
        P.copy("scalar", WT.t[:], psw.t[:, 0:128], psw.all(), WT.all())
        P.copy("vector", U.t[:], psw.t[:, 128:256], psw.all(), U.all())
        psw.busy = False
        si = 2 * u
        Sa, Sb, Sn = S[si % 3], S[(si + 1) % 3], S[(si + 2) % 3]
        ps1 = _ps(P)
        P.mm(ps1.t[:, 0:128], WT.t[:], Sa.t[:], True, True, WT.all() + Sa.all(), ps1.all())
        pso = _ps(P)
        P.mm(pso.t[:, 0:128], qdA.t[:], Sa.t[:], True, False, qdA.all() + Sa.all(), pso.all())
        yield
        P.op("vector", _I("tensor_tensor", out=vnew.t[0:64, :], in0=U.t[0:64, :], in1=ps1.t[0:64, 0:128], op=ALU.subtract), U.all() + ps1.all(), vnew.all())
        ps1.busy = False
        pss = _ps(P)
        P.mm(pss.t[:, 0:128], kdec.t[0:64, :], vnew.t[0:64, :], True, True, kdec.all() + vnew.all(), pss.all())
        yield
        P.op("vector", _I("scalar_tensor_tensor", out=Sb.t[:], in0=Sa.t[:], scalar=eglb.t[:, h, 2 * u:2 * u + 1], in1=pss.t[:, 0:128],
                          op0=ALU.mult, op1=ALU.add), Sa.all() + pss.all() + eglb.all(), Sb.all())
        pss.busy = False
        ps2 = _ps(P)
        P.mm(ps2.t[:, 0:128], WT.t[:], Sb.t[:], True, True, WT.all() + Sb.all(), ps2.all())
        P.mm(pso.t[:, 0:128], qdB.t[:], Sb.t[:], False, False, qdB.all() + Sb.all(), pso.all())
        yield
        P.op("vector", _I("tensor_tensor", out=vnew.t[64:128, :], in0=U.t[64:128, :], in1=ps2.t[64:128, 0:128], op=ALU.subtract), U.all() + ps2.all(), vnew.all())
        ps2.busy = False
        P.mm(pso.t[:, 0:128], attnT.t[:], vnew.t[:], False, True, attnT.all() + vnew.all(), pso.all())
        pss2 = _ps(P)
        P.mm(pss2.t[:, 0:128], kdec.t[64:128, :], vnew.t[64:128, :], True, True, kdec.all() + vnew.all(), pss2.all())
        yield
        P.op("vector", _I("scalar_tensor_tensor", out=Sn.t[:], in0=Sb.t[:], scalar=eglb.t[:, h, 2 * u + 1:2 * u + 2], in1=pss2.t[:, 0:128],
                          op0=ALU.mult, op1=ALU.add), Sb.all() + pss2.all() + eglb.all(), Sn.all())
        pss2.busy = False
        junk = t[0]
        P.op("scalar", _I("activation", out=junk.t[:], in_=pso.t[:, 0:128], func=AF.Square, accum_out=ssu.t[:, 0:1]), pso.all(), junk.all() + ssu.all())
        _rstd(P, ssu.t[:, 0:1], ssu.all(), ssu, ssu.t[:, 1:2], 1.0 / 128)
        P.op("vector", _I("tensor_scalar", out=o_n.t[:], in0=pso.t[:, 0:128], scalar1=ssu.t[:, 1:2], scalar2=None, op0=ALU.mult), pso.all() + ssu.all(), o_n.all())
        pso.busy = False
        psy = _ps(P)
        P.tr(psy.t[:, 0:128], o_n.t[:], ident.t[:], o_n.all() + ident.all(), psy.all())
        yield
        P.op("vector", _I("scalar_tensor_tensor", out=yT.t[:, usl], in0=psy.t[:, 0:128], scalar=nrm.t[:, i:i + 1], in1=zs.t[:, usl],
                          op0=ALU.mult, op1=ALU.mult), psy.all() + nrm.all() + zs.all(), yT.all())
        psy.busy = False

    for h0 in range(0, 6, NH):
        heads = list(range(h0, min(6, h0 + NH)))
        for hh, h in enumerate(heads):
            head_prep(h, HB[hh])
        for u in range(16):
            _conv_some(P, 2)
            gens = [unit_gen(h, HB[hh], u) for hh, h in enumerate(heads)]
            while gens:
                nxt = []
                for g in gens:
                    try:
                        next(g)
                        nxt.append(g)
                    except StopIteration:
                        pass
                gens = nxt
        for hh, h in enumerate(heads):
            P.dma(P.ydn.t[:, h, :], HB[hh]["yT"].t[:], r=HB[hh]["yT"].all(), w=P.ydn.all())
    c.release(m_phase)


def _deltanet3(P, s, i, hT):
    c, k = P.c, P.k
    ident, masks, sel6 = k["ident"], k["masks"], k["sel6"]
    mDA, mDP, mDQ = masks.t[:, 0:128], masks.t[:, 128:256], masks.t[:, 256:384]
    NH = DN_NH
    m_phase = c.mark()
    tok = c.sb("dn_tok", [128, 16, 128], F32)
    bgtok = c.sb("dn_bgtok", [128, 16, 8], F32)
    eglb = c.sb("dn_eglb", [128, 6, 32], F32)
    negA = c.sb("dn_negA", [6, 1], F32)
    identb = c.sb("dn_identb", [128, 128], BF16)
    ghl = c.sb("dn_ghl", [38, T], BF16)
    gbhl = c.sb("dn_gbhl", [38, T], BF16)
    sel38 = c.sb("dn_sel38", [38, 768], BF16)
    P.copy("vector", identb.t[:], ident.t[:], ident.all(), identb.all())
    m1 = c.mark()
    gamT = c.sb("dn_gamT", [6, T], F32)
    gbT = c.sb("dn_gbT", [6, T], F32)
    wab = c.sb("dn_wab", [128, 8, 12], BF16)
    _load_wcols(P, wab, i, C_DNA, 12)
    t0_ = c.sb("dn_t0", [6, T], F32)
    t1_ = c.sb("dn_t1", [6, T], F32)
    lnb = c.sb("dn_lnb", [6, T], F32)
    stk = c.sb("dn_stk", [128, T], F32)
    P.op("gpsimd", _I("memset", stk.t[:], 0.0), w=stk.all())
    p6 = k["dnp6"]
    P.op("scalar", _I("activation", out=negA.t[:], in_=p6.t[:, 2 * i:2 * i + 1], func=AF.Exp), p6.all(), negA.all())
    P.op("vector", _I("tensor_scalar", out=negA.t[:], in0=negA.t[:], scalar1=-1.0, scalar2=None, op0=ALU.mult), negA.all(), negA.all())

    def ev_a(nt, ps):
        sl = slice(nt * 512, (nt + 1) * 512)
        P.op("scalar", _I("activation", out=t0_.t[:, sl], in_=ps.t[0:6, :], func=AF.Exp, bias=p6.t[:, 2 * i + 1:2 * i + 2]),
             ps.all() + p6.all(), t0_.all())
    _proj_fm(P, hT, wab, 0, 6, ev_a)
    P.op("scalar", _I("activation", out=t0_.t[:], in_=t0_.t[:], func=AF.Ln, bias=1.0), t0_.all(), t0_.all())
    P.op("vector", _I("tensor_scalar", out=t0_.t[:], in0=t0_.t[:], scalar1=negA.t[:, 0:1], scalar2=None, op0=ALU.mult),
         t0_.all() + negA.all(), t0_.all())
    gres = _cumsum64(P, t0_, t1_, 6)
    P.copy("vector", gamT.t[:], gres.t[:], gres.all(), gamT.all())

    def ev_b(nt, ps):
        sl = slice(nt * 512, (nt + 1) * 512)
        P.op("scalar", _I("activation", out=lnb.t[:, sl], in_=ps.t[0:6, :], func=AF.Exp, scale=-1.0), ps.all(), lnb.all())
    _proj_fm(P, hT, wab, 6, 6, ev_b)
    P.op("scalar", _I("activation", out=lnb.t[:], in_=lnb.t[:], func=AF.Ln, bias=1.0), lnb.all(), lnb.all())
    P.op("vector", _I("tensor_scalar", out=lnb.t[:], in0=lnb.t[:], scalar1=-1.0, scalar2=None, op0=ALU.mult), lnb.all(), lnb.all())
    P.op("vector", _I("tensor_tensor", out=gbT.t[:], in0=gamT.t[:], in1=lnb.t[:], op=ALU.add), gamT.all() + lnb.all(), gbT.all())
    P.op("vector", _I("tensor_scalar", out=stk.t[0:6, :], in0=gamT.t[:], scalar1=-1.0, scalar2=None, op0=ALU.mult), gamT.all(), stk.all())
    P.copy("vector", stk.t[32:38, :], gbT.t[:], gbT.all(), stk.all())
    g3 = gamT.t[:].rearrange("p (c t) -> p c t", t=64)
    P.op("vector", _I("tensor_tensor", out=t0_.t[:].rearrange("p (c t) -> p c t", t=64), in0=g3[:, :, 63:64].to_broadcast([6, 32, 64]),
                      in1=g3, op=ALU.subtract), gamT.all(), t0_.all())
    P.op("scalar", _I("activation", out=stk.t[64:70, :], in_=t0_.t[:], func=AF.Exp), t0_.all(), stk.all())
    P.op("scalar", _I("activation", out=stk.t[96:102, :], in_=lnb.t[:], func=AF.Exp), lnb.all(), stk.all())
    for u4 in range(4):
        ps = _ps(P)
        for j in range(4):
            u = u4 * 4 + j
            P.tr(ps.t[:, j * 128:(j + 1) * 128], stk.t[:, u * 128:(u + 1) * 128], ident.t[:], stk.all() + ident.all(), ps.all())
        P.copy("vector", tok.t[:, u4 * 4:(u4 + 1) * 4, :].rearrange("p a b -> p (a b)"), ps.t[:], ps.all(), tok.all())
        ps.busy = False
    P.op("scalar", _I("activation", out=bgtok.t[:, :, 0:6], in_=tok.t[:, :, 32:38], func=AF.Exp), tok.all(), bgtok.all())
    P.op("scalar", _I("activation", out=t1_.t[:, 0:32], in_=g3[:, :, 63], func=AF.Exp), gamT.all(), t1_.all())
    ps = _ps(P)
    for h in range(6):
        P.mm(ps.t[:, h * 32:(h + 1) * 32], sel6.t[:, h * 128:(h + 1) * 128], t1_.t[:, 0:32], True, True, sel6.all() + t1_.all(), ps.all())
    P.copy("vector", eglb.t[:].rearrange("p a b -> p (a b)"), ps.t[:, 0:192], ps.all(), eglb.all())
    ps.busy = False
    for src_, dst_ in ((gamT, ghl), (gbT, gbhl)):
        P.op("gpsimd", _I("memset", dst_.t[:], 0.0), w=dst_.all())
        P.copy("vector", dst_.t[0:6, :], src_.t[:], src_.all(), dst_.all())
        P.op("vector", _I("tensor_tensor", out=t0_.t[:], in0=src_.t[:], in1=dst_.t[0:6, :], op=ALU.subtract), src_.all() + dst_.all(), t0_.all())
        P.copy("vector", dst_.t[32:38, :], t0_.t[:], t0_.all(), dst_.all())
    P.op("gpsimd", _I("memset", sel38.t[:], 0.0), w=sel38.all())
    P.copy("vector", sel38.t[0:6, :], sel6.t[:], sel6.all(), sel38.all())
    P.copy("vector", sel38.t[32:38, :], sel6.t[:], sel6.all(), sel38.all())
    c.release(m1)
    NU = DN_NU
    cw = k["dnconv"]
    nrm = k["dnnorm"]
    HB = []
    for hh in range(NH):
        d = {}
        d["cq"] = c.sb(f"dn_cq{hh}", [128, T], BF16)
        d["ck"] = c.sb(f"dn_ck{hh}", [128, T], BF16)
        d["cv"] = c.sb(f"dn_cv{hh}", [128, T], BF16)
        d["zs"] = c.sb(f"dn_zs{hh}", [128, T], BF16)
        d["yT"] = c.sb(f"dn_yT{hh}", [128, T], BF16)
        d["S"] = [c.sb(f"dn_S{hh}_{j}", [128, 128], F32) for j in range(4)]
        d["Sb16"] = [c.sb(f"dn_Sb{hh}_{j}", [128, 128], BF16) for j in range(4)]
        HB.append(d)

    class QS:
        def __init__(self, bank, q):
            self.bank, self.q = bank, q
            self.t = bank.t[:, q * 128:(q + 1) * 128]
            self.tb = bank.t[:, q * 128:(q + 1) * 128].bitcast(BF16)
            self.busy = False

        def all(self):
            return self.bank.all()
    NQ = 32 // (NH * NU)
    QP = [[QS(P.PS[(NQ * kk + j) // 4], (NQ * kk + j) % 4) for j in range(NQ)] for kk in range(NH * NU)]
    qrr = [0] * (NH * NU)

    def qa(kk):
        for _ in range(NQ):
            j = qrr[kk]
            qrr[kk] = (j + 1) % NQ
            if not QP[kk][j].busy:
                QP[kk][j].busy = True
                return QP[kk][j]
        raise RuntimeError("no free PSUM quarter")

    def head_prep(h, d, raw, cac, rin, wq):
        cols = [C_DNQ + h * 128, C_DNK + h * 128, C_DNV + h * 128, C_DNZ + h * 128]
        for j in range(4):
            _load_wcols(P, wq[j], i, cols[j], 128)
        for j, dst in enumerate((d["cq"], d["ck"], d["cv"])):
            def ev_raw(nt, ps):
                sl = slice(nt * 512, (nt + 1) * 512)
                P.copy(P.ev_eng(), raw.t[:, sl], ps.t[:], ps.all(), raw.all())
            _proj_fm(P, hT, wq[j], 0, 128, ev_raw)
            cb = (i * 18 + j * 6 + h) * 4
            P.op("vector", _I("tensor_scalar", out=cac.t[:], in0=raw.t[:], scalar1=cw.t[:, cb + 3:cb + 4], scalar2=None, op0=ALU.mult),
                 raw.all() + cw.all(), cac.all())
            for sft in (1, 2, 3):
                P.op("vector", _I("scalar_tensor_tensor", out=cac.t[:, sft:], in0=raw.t[:, :T - sft], scalar=cw.t[:, cb + 3 - sft:cb + 4 - sft],
                                  in1=cac.t[:, sft:], op0=ALU.mult, op1=ALU.add), raw.all() + cw.all() + cac.all(), cac.all())
            if j == 2:
                P.op("scalar", _I("activation", out=dst.t[:], in_=cac.t[:], func=AF.Silu), cac.all(), dst.all())
                continue
            P.op("scalar", _I("activation", out=cac.t[:], in_=cac.t[:], func=AF.Silu), cac.all(), cac.all())
            P.op("scalar", _I("activation", out=raw.t[:], in_=cac.t[:], func=AF.Square), cac.all(), raw.all())
            for nt in range(4):
                sl = slice(nt * 512, (nt + 1) * 512)
                ps = _ps(P)
                P.mm(ps.t[:], P.ones_f.t[:], raw.t[:, sl], True, True, P.ones_f.all() + raw.all(), ps.all())
                _rstd(P, ps.t[:], ps.all(), rin, rin.t[:, sl], 1.0, extra_bias=(-0.5 * np.log(128.0) if j == 0 else 0.0))
                ps.busy = False
            P.op("vector", _I("tensor_tensor", out=dst.t[:], in0=cac.t[:], in1=rin.t[:], op=ALU.mult), cac.all() + rin.all(), dst.all())
        zs = d["zs"]

        def ev_z(nt, ps):
            sl = slice(nt * 512, (nt + 1) * 512)
            P.op("scalar", _I("activation", out=zs.t[:, sl], in_=ps.t[:], func=AF.Silu), ps.all(), zs.all())
        _proj_fm(P, hT, wq[3], 0, 128, ev_z)
        P.op("gpsimd", _I("memset", d["S"][0].t[:], 0.0), w=d["S"][0].all())
        P.op("gpsimd", _I("memset", d["Sb16"][0].t[:], 0.0), w=d["Sb16"][0].all())

    def unit_gen(h, d, u, tl, ssu, kk, tlb):
        cq, ck, cv, zs, yT, S, S16 = d["cq"], d["ck"], d["cv"], d["zs"], d["yT"], d["S"], d["Sb16"]
        qdA, qdB, kbg, kdec, vb, attnT, WT, vnew = tlb[0:8]
        tb_ = tlb[8:16]
        U, o_n = tl[0:2]
        t = tl[2:10]
        tA, DA, tB, DP, tC, DQ, Eb = t[0], t[1], t[2], t[3], t[4], t[5], t[6]
        t0 = u * 128
        usl = slice(t0, t0 + 128)
        ngam = tok.t[:, u, h:h + 1]
        gbt = tok.t[:, u, 32 + h:33 + h]
        kd = tok.t[:, u, 64 + h:65 + h]
        beta = tok.t[:, u, 96 + h:97 + h]
        bg = bgtok.t[:, u, h:h + 1]
        selh = sel38.t[:, h * 128:(h + 1) * 128]
        pg, pgb, pst, pkk, pkq = qa(kk), qa(kk), qa(kk), qa(kk), qa(kk)
        P.mm(pg.t, selh, ghl.t[:, usl], True, True, sel38.all() + ghl.all(), pg.all())
        P.mm(pgb.t, selh, gbhl.t[:, usl], True, True, sel38.all() + gbhl.all(), pgb.all())
        P.tr(pst.tb[:, 0:128], ck.t[:, usl], identb.t[:], ck.all() + identb.all(), pst.all())
        P.tr(pst.tb[:, 128:256], cv.t[:, usl], identb.t[:], cv.all() + identb.all(), pst.all())
        P.mm(pkk.t, ck.t[:, usl], ck.t[:, usl], True, True, ck.all(), pkk.all())
        P.mm(pkq.t, ck.t[:, usl], cq.t[:, usl], True, True, ck.all() + cq.all(), pkq.all())
        yield
        P.op("vector", _I("scalar_tensor_tensor", out=tA.t[:], in0=pg.t, scalar=-1.0, in1=mDA, op0=ALU.mult, op1=ALU.add), pg.all() + masks.all(), tA.all())
        P.op("vector", _I("tensor_tensor", out=tB.t[:], in0=pgb.t, in1=mDP, op=ALU.add), pgb.all() + masks.all(), tB.all())
        P.op("vector", _I("tensor_tensor", out=tC.t[:], in0=pg.t, in1=mDQ, op=ALU.add), pg.all() + masks.all(), tC.all())
        P.op("scalar", _I("activation", out=Eb.t[:], in_=pg.t, func=AF.Exp), pg.all(), Eb.all())
        pg.busy = False
        pgb.busy = False
        P.op("scalar", _I("activation", out=DA.t[:], in_=tA.t[:], func=AF.Exp, bias=gbt), tA.all() + tok.all(), DA.all())
        P.op("scalar", _I("activation", out=DP.t[:], in_=tB.t[:], func=AF.Exp, bias=ngam), tB.all() + tok.all(), DP.all())
        P.op("scalar", _I("activation", out=DQ.t[:], in_=tC.t[:], func=AF.Exp, bias=ngam), tC.all() + tok.all(), DQ.all())
        P.op("vector", _I("tensor_scalar", out=kbg.t[:], in0=pst.tb[:, 0:128], scalar1=bg, scalar2=None, op0=ALU.mult), pst.all() + bgtok.all(), kbg.all())
        P.op("vector", _I("tensor_scalar", out=kdec.t[:], in0=pst.tb[:, 0:128], scalar1=kd, scalar2=None, op0=ALU.mult), pst.all() + tok.all(), kdec.all())
        P.op("vector", _I("tensor_scalar", out=vb.t[:], in0=pst.tb[:, 128:256], scalar1=beta, scalar2=None, op0=ALU.mult), pst.all() + tok.all(), vb.all())
        pst.busy = False
        yield
        P.op("gpsimd", _I("tensor_tensor", out=qdA.t[:, 0:64], in0=cq.t[:, t0:t0 + 64], in1=Eb.t[:, 0:64], op=ALU.mult), cq.all() + Eb.all(), qdA.all())
        P.op("gpsimd", _I("tensor_tensor", out=qdB.t[:, 64:128], in0=cq.t[:, t0 + 64:t0 + 128], in1=Eb.t[:, 64:128], op=ALU.mult), cq.all() + Eb.all(), qdB.all())
        A0, P0, X0 = tb_[0], tb_[2], tb_[4]
        P.op("vector", _I("tensor_tensor", out=A0.t[:], in0=pkk.t, in1=DA.t[:], op=ALU.mult), pkk.all() + DA.all(), A0.all())
        P.op("vector", _I("tensor_tensor", out=P0.t[:], in0=pkk.t, in1=DP.t[:], op=ALU.mult), pkk.all() + DP.all(), P0.all())
        P.op("vector", _I("tensor_tensor", out=attnT.t[:], in0=pkq.t, in1=DQ.t[:], op=ALU.mult), pkq.all() + DQ.all(), attnT.all())
        pkk.busy = False
        pkq.busy = False
        P.op("gpsimd", _I("tensor_tensor", out=X0.t[:], in0=ident.t[:], in1=P0.t[:], op=ALU.subtract), ident.all() + P0.all(), X0.all())
        yield
        Am, Pm, X = A0, P0, X0
        Abuf, Pbuf, Xbuf = [tb_[1], tb_[6]], [tb_[3], tb_[7]], [tb_[5], tb_[4]]
        for n in range(5):
            An, Pn, Xn = Abuf[n % 2], Pbuf[n % 2], Xbuf[n % 2]
            pa_ = qa(kk)
            P.mm(pa_.t, Pm.t[:], Am.t[:], True, True, Pm.all() + Am.all(), pa_.all())
            if n < 4:
                pp_ = qa(kk)
                P.mm(pp_.t, Am.t[:], Pm.t[:], True, True, Pm.all() + Am.all(), pp_.all())
            yield
            P.copy("scalar", An.t[:], pa_.t, pa_.all(), An.all())
            pa_.busy = False
            if n < 4:
                P.copy("scalar", Pn.t[:], pp_.t, pp_.all(), Pn.all())
                pp_.busy = False
            px_ = qa(kk)
            P.mm(px_.t, An.t[:], X.t[:], True, True, An.all() + X.all(), px_.all())
            yield
            P.op("vector", _I("tensor_tensor", out=Xn.t[:], in0=px_.t, in1=X.t[:], op=ALU.add), px_.all() + X.all(), Xn.all())
            px_.busy = False
            Am, Pm, X = An, Pn, Xn
        TTm = X
        pw_, pu_ = qa(kk), qa(kk)
        P.mm(pw_.t, kbg.t[:], TTm.t[:], True, True, kbg.all() + TTm.all(), pw_.all())
        P.mm(pu_.t, TTm.t[:], vb.t[:], True, True, vb.all() + TTm.all(), pu_.all())
        yield
        P.copy("scalar", WT.t[:], pw_.t, pw_.all(), WT.all())
        P.copy("vector", U.t[:], pu_.t, pu_.all(), U.all())
        pw_.busy = False
        pu_.busy = False
        si = 2 * u
        Sa, Sb, Sn = S[si % 4], S[(si + 1) % 4], S[(si + 2) % 4]
        Sa6, Sb6, Sn6 = S16[si % 4], S16[(si + 1) % 4], S16[(si + 2) % 4]
        ps1 = qa(kk)
        P.mm(ps1.t, WT.t[:], Sa6.t[:], True, True, WT.all() + Sa6.all(), ps1.all())
        yield
        P.op("vector", _I("tensor_tensor", out=vnew.t[0:64, :], in0=U.t[0:64, :], in1=ps1.t[0:64, :], op=ALU.subtract), U.all() + ps1.all(), vnew.all())
        ps1.busy = False
        pss = qa(kk)
        P.mm(pss.t, kdec.t[0:64, :], vnew.t[0:64, :], True, True, kdec.all() + vnew.all(), pss.all())
        yield
        P.op("vector", _I("scalar_tensor_tensor", out=Sb.t[:], in0=Sa.t[:], scalar=eglb.t[:, h, 2 * u:2 * u + 1], in1=pss.t,
                          op0=ALU.mult, op1=ALU.add), Sa.all() + pss.all() + eglb.all(), Sb.all())
        pss.busy = False
        P.copy("scalar", Sb6.t[:], Sb.t[:], Sb.all(), Sb6.all())
        ps2 = qa(kk)
        P.mm(ps2.t, WT.t[:], Sb6.t[:], True, True, WT.all() + Sb6.all(), ps2.all())
        yield
        P.op("vector", _I("tensor_tensor", out=vnew.t[64:128, :], in0=U.t[64:128, :], in1=ps2.t[64:128, :], op=ALU.subtract), U.all() + ps2.all(), vnew.all())
        ps2.busy = False
        pss2 = qa(kk)
        P.mm(pss2.t, kdec.t[64:128, :], vnew.t[64:128, :], True, True, kdec.all() + vnew.all(), pss2.all())
        pso = qa(kk)
        P.mm(pso.t, qdA.t[:], Sa6.t[:], True, False, qdA.all() + Sa6.all(), pso.all())
        P.mm(pso.t, qdB.t[:], Sb6.t[:], False, False, qdB.all() + Sb6.all(), pso.all())
        P.mm(pso.t, attnT.t[:], vnew.t[:], False, True, attnT.all() + vnew.all(), pso.all())
        yield
        P.op("vector", _I("scalar_tensor_tensor", out=Sn.t[:], in0=Sb.t[:], scalar=eglb.t[:, h, 2 * u + 1:2 * u + 2], in1=pss2.t,
                          op0=ALU.mult, op1=ALU.add), Sb.all() + pss2.all() + eglb.all(), Sn.all())
        pss2.busy = False
        P.copy("scalar", Sn6.t[:], Sn.t[:], Sn.all(), Sn6.all())
        junk = t[0]
        P.op("scalar", _I("activation", out=junk.t[:], in_=pso.t, func=AF.Square, accum_out=ssu.t[:, 0:1]), pso.all(), junk.all() + ssu.all())
        _rstd(P, ssu.t[:, 0:1], ssu.all(), ssu, ssu.t[:, 1:2], 1.0 / 128)
        P.op("vector", _I("tensor_scalar", out=o_n.t[:], in0=pso.t, scalar1=ssu.t[:, 1:2], scalar2=None, op0=ALU.mult), pso.all() + ssu.all(), o_n.all())
        pso.busy = False
        psy = qa(kk)
        P.tr(psy.t, o_n.t[:], ident.t[:], o_n.all() + ident.all(), psy.all())
        yield
        P.op("vector", _I("scalar_tensor_tensor", out=yT.t[:, usl], in0=psy.t, scalar=nrm.t[:, i:i + 1], in1=zs.t[:, usl],
                          op0=ALU.mult, op1=ALU.mult), psy.all() + nrm.all() + zs.all(), yT.all())
        psy.busy = False

    STAG = 7
    for h0 in range(0, 6, NH):
        heads = list(range(h0, min(6, h0 + NH)))
        mg = c.mark()
        wq = [c.sb(f"dn_w{j}", [128, 8, 128], BF16) for j in range(4)]
        raw = c.sb("dn_raw", [128, T], F32)
        cac = c.sb("dn_cac", [128, T], F32)
        rin = c.sb("dn_rin", [128, T], F32)
        for hh, h in enumerate(heads):
            head_prep(h, HB[hh], raw, cac, rin, wq)
        c.release(mg)
        mt = c.mark()
        TL = [[[c.sb(f"dn_tl{hh}_{uu}_{j}", [128, 128], F32) for j in range(10)] for uu in range(NU)] for hh in range(NH)]
        TLB = [[[c.sb(f"dn_tlb{hh}_{uu}_{j}", [128, 128], BF16) for j in range(16)] for uu in range(NU)] for hh in range(NH)]
        SS = [[c.sb(f"dn_ss{hh}_{uu}", [128, 2], F32) for uu in range(NU)] for hh in range(NH)]
        for hh in range(NH):
            for uu in range(NU):
                P.op("gpsimd", _I("memset", TLB[hh][uu][0].t[:], 0.0), w=TLB[hh][uu][0].all())
                P.op("gpsimd", _I("memset", TLB[hh][uu][1].t[:], 0.0), w=TLB[hh][uu][1].all())
        nxt_u = [0] * len(heads)
        act = []
        last = [None] * len(heads)
        while True:
            for hh, h in enumerate(heads):
                infl = sum(1 for a_ in act if a_[1] == hh)
                if nxt_u[hh] < 16 and infl < NU and (last[hh] is None or last[hh][2] >= STAG or last[hh] not in act):
                    u = nxt_u[hh]
                    nxt_u[hh] += 1
                    _conv_some(P, 1)
                    ent = [unit_gen(h, HB[hh], u, TL[hh][u % NU], SS[hh][u % NU], hh * NU + (u % NU), TLB[hh][u % NU]), hh, 0]
                    act.append(ent)
                    last[hh] = ent
            if not act:
                break
            keep = []
            for ent in act:
                try:
                    next(ent[0])
                    ent[2] += 1
                    keep.append(ent)
                except StopIteration:
                    pass
            act = keep
        for hh, h in enumerate(heads):
            P.dma(P.ydn.t[:, h, :], HB[hh]["yT"].t[:], r=HB[hh]["yT"].all(), w=P.ydn.all())
        c.release(mt)
    c.release(m_phase)


DN_NU = 2
```
